# Optimizing a Trainium2 kernel written in Bass

```python
import jax, jax.numpy as jnp
from jax import lax
import numpy as np

D_MODEL = 1024
BATCH = 8
SEQ = 2048
DEPTH = 2

N_EVEN = (DEPTH + 1) // 2
N_ODD = DEPTH // 2
MIX_DIM = D_MODEL
HEAD_DIM = 64
ROPE_THETA = 10000.0
NORM_EPS = 1e-6
NEG_INF = -1e30
BIG = 1e9
NSA_DIM = D_MODEL // 2
N_Q_HEADS = NSA_DIM // HEAD_DIM
GQA = 4
N_KV_HEADS = N_Q_HEADS // GQA
KV_DIM = N_KV_HEADS * HEAD_DIM
CMP_BLOCK = 32
CMP_STRIDE = 16
CMP_HIDDEN = 256
SEL_BLOCK = 64
N_SEL = 8
N_LOCAL = 2
WINDOW = 512
Q_BLOCK = 128
ATTN_SCALE = HEAD_DIM ** -0.5
CONV_DIM = D_MODEL // 2
CONV_WIDTH = 3
EVEN_SIZES = (NSA_DIM,) + (KV_DIM,) * 6 + (N_Q_HEADS * 3,) + (CONV_DIM,) * 3
EVEN_COLS = sum(EVEN_SIZES)
RWKV_DIM = D_MODEL // 2
N_RWKV_HEADS = RWKV_DIM // HEAD_DIM
DECAY_LORA = 64
AAA_LORA = 64
GATE_LORA = 128
RWKV_SIZES = (RWKV_DIM,) * 3 + (DECAY_LORA, AAA_LORA, GATE_LORA)
RWKV_COLS = sum(RWKV_SIZES)
LNX_EPS = 64e-5
POOL_DIM = D_MODEL // 2
POOL_WINDOWS = (2, 4, 8, 16)
POOL_GROUP = POOL_DIM // len(POOL_WINDOWS)
ODD_COLS = RWKV_COLS + POOL_DIM
N_EXPERTS = 16
N_EXPERT_GROUPS = 4
EXPERTS_PER_GROUP = N_EXPERTS // N_EXPERT_GROUPS
GROUP_SCORE_TOPK = 2
TOP_K = 2
D_EXPERT = D_MODEL // 2

kernel_name = 'hybrid_nsa_conv_rwkv7_pool_moe'


def rms_norm(x, g):
    xf = x.astype(jnp.float32)
    y = xf * lax.rsqrt(jnp.mean(xf * xf, axis=-1, keepdims=True) + NORM_EPS)
    return (y * g.astype(jnp.float32)).astype(x.dtype)


def modulate(h, shift, scale):
    return h * (1 + scale[:, None, :]) + shift[:, None, :]


def split_cols(t, sizes):
    bounds = [int(b) for b in np.cumsum(sizes)[:-1]]
    return jnp.split(t, bounds, axis=-1)


def to_heads(t):
    b, s, _ = t.shape
    return t.reshape(b, s, -1, HEAD_DIM)


def rope(t):
    s = t.shape[1]
    half = HEAD_DIM // 2
    inv = ROPE_THETA ** (-jnp.arange(half, dtype=jnp.float32) / half)
    ang = jnp.arange(s, dtype=jnp.float32)[:, None] * inv[None, :]
    cos = jnp.cos(ang)[None, :, None, :]
    sin = jnp.sin(ang)[None, :, None, :]
    tf = t.astype(jnp.float32)
    t1, t2 = tf[..., :half], tf[..., half:]
    return jnp.concatenate([t1 * cos - t2 * sin, t2 * cos + t1 * sin], axis=-1).astype(t.dtype)


def masked_softmax(s, mask):
    p = jax.nn.softmax(jnp.where(mask, s.astype(jnp.float32), NEG_INF), axis=-1)
    return p * mask


def nsa_compressed(q, k_tok, v_tok, cmp_pos, cmp_w1, cmp_w2):
    b, s = q.shape[:2]
    n_cmp = (s - CMP_BLOCK) // CMP_STRIDE + 1
    idx = jnp.arange(n_cmp)[:, None] * CMP_STRIDE + jnp.arange(CMP_BLOCK)[None, :]

    def compress(tok, j):
        blocks = tok[:, idx] + cmp_pos[j][None, None, :, None, :]
        flat = blocks.transpose(0, 1, 3, 2, 4).reshape(b, n_cmp, N_KV_HEADS, CMP_BLOCK * HEAD_DIM)
        return jax.nn.gelu(flat @ cmp_w1[j]) @ cmp_w2[j]

    kc = compress(k_tok, 0)
    vc = compress(v_tok, 1)
    t = jnp.arange(s)
    mask = (jnp.arange(n_cmp) * CMP_STRIDE + CMP_BLOCK - 1)[None, :] <= t[:, None]
    sc = jnp.einsum('bshgd,bchd->bhgsc', q, kc) * ATTN_SCALE
    p = masked_softmax(sc, mask)
    out = jnp.einsum('bhgsc,bchd->bshgd', p.astype(vc.dtype), vc)
    return out, p.sum(axis=2)


def nsa_select(imp):
    s, n_cmp = imp.shape[2], imp.shape[3]
    n_blk = s // SEL_BLOCK
    r = SEL_BLOCK // CMP_STRIDE
    c = CMP_BLOCK // CMP_STRIDE
    need = r * n_blk + c - 1
    imp = jnp.pad(imp, ((0, 0), (0, 0), (0, 0), (0, need - n_cmp)))
    p_slc = jnp.zeros(imp.shape[:3] + (n_blk,), jnp.float32)
    for m in range(r):
        for n in range(c):
            p_slc = p_slc + imp[..., m + n: m + n + r * n_blk: r]
    t = jnp.arange(s)[:, None]
    j = jnp.arange(n_blk)[None, :]
    cur = t // SEL_BLOCK
    valid = j * SEL_BLOCK <= t
    forced = (j == 0) | ((cur - j >= 0) & (cur - j < N_LOCAL))
    score = jnp.where(forced, BIG, jnp.where(valid, p_slc, -BIG))
    top, idx = lax.top_k(score, min(N_SEL, n_blk))
    return idx, top > -0.5 * BIG


def nsa_selected(q, k, v, sel_idx, sel_ok):
    b, s = q.shape[:2]
    n_blk = s // SEL_BLOCK
    n_qb = s // Q_BLOCK
    n_sel = sel_idx.shape[-1]
    kb = k.reshape(b, n_blk, SEL_BLOCK, N_KV_HEADS, HEAD_DIM).transpose(0, 3, 1, 2, 4)
    vb = v.reshape(b, n_blk, SEL_BLOCK, N_KV_HEADS, HEAD_DIM).transpose(0, 3, 1, 2, 4)
    qs = jnp.moveaxis(q.reshape(b, n_qb, Q_BLOCK, N_KV_HEADS, GQA, HEAD_DIM), 1, 0)
    ids = jnp.moveaxis(sel_idx.reshape(b, N_KV_HEADS, n_qb, Q_BLOCK, n_sel), 2, 0)
    oks = jnp.moveaxis(sel_ok.reshape(b, N_KV_HEADS, n_qb, Q_BLOCK, n_sel), 2, 0)
    bi = jnp.arange(b)[:, None, None, None]
    hi = jnp.arange(N_KV_HEADS)[None, :, None, None]

    def one_block(args):
        blk, qb, ib, ok = args
        kg = kb[bi, hi, ib]
        vg = vb[bi, hi, ib]
        t = blk * Q_BLOCK + jnp.arange(Q_BLOCK)
        kpos = ib[..., None] * SEL_BLOCK + jnp.arange(SEL_BLOCK)
        mask = ok[..., None] & (kpos <= t[:, None, None])
        mask = mask.reshape(b, N_KV_HEADS, 1, Q_BLOCK, n_sel * SEL_BLOCK)
        sc = jnp.einsum('bqhgd,bhqnkd->bhgqnk', qb, kg).reshape(
            b, N_KV_HEADS, GQA, Q_BLOCK, n_sel * SEL_BLOCK) * ATTN_SCALE
        p = masked_softmax(sc, mask).reshape(b, N_KV_HEADS, GQA, Q_BLOCK, n_sel, SEL_BLOCK)
        return jnp.einsum('bhgqnk,bhqnkd->bqhgd', p.astype(vg.dtype), vg)

    out = lax.map(one_block, (jnp.arange(n_qb), qs, ids, oks))
    return jnp.moveaxis(out, 0, 1).reshape(b, s, N_KV_HEADS, GQA, HEAD_DIM)


def nsa_window(q, k, v):
    b, s = q.shape[:2]
    kp = jnp.pad(k, ((0, 0), (WINDOW, 0), (0, 0), (0, 0)))
    vp = jnp.pad(v, ((0, 0), (WINDOW, 0), (0, 0), (0, 0)))
    span = WINDOW + Q_BLOCK

    def one_block(blk):
        start = blk * Q_BLOCK
        qb = lax.dynamic_slice_in_dim(q, start, Q_BLOCK, axis=1)
        kw = lax.dynamic_slice_in_dim(kp, start, span, axis=1)
        vw = lax.dynamic_slice_in_dim(vp, start, span, axis=1)
        t = start + jnp.arange(Q_BLOCK)
        s_pos = start - WINDOW + jnp.arange(span)
        diff = t[:, None] - s_pos[None, :]
        mask = (diff >= 0) & (diff < WINDOW) & (s_pos[None, :] >= 0)
        sc = jnp.einsum('bqhgd,bkhd->bhgqk', qb, kw) * ATTN_SCALE
        p = masked_softmax(sc, mask)
        return jnp.einsum('bhgqk,bkhd->bqhgd', p.astype(vw.dtype), vw)

    out = lax.map(one_block, jnp.arange(s // Q_BLOCK))
    return jnp.moveaxis(out, 0, 1).reshape(b, s, N_KV_HEADS, GQA, HEAD_DIM)


def short_conv(xb, b_gate, c_gate, conv_w):
    s = xb.shape[1]
    u = jnp.pad(c_gate * xb, ((0, 0), (CONV_WIDTH - 1, 0), (0, 0)))
    y = conv_w[CONV_WIDTH - 1] * u[:, CONV_WIDTH - 1: CONV_WIDTH - 1 + s]
    for kk in range(CONV_WIDTH - 1):
        y = y + conv_w[kk] * u[:, kk: kk + s]
    return b_gate * y


def nsa_conv_mixer(h, w_in, cmp_pos, cmp_w1, cmp_w2, conv_w, w_out):
    b, s, _ = h.shape
    q, kc, vc, ks, vs, kw, vw, gl, xb, bg, cg = split_cols(h @ w_in, EVEN_SIZES)
    q_h = to_heads(q)
    q_nope = q_h.reshape(b, s, N_KV_HEADS, GQA, HEAD_DIM)
    q_rot = rope(q_h).reshape(b, s, N_KV_HEADS, GQA, HEAD_DIM)
    o_cmp, imp = nsa_compressed(q_nope, to_heads(kc), to_heads(vc), cmp_pos, cmp_w1, cmp_w2)
    sel_idx, sel_ok = nsa_select(imp)
    o_slc = nsa_selected(q_rot, rope(to_heads(ks)), to_heads(vs), sel_idx, sel_ok)
    o_win = nsa_window(q_rot, rope(to_heads(kw)), to_heads(vw))
    g = jax.nn.sigmoid(gl).reshape(b, s, N_KV_HEADS, GQA, 3)
    o_nsa = (g[..., 0:1] * o_cmp + g[..., 1:2] * o_slc + g[..., 2:3] * o_win).reshape(b, s, NSA_DIM)
    y_conv = short_conv(xb, bg, cg, conv_w)
    return jnp.concatenate([o_nsa.astype(h.dtype), y_conv.astype(h.dtype)], axis=-1) @ w_out


def rwkv7_scan(r, w, k, v, a, bb):
    b, s, h, n = r.shape

    def step(state, inp):
        r_t, w_t, k_t, v_t, a_t, b_t = inp
        sa = jnp.einsum('bhij,bhj->bhi', state, a_t)
        state = (state * w_t[:, :, None, :] + sa[..., None] * b_t[:, :, None, :]
                 + v_t[..., None] * k_t[:, :, None, :])
        return state, jnp.einsum('bhij,bhj->bhi', state, r_t)

    xs = tuple(jnp.moveaxis(t, 1, 0) for t in (r, w, k, v, a, bb))
    _, ys = lax.scan(step, jnp.zeros((b, h, n, n), jnp.float32), xs)
    return jnp.moveaxis(ys, 0, 1)


def multiscale_pool(u, pool_w, pool_scale):
    b, s, _ = u.shape
    ug = u.reshape(b, s, len(POOL_WINDOWS), POOL_GROUP).astype(jnp.float32)
    cs = jnp.pad(jnp.cumsum(ug, axis=1), ((0, 0), (1, 0), (0, 0), (0, 0)))
    end = jnp.arange(1, s + 1)
    outs = []
    for gi, win in enumerate(POOL_WINDOWS):
        start = jnp.maximum(end - win, 0)
        mean = (cs[:, end, gi] - cs[:, start, gi]) / (end - start).astype(jnp.float32)[None, :, None]
        outs.append(mean - ug[:, :, gi])
    pooled = jnp.stack(outs, axis=2).astype(u.dtype)
    mixed = jnp.einsum('bsgc,gcd->bsgd', pooled, pool_w).reshape(b, s, POOL_DIM)
    return mixed * pool_scale


def rwkv_pool_mixer(h, w_in, mu, w0, w2, a0, a2, g2, k_k, k_a, r_k, lnx_w, lnx_b,
                    pool_w, pool_scale, w_out):
    b, s, _ = h.shape
    f32 = jnp.float32
    proj = h @ w_in
    rw, u = proj[..., :RWKV_COLS], proj[..., RWKV_COLS:]
    rw_prev = jnp.pad(rw, ((0, 0), (1, 0), (0, 0)))[:, :s]
    rw = rw + (rw_prev - rw) * mu
    r, k, v, wl, al, gl = split_cols(rw, RWKV_SIZES)
    w_log = -jax.nn.softplus(-(w0 + jnp.tanh(wl) @ w2).astype(f32)) - 0.5
    decay = jnp.exp(-jnp.exp(w_log))
    a = jax.nn.sigmoid((a0 + al @ a2).astype(f32))
    g = jax.nn.sigmoid(gl) @ g2
    k_mod = k.astype(f32) * (1 + (a - 1) * k_a.astype(f32))

    def hd(t):
        return t.astype(f32).reshape(b, s, N_RWKV_HEADS, HEAD_DIM)

    kk = hd(k * k_k)
    kk = kk / jnp.maximum(jnp.sqrt(jnp.sum(kk * kk, axis=-1, keepdims=True)), 1e-12)
    r_h, k_h, v_h = hd(r), hd(k_mod), hd(v)
    y = rwkv7_scan(r_h, hd(decay), k_h, v_h, -kk, kk * hd(a))
    mean = jnp.mean(y, axis=-1, keepdims=True)
    var = jnp.mean(jnp.square(y - mean), axis=-1, keepdims=True)
    y = ((y - mean) * lax.rsqrt(var + LNX_EPS)).reshape(b, s, RWKV_DIM) * lnx_w + lnx_b
    bonus = jnp.sum(r_h * k_h * r_k, axis=-1, keepdims=True) * v_h
    o_rwkv = ((y + bonus.reshape(b, s, RWKV_DIM)) * g).astype(h.dtype)
    o_pool = multiscale_pool(u, pool_w, pool_scale).astype(h.dtype)
    return jnp.concatenate([o_rwkv, o_pool], axis=-1) @ w_out


def grouped_moe(h, router_w, router_b, w_gate, w_up, w_down):
    b, s, d = h.shape
    t = h.reshape(b * s, d)
    scores = jax.nn.sigmoid((t @ router_w).astype(jnp.float32))
    biased = scores + router_b.astype(jnp.float32)
    grp = biased.reshape(-1, N_EXPERT_GROUPS, EXPERTS_PER_GROUP)
    grp_score = lax.top_k(grp, GROUP_SCORE_TOPK)[0].sum(axis=-1)
    top_grp = jnp.argmax(grp_score, axis=-1)
    in_group = (jnp.arange(N_EXPERTS) // EXPERTS_PER_GROUP)[None, :] == top_grp[:, None]
    _, idx = lax.top_k(jnp.where(in_group, biased, NEG_INF), TOP_K)
    w_sel = jnp.take_along_axis(scores, idx, axis=-1)
    w_sel = w_sel / jnp.sum(w_sel, axis=-1, keepdims=True)
    gates = jnp.sum(jax.nn.one_hot(idx, N_EXPERTS, dtype=jnp.float32) * w_sel[..., None], axis=1)
    gates = gates.astype(t.dtype)
    out = jnp.zeros_like(t)
    for e in range(N_EXPERTS):
        he = jax.nn.silu(t @ w_gate[e]) * (t @ w_up[e])
        out = out + gates[:, e:e + 1] * (he @ w_down[e])
    return out.reshape(b, s, d)


def setup_inputs(seed: int = 0) -> dict:
    key = jax.random.key(seed)
    ks = iter(jax.random.split(key, 48))
    f32 = jnp.float32

    def nrm(shape, scale):
        return scale * jax.random.normal(next(ks), shape, f32)

    def uni(shape, lo, hi):
        return jax.random.uniform(next(ks), shape, f32, lo, hi)

    D = D_MODEL
    return {
        'x': nrm((BATCH, SEQ, D), 1.0),
        'c': nrm((BATCH, D), 1.0),
        'ada_w': nrm((DEPTH, D, 6 * D), 0.5 * D ** -0.5),
        'ada_b': nrm((DEPTH, 6 * D), 0.02),
        'norm_mix': 1.0 + nrm((DEPTH, D), 0.05),
        'norm_ffn': 1.0 + nrm((DEPTH, D), 0.05),
        'even_w_in': nrm((N_EVEN, D, EVEN_COLS), D ** -0.5),
        'even_cmp_pos': nrm((N_EVEN, 2, CMP_BLOCK, HEAD_DIM), 0.02),
        'even_cmp_w1': nrm((N_EVEN, 2, CMP_BLOCK * HEAD_DIM, CMP_HIDDEN), (CMP_BLOCK * HEAD_DIM) ** -0.5),
        'even_cmp_w2': nrm((N_EVEN, 2, CMP_HIDDEN, HEAD_DIM), CMP_HIDDEN ** -0.5),
        'even_conv_w': nrm((N_EVEN, CONV_WIDTH, CONV_DIM), CONV_WIDTH ** -0.5),
        'even_w_out': nrm((N_EVEN, MIX_DIM, D), MIX_DIM ** -0.5),
        'odd_w_in': nrm((N_ODD, D, ODD_COLS), D ** -0.5),
        'odd_mu': uni((N_ODD, RWKV_COLS), 0.0, 1.0),
        'odd_w0': uni((N_ODD, RWKV_DIM), -6.0, -1.0),
        'odd_w2': nrm((N_ODD, DECAY_LORA, RWKV_DIM), 0.1),
        'odd_a0': nrm((N_ODD, RWKV_DIM), 0.1),
        'odd_a2': nrm((N_ODD, AAA_LORA, RWKV_DIM), AAA_LORA ** -0.5),
        'odd_g2': nrm((N_ODD, GATE_LORA, RWKV_DIM), GATE_LORA ** -0.5),
        'odd_k_k': 0.85 + nrm((N_ODD, RWKV_DIM), 0.05),
        'odd_k_a': 1.0 + nrm((N_ODD, RWKV_DIM), 0.05),
        'odd_r_k': nrm((N_ODD, N_RWKV_HEADS, HEAD_DIM), 0.1),
        'odd_lnx_w': 1.0 + nrm((N_ODD, RWKV_DIM), 0.05),
        'odd_lnx_b': nrm((N_ODD, RWKV_DIM), 0.02),
        'odd_pool_w': nrm((N_ODD, len(POOL_WINDOWS), POOL_GROUP, POOL_GROUP), POOL_GROUP ** -0.5),
        'odd_pool_scale': 1.0 + nrm((N_ODD, POOL_DIM), 0.1),
        'odd_w_out': nrm((N_ODD, MIX_DIM, D), MIX_DIM ** -0.5),
        'router_w': nrm((D, N_EXPERTS), D ** -0.5),
        'router_b': nrm((N_EXPERTS,), 0.01),
        'moe_w_gate': nrm((DEPTH, N_EXPERTS, D, D_EXPERT), D ** -0.5),
        'moe_w_up': nrm((DEPTH, N_EXPERTS, D, D_EXPERT), D ** -0.5),
        'moe_w_down': nrm((DEPTH, N_EXPERTS, D_EXPERT, D), D_EXPERT ** -0.5),
        'final_norm': 1.0 + nrm((D,), 0.05),
    }


def reference(x, c, ada_w, ada_b, norm_mix, norm_ffn,
              even_w_in, even_cmp_pos, even_cmp_w1, even_cmp_w2, even_conv_w, even_w_out,
              odd_w_in, odd_mu, odd_w0, odd_w2, odd_a0, odd_a2, odd_g2, odd_k_k, odd_k_a, odd_r_k,
              odd_lnx_w, odd_lnx_b, odd_pool_w, odd_pool_scale, odd_w_out,
              router_w, router_b, moe_w_gate, moe_w_up, moe_w_down, final_norm):
    cond = jax.nn.silu(c)
    for layer in range(DEPTH):
        mod = cond @ ada_w[layer] + ada_b[layer]
        sh1, sc1, g1, sh2, sc2, g2 = jnp.split(mod, 6, axis=-1)
        h = modulate(rms_norm(x, norm_mix[layer]), sh1, sc1)
        i = layer // 2
        if layer % 2 == 0:
            y = nsa_conv_mixer(h, even_w_in[i], even_cmp_pos[i], even_cmp_w1[i], even_cmp_w2[i],
                               even_conv_w[i], even_w_out[i])
        else:
            y = rwkv_pool_mixer(h, odd_w_in[i], odd_mu[i], odd_w0[i], odd_w2[i], odd_a0[i], odd_a2[i],
                                odd_g2[i], odd_k_k[i], odd_k_a[i], odd_r_k[i], odd_lnx_w[i], odd_lnx_b[i],
                                odd_pool_w[i], odd_pool_scale[i], odd_w_out[i])
        x = x + g1[:, None, :] * y
        h = modulate(rms_norm(x, norm_ffn[layer]), sh2, sc2)
        x = x + g2[:, None, :] * grouped_moe(h, router_w, router_b, moe_w_gate[layer],
                                             moe_w_up[layer], moe_w_down[layer])
    return rms_norm(x, final_norm)
```

```python
import os
import numpy as np
from contextlib import ExitStack
import concourse.bass as bass
import concourse.mybir as mybir
from concourse.bass_utils import run_bass_kernel_spmd

F32 = mybir.dt.float32
BF16 = mybir.dt.bfloat16
AF = mybir.ActivationFunctionType
ALU = mybir.AluOpType
AX = mybir.AxisListType

S = 2048
D = 1024
NT = S // 128
NS_DMA = 8
EPS = 1e-6
NEGB = -30000.0


class Buf:
    __slots__ = ("name", "w", "r", "excl")

    def __init__(self, name, excl=False):
        self.name = name
        self.w = None
        self.r = {}
        self.excl = excl


class Kern:
    def __init__(self, nc, es):
        self.nc = nc
        self.es = es
        self.eng = {"pe": nc.tensor, "dve": nc.vector, "act": nc.scalar, "pool": nc.gpsimd, "sp": nc.sync}
        self.sem = {n: es.enter_context(nc.semaphore("s_" + n)) for n in self.eng}
        self.cnt = {n: 0 for n in self.eng}
        self.seen = {n: {} for n in self.eng}
        self.dsem = {q: [es.enter_context(nc.semaphore("d_%s%d" % (q, i))) for i in range(NS_DMA)]
                     for q in ("sp", "pool", "act")}
        self.duse = {q: [0] * NS_DMA for q in self.dsem}
        self.dnext = {q: 0 for q in self.dsem}
        self.uid = 0
        self.banks = []
        self.bank_i = 0
        self.bank_rng = (0, 8)

    def wait(self, e, tok):
        _, key, h, v = tok
        if self.seen[e].get(key, 0) < v:
            self.eng[e].wait_ge(h, v)
            self.seen[e][key] = v

    def deps(self, e, r, w):
        toks = []
        for b in r:
            if b.w is not None and not (b.w[0] == e and e == "pe"):
                toks.append(b.w)
            if b.excl:
                for t in b.r.values():
                    if t[0] != e:
                        toks.append(t)
        for b in w:
            if b.w is not None and (b.w[0] != e or e != "pe"):
                toks.append(b.w)
            for t in b.r.values():
                if t[0] != e or e != "pe":
                    toks.append(t)
        return toks

    def op(self, e, fn, r=(), w=()):
        for t in self.deps(e, r, w):
            self.wait(e, t)
        ins = fn(self.eng[e])
        self.cnt[e] += 1
        ins.then_inc(self.sem[e], 1)
        T = (e, "s_" + e, self.sem[e], self.cnt[e])
        for b in r:
            b.r[e] = T
        for b in w:
            b.w = T
            b.r = {}
        return T

    def dma(self, out, in_, r=(), w=(), q="sp"):
        for t in self.deps("dma", r, w):
            self.wait(q, t)
        i = self.dnext[q]
        self.dnext[q] = (i + 1) % NS_DMA
        u = self.duse[q][i]
        key = "d_%s%d" % (q, i)
        if u > 0:
            self.wait(q, ("dma", key, self.dsem[q][i], 16 * u))
        ins = self.eng[q].dma_start(out=out, in_=in_)
        ins.then_inc(self.dsem[q][i], 16)
        self.duse[q][i] = u + 1
        T = ("dma", key, self.dsem[q][i], 16 * (u + 1))
        self.uid += 1
        for b in r:
            b.r[("dma", self.uid)] = T
        for b in w:
            b.w = T
            b.r = {}
        return T

    def barrier(self):
        toks = [(n, "s_" + n, self.sem[n], self.cnt[n]) for n in self.eng if self.cnt[n] > 0]
        for q in self.dsem:
            for i in range(NS_DMA):
                if self.duse[q][i] > 0:
                    toks.append(("dma", "d_%s%d" % (q, i), self.dsem[q][i], 16 * self.duse[q][i]))
        for e in self.eng:
            for t in toks:
                self.wait(e, t)

    def finish(self):
        for q in self.dsem:
            for i in range(NS_DMA):
                if self.duse[q][i] > 0:
                    self.wait("sp", ("dma", "d_%s%d" % (q, i), self.dsem[q][i], 16 * self.duse[q][i]))

    def bank(self):
        lo, hi = self.bank_rng
        if not (lo <= self.bank_i < hi):
            self.bank_i = lo
        b = self.banks[self.bank_i]
        self.bank_i += 1
        if self.bank_i >= hi:
            self.bank_i = lo
        return b


class Arena:
    def __init__(self, ap, nbytes):
        self.ap = ap
        self.n = nbytes
        self.off = 0
        self.marks = []

    def alloc(self, nbytes, dtype=F32, shape=None):
        nbytes = (nbytes + 31) // 32 * 32
        assert self.off + nbytes <= self.n, ("arena overflow", self.off, nbytes, self.n)
        v = self.ap[:, self.off // 4:(self.off + nbytes) // 4]
        self.off += nbytes
        if dtype != F32:
            v = v.bitcast(dtype)
        return v

    def alloc_top(self, nbytes, dtype=F32):
        nbytes = (nbytes + 31) // 32 * 32
        self.n -= nbytes
        assert self.off <= self.n, ("arena overflow(top)", self.off, nbytes, self.n)
        v = self.ap[:, self.n // 4:(self.n + nbytes) // 4]
        if dtype != F32:
            v = v.bitcast(dtype)
        return v

    def release_top(self, nbytes):
        self.n += (nbytes + 31) // 32 * 32

    def mark(self):
        self.marks.append(self.off)

    def release(self):
        self.off = self.marks.pop()


def f32(a, n):
    return a.alloc(4 * n, F32)[:, 0:n]


def bf(a, n):
    return a.alloc(2 * n, BF16)[:, 0:n]


def build_program(stages=("mix0", "moe0", "mix1", "moe1", "final"), dbg=None):
    nc = bass.Bass("TRN2", target_bir_lowering=False)
    es = ExitStack()
    with es:
        P = _Prog(nc, es, stages, dbg)
        P.emit()
    return nc


class _Prog:
    def __init__(self, nc, es, stages, dbg):
        self.nc = nc
        self.es = es
        self.stages = stages
        self.dbg = dbg
        self.K = Kern(nc, es)
        di = lambda name, shape: nc.dram_tensor(name, list(shape), F32, kind="ExternalInput").ap()
        self.x_in = di("x", [S, D])
        self.c_in = di("c", [128, 8])
        self.ada_w = di("ada_w", [2, 12, 128, 8, 512])
        self.ada_b = di("ada_b", [2, 6 * D])
        self.norm_mix = di("norm_mix", [2, D])
        self.norm_ffn = di("norm_ffn", [2, D])
        self.final_norm = di("final_norm", [1, D])
        self.router_w = di("router_w", [128, 8, 16])
        self.router_b = di("router_b", [1, 16])
        self.moe_wg = di("moe_w_gate", [2, 16, D, 512])
        self.moe_wu = di("moe_w_up", [2, 16, D, 512])
        self.moe_wd = di("moe_w_down", [2, 16, 512, D])
        self.ident_in = di("ident", [128, 128])
        dib = lambda name, shape: nc.dram_tensor(name, list(shape), BF16, kind="ExternalInput").ap()
        self.even_w = di("even_w", [N_EVEN_BLK, 128, 8, 128])
        self.even_w_out = di("even_w_out", [D, D])
        self.conv_w = di("conv_w", [128, 4, 3])
        self.rope_cos = di("rope_cos", [128, S])
        self.rope_sin = di("rope_sin", [128, S])
        self.cmp_pos = di("cmp_pos", [128, 2, 32])
        self.cmp_w1 = di("cmp_w1", [2, 4, 128, 8, 256])
        self.cmp_w2 = di("cmp_w2", [128, 2, 2, 64])
        self.selmask_in = dib("selmask", [128, 3, NT, 32])
        self.expand_in = dib("expand", [32, 16, 128])
        self.wbias_in = dib("wbias", [128, 8, 512])
        self.identb_in = dib("identb", [128, 128])
        self.cmpbias_in = dib("cmpbias", [128, NT, 127])
        self.odd_w = di("odd_w", [N_ODD_BLK, 128, 8, 128])
        self.odd_w_out = di("odd_w_out", [D, D])
        self.pool_w = di("pool_w", [4, 128, 128])
        self.pool_scale = di("pool_scale", [128, 4])
        self.invc_in = di("invc", [128, 4, 16])
        self.chv_in = di("chv", [128, 7, 4])
        self.mu_in = di("mu", [128, 14])
        self.blockones_in = di("blockones", [128, 128])
        self.rmasks_in = di("rmasks", [128, 3, 128])
        self.wa2_in = di("wa2", [128, 512])
        self.g2_in = di("g2", [128, 512])
        self.out = nc.dram_tensor("out", [S, D], F32, kind="ExternalOutput").ap()
        self.modscr = nc.dram_tensor("modscr", [2, 6 * D], F32, kind="Internal").ap()

    def emit(self):
        nc, K, es = self.nc, self.K, self.es
        ARENA_BYTES = 206 * 1024
        arena_t = es.enter_context(nc.sbuf_tensor("arena", [128, ARENA_BYTES // 4], F32))
        self.A = A = Arena(arena_t, ARENA_BYTES)
        for i in range(8):
            pt = es.enter_context(nc.psum_tensor("bank%d" % i, [128, 512], F32))
            K.banks.append((pt, Buf("bank%d" % i, excl=True)))

        self.x_sb = f32(A, NT * D).rearrange("p (t d) -> p t d", t=NT)
        self.xb = [Buf("x%d" % t) for t in range(NT)]
        self.ident = f32(A, 128)
        self.b_ident = Buf("ident")
        self.ones_row = f32(A, 128)
        self.b_ones = Buf("ones")
        self.modrow = f32(A, 0)
        self.bcall = f32(A, 3 * D)
        self.bc = [self.bcall[:, i * D:(i + 1) * D] for i in range(3)]
        self.b_bc = [Buf("bc%d" % i) for i in range(3)]
        self.small = f32(A, 64)
        self.b_small = Buf("small")

        K.dma(self.ident, self.ident_in, w=[self.b_ident])
        K.op("dve", lambda e: e.memset(self.ones_row, 1.0), w=[self.b_ones])
        xin = self.x_in.rearrange("(t p) d -> p t d", p=128)
        for t in range(NT):
            K.dma(self.x_sb[:, t, :], xin[:, t, :], w=[self.xb[t]])

        self.compute_mod()

        for st in self.stages:
            if st == "mix0":
                self.even_mixer()
            elif st == "mix1":
                self.odd_mixer()
            elif st.startswith("moe"):
                self.moe_layer(int(st[3]))
            elif st == "final":
                self.final()
        if "final" not in self.stages:
            oview = self.out.rearrange("(t p) d -> p t d", p=128)
            for t in range(NT):
                K.dma(oview[:, t, :], self.x_sb[:, t, :], r=[self.xb[t]])
        K.finish()

    def compute_mod(self):
        nc, K, A = self.nc, self.K, self.A
        A.mark()
        cT = f32(A, 8)
        b_c = Buf("cT")
        row = f32(A, 6 * D)
        b_row = Buf("row")
        brow = f32(A, 6 * D)
        b_brow = Buf("brow")
        stg = [f32(A, 4096).rearrange("p (k n) -> p k n", k=8) for _ in range(2)]
        b_stg = [Buf("mstg0"), Buf("mstg1")]
        K.dma(cT, self.c_in, w=[b_c])
        K.op("act", lambda e: e.activation(out=cT, in_=cT, func=AF.Silu), r=[b_c], w=[b_c])
        j = 0
        for l in range(2):
            K.dma(brow[0:1, :], self.ada_b[l:l + 1, :], w=[b_brow])
            for nb in range(12):
                s_, bs_ = stg[j % 2], b_stg[j % 2]
                j += 1
                K.dma(s_, self.ada_w[l, nb], w=[bs_])
                pt, pb = K.bank()
                for kc in range(8):
                    K.op("pe", lambda e, kc=kc, s_=s_, pt=pt: e.matmul(
                        pt[0:1, :], lhsT=cT[:, kc:kc + 1], rhs=s_[:, kc, :], start=(kc == 0), stop=(kc == 7)),
                        r=[b_c, bs_], w=[pb])
                K.op("dve", lambda e, pt=pt, nb=nb: e.tensor_tensor(
                    out=row[0:1, nb * 512:(nb + 1) * 512], in0=pt[0:1, :], in1=brow[0:1, nb * 512:(nb + 1) * 512],
                    op=ALU.add), r=[pb, b_brow], w=[b_row])
            K.dma(self.modscr[l:l + 1, :], row[0:1, :], r=[b_row])
        self.b_modscr = Buf("modscr")
        K.barrier()
        A.release()

    def bcast_row(self, dst, b_dst, row_ap, b_row):
        K = self.K
        for h in range(2):
            pt, pb = K.bank()
            K.op("pe", lambda e, pt=pt, h=h: e.matmul(pt[:, :], lhsT=self.ones_row[0:1, :],
                                                       rhs=row_ap[0:1, h * 512:(h + 1) * 512], start=True, stop=True),
                 r=[self.b_ones, b_row], w=[pb])
            K.op("act", lambda e, pt=pt, h=h: e.copy(out=dst[:, h * 512:(h + 1) * 512], in_=pt[:, :]),
                 r=[pb], w=[b_dst])

    def load_mod_bc(self, l, which, norm_w_ap):
        K, A = self.K, self.A
        A.mark()
        rows = f32(A, 4 * D)
        b_rows = Buf("modrows")
        base = which * 3 * D
        K.dma(rows[0:1, 0:3 * D], self.modscr[l:l + 1, base:base + 3 * D], w=[b_rows])
        K.dma(rows[0:1, 3 * D:4 * D], norm_w_ap, w=[b_rows])
        K.op("dve", lambda e: e.scalar_tensor_tensor(out=rows[0:1, D:2 * D], in0=rows[0:1, D:2 * D], scalar=1.0,
                                                     in1=rows[0:1, 3 * D:4 * D], op0=ALU.add, op1=ALU.mult),
             r=[b_rows], w=[b_rows])
        self.bcast_row(self.bc[0], self.b_bc[0], rows[0:1, D:2 * D], b_rows)
        self.bcast_row(self.bc[1], self.b_bc[1], rows[0:1, 0:D], b_rows)
        self.bcast_row(self.bc[2], self.b_bc[2], rows[0:1, 2 * D:3 * D], b_rows)
        K.barrier()
        A.release()

    def norm_tile(self, t, htmp, b_htmp, scr, b_scr, stat, b_stat):
        K = self.K
        xt = self.x_sb[:, t, :]
        K.op("act", lambda e: e.activation(out=scr, in_=xt, func=AF.Square, accum_out=stat[:, 0:1]),
             r=[self.xb[t]], w=[b_scr, b_stat])
        cut = os.environ.get("KCUT", "z")
        if cut == "a": return
        K.op("act", lambda e: e.activation(out=stat[:, 1:2], in_=stat[:, 0:1], func=AF.Sqrt, scale=1.0 / D, bias=self.epsb),
             r=[b_stat, self.b_small], w=[b_stat])
        if cut == "b": return
        K.op("dve", lambda e: e.reciprocal(out=stat[:, 2:3], in_=stat[:, 1:2]), r=[b_stat], w=[b_stat])
        if cut == "c": return
        K.op("dve", lambda e: e.scalar_tensor_tensor(out=htmp, in0=xt, scalar=stat[:, 2:3], in1=self.bc[0],
                                                     op0=ALU.mult, op1=ALU.mult),
             r=[self.xb[t], b_stat, self.b_bc[0]], w=[b_htmp])
        if cut == "d": return
        K.op("dve", lambda e: e.tensor_tensor(out=htmp, in0=htmp, in1=self.bc[1], op=ALU.add),
             r=[b_htmp, self.b_bc[1]], w=[b_htmp])

    def make_eps(self):
        K = self.K
        self.epsb = self.small[:, 0:1]
        K.op("dve", lambda e: e.memset(self.epsb, EPS), w=[self.b_small])

    def moe_layer(self, l):
        nc, K, A = self.nc, self.K, self.A
        if not hasattr(self, "epsb"):
            self.make_eps()
        self.load_mod_bc(l, 1, self.norm_ffn[l:l + 1, :])
        if os.environ.get("KDBG") == "modbc":
            for t in range(3):
                K.op("dve", lambda e, t=t: e.tensor_copy(out=self.x_sb[:, t, :], in_=self.bc[t]), r=[self.b_bc[t]], w=[self.xb[t]])
            return
        A.mark()
        hT = bf(A, 8 * S).rearrange("p (k s) -> p k s", k=8)
        b_hT = [Buf("hT%d" % t) for t in range(NT)]
        logits = f32(A, NT * 16).rearrange("p (t e) -> p t e", t=NT)
        b_log = Buf("logits")
        gates = f32(A, NT * 16).rearrange("p (t e) -> p t e", t=NT)
        b_gates = Buf("gates")
        rw = f32(A, 8 * 16).rearrange("p (k e) -> p k e", k=8)
        b_rw = Buf("rw")
        rb = f32(A, 16)
        b_rb = Buf("rb")
        K.dma(rw, self.router_w, w=[b_rw])
        rbrow = f32(A, 16)
        b_rbrow = Buf("rbrow")
        K.dma(rbrow[0:1, :], self.router_b, w=[b_rbrow])
        pt, pb = K.bank()
        K.op("pe", lambda e: e.matmul(pt[:, 0:16], lhsT=self.ones_row[0:1, :], rhs=rbrow[0:1, :], start=True, stop=True),
             r=[self.b_ones, b_rbrow], w=[pb])
        K.op("act", lambda e: e.copy(out=rb, in_=pt[:, 0:16]), r=[pb], w=[b_rb])

        A.mark()
        htmp = [f32(A, D) for _ in range(2)]
        b_htmp = [Buf("htmp0"), Buf("htmp1")]
        scr = f32(A, D)
        b_scr = Buf("scr")
        stats = [f32(A, 4) for _ in range(2)]
        b_stats = [Buf("st0"), Buf("st1")]
        hTf = [f32(A, 8 * 128).rearrange("p (k s) -> p k s", k=8) for _ in range(2)]
        b_hTf = [Buf("hTf0"), Buf("hTf1")]
        for t in range(NT):
            i = t % 2
            self.norm_tile(t, htmp[i], b_htmp[i], scr, b_scr, stats[i], b_stats[i])
            if os.environ.get("KDBG") == "n1":
                K.op("dve", lambda e, t=t, i=i: e.tensor_copy(out=self.x_sb[:, t, :], in_=htmp[i]), r=[b_htmp[i]], w=[self.xb[t]])
                continue
            for hf in range(2):
                pt, pb = K.bank()
                for kk in range(4):
                    kc = hf * 4 + kk
                    K.op("pe", lambda e, pt=pt, kk=kk, kc=kc, i=i: e.transpose(
                        out=pt[:, kk * 128:(kk + 1) * 128], in_=htmp[i][:, kc * 128:(kc + 1) * 128], identity=self.ident),
                        r=[b_htmp[i], self.b_ident], w=[pb])
                K.op("act", lambda e, pt=pt, hf=hf, i=i: e.copy(
                    out=hTf[i][:, hf * 4:(hf + 1) * 4, :], in_=pt[:, :].rearrange("p (k s) -> p k s", k=4)),
                    r=[pb], w=[b_hTf[i]])
                K.op("dve", lambda e, hf=hf, t=t, i=i: e.tensor_copy(
                    out=hT[:, hf * 4:(hf + 1) * 4, t * 128:(t + 1) * 128], in_=hTf[i][:, hf * 4:(hf + 1) * 4, :]),
                    r=[b_hTf[i]], w=[b_hT[t]])
            if os.environ.get("KCUT") == "t":
                continue
            pt, pb = K.bank()
            for kc in range(8):
                K.op("pe", lambda e, pt=pt, kc=kc, i=i: e.matmul(pt[:, 0:16], lhsT=hTf[i][:, kc, :], rhs=rw[:, kc, :],
                                                               start=(kc == 0), stop=(kc == 7)),
                     r=[b_hTf[i], b_rw], w=[pb])
            K.op("act", lambda e, pt=pt, t=t: e.activation(out=logits[:, t, :], in_=pt[:, 0:16], func=AF.Sigmoid),
                 r=[pb], w=[b_log])
        K.barrier()
        A.release()
        if os.environ.get("KDBG") in ("norm", "n1"):
            A.release(); return

        A.mark()
        self.routing(logits, b_log, rb, b_rb, gates, b_gates)
        A.release()
        if os.environ.get("KDBG") == "route":
            for t in range(NT):
                K.op("dve", lambda e, t=t: e.tensor_copy(out=self.x_sb[:, t, 0:16], in_=gates[:, t, :]), r=[b_gates], w=[self.xb[t]])
                K.op("dve", lambda e, t=t: e.tensor_copy(out=self.x_sb[:, t, 16:32], in_=logits[:, t, :]), r=[b_log], w=[self.xb[t]])
            K.barrier(); A.release(); return
        NEXP = int(os.environ.get("KNEXP", "16"))

        A.mark()
        NW = 4
        wbf = [bf(A, 4096) for _ in range(NW)]
        b_wbf = [Buf("wbf%d" % i) for i in range(NW)]
        stg = [f32(A, 4096) for _ in range(2)]
        b_stg = [Buf("stg0"), Buf("stg1")]
        heT = bf(A, 4 * S).rearrange("p (c s) -> p c s", c=4)
        b_heT = [[Buf("heT%d_%d" % (c, g)) for g in range(4)] for c in range(4)]
        sil = [f32(A, 512) for _ in range(2)]
        b_sil = [Buf("sil0"), Buf("sil1")]
        g2bc = self.bc[2]
        st = {"w": 0, "s": 0, "sil": 0}

        def load_w(kind, e):
            wi = st["w"] % NW
            st["w"] += 1
            si = st["s"] % 2
            st["s"] += 1
            if kind == 2:
                src = self.moe_wd[l, e].rearrange("(c p) n -> p c n", p=128)
                sv = stg[si].rearrange("p (c n) -> p c n", c=4)
                K.dma(sv, src, w=[b_stg[si]])
                wv = wbf[wi].rearrange("p (c n) -> p c n", c=4)
                for c in range(4):
                    K.op("pool", lambda e_, c=c: e_.tensor_tensor(out=wv[:, c, :], in0=sv[:, c, :], in1=g2bc, op=ALU.mult),
                         r=[b_stg[si], self.b_bc[2]], w=[b_wbf[wi]])
                return wv, b_wbf[wi]
            else:
                src = (self.moe_wg if kind == 0 else self.moe_wu)[l, e].rearrange("(k p) n -> p k n", p=128)
                sv = stg[si].rearrange("p (k n) -> p k n", k=8)
                K.dma(sv, src, w=[b_stg[si]])
                wv = wbf[wi].rearrange("p (k n) -> p k n", k=8)
                K.op("pool", lambda e_: e_.tensor_copy(out=wbf[wi], in_=stg[si]), r=[b_stg[si]], w=[b_wbf[wi]])
                return wv, b_wbf[wi]

        nxt = [load_w(0, 0), load_w(1, 0), load_w(2, 0)]
        for e in range(NEXP):
            (wg, b_wg), (wu, b_wu), (wd, b_wd) = nxt
            for c in range(4):
                for g in range(4):
                    pg, pgb = K.bank()
                    for kc in range(8):
                        K.op("pe", lambda e_, pg=pg, kc=kc, c=c, g=g: e_.matmul(
                            pg[:, :], lhsT=wg[:, kc, c * 128:(c + 1) * 128], rhs=hT[:, kc, g * 512:(g + 1) * 512],
                            start=(kc == 0), stop=(kc == 7)), r=[b_wg] + b_hT[4 * g:4 * g + 4], w=[pgb])
                    pu, pub = K.bank()
                    for kc in range(8):
                        K.op("pe", lambda e_, pu=pu, kc=kc, c=c, g=g: e_.matmul(
                            pu[:, :], lhsT=wu[:, kc, c * 128:(c + 1) * 128], rhs=hT[:, kc, g * 512:(g + 1) * 512],
                            start=(kc == 0), stop=(kc == 7)), r=[b_wu] + b_hT[4 * g:4 * g + 4], w=[pub])
                    si = st["sil"] % 2
                    st["sil"] += 1
                    K.op("act", lambda e_, pg=pg, si=si: e_.activation(out=sil[si], in_=pg[:, :], func=AF.Silu),
                         r=[pgb], w=[b_sil[si]])
                    K.op("dve", lambda e_, pu=pu, si=si, c=c, g=g: e_.tensor_tensor(
                        out=heT[:, c, g * 512:(g + 1) * 512], in0=sil[si], in1=pu[:, :], op=ALU.mult),
                        r=[b_sil[si], pub], w=[b_heT[c][g]])
            if e + 1 < NEXP:
                nxt = [load_w(0, e + 1), load_w(1, e + 1)]
            for t in range(NT):
                for hf in range(2):
                    pd, pdb = K.bank()
                    for c in range(4):
                        K.op("pe", lambda e_, pd=pd, c=c, t=t, hf=hf: e_.matmul(
                            pd[:, :], lhsT=heT[:, c, t * 128:(t + 1) * 128], rhs=wd[:, c, hf * 512:(hf + 1) * 512],
                            start=(c == 0), stop=(c == 3)), r=[b_wd, b_heT[c][t // 4]], w=[pdb])
                    xs = self.x_sb[:, t, hf * 512:(hf + 1) * 512]
                    K.op("dve", lambda e_, pd=pd, xs=xs, t=t, e=e: e_.scalar_tensor_tensor(
                        out=xs, in0=pd[:, :], scalar=gates[:, t, e:e + 1], in1=xs, op0=ALU.mult, op1=ALU.add),
                        r=[pdb, b_gates, self.xb[t]], w=[self.xb[t]])
            if e + 1 < NEXP:
                nxt.append(load_w(2, e + 1))
        K.barrier()
        A.release()
        A.release()

    def routing(self, scores, b_sc, rb, b_rb, gates, b_gates):
        K, A = self.K, self.A
        BIGR = 1000.0
        n = NT * 16
        biased = f32(A, n)
        t1 = f32(A, n)
        t2 = f32(A, n)
        m1 = f32(A, NT * 4)
        m2 = f32(A, NT * 4)
        gs = f32(A, NT * 4)
        gm = f32(A, NT)
        ing = f32(A, NT * 4)
        b_r = Buf("routing")
        sc2 = scores.rearrange("p t e -> p (t e)")
        v3 = lambda a: a.rearrange("p (t e) -> p t e", e=16)
        g4 = lambda a: a.rearrange("p (g e) -> p g e", e=4)
        bc4 = lambda a: a.unsqueeze(2).to_broadcast([128, NT * 4, 4])
        R = [b_r, b_sc, b_rb]
        W = [b_r]
        dve = lambda fn, r=R, w=W: K.op("dve", fn, r=r, w=w)
        dve(lambda e: e.tensor_tensor(out=v3(biased), in0=scores, in1=rb.unsqueeze(1).to_broadcast([128, NT, 16]), op=ALU.add))
        dve(lambda e: e.tensor_reduce(out=m1, in_=g4(biased), axis=AX.X, op=ALU.max))
        dve(lambda e: e.tensor_tensor(out=g4(t1), in0=g4(biased), in1=bc4(m1), op=ALU.is_equal))
        dve(lambda e: e.scalar_tensor_tensor(out=t1, in0=t1, scalar=-BIGR, in1=biased, op0=ALU.mult, op1=ALU.add))
        dve(lambda e: e.tensor_reduce(out=m2, in_=g4(t1), axis=AX.X, op=ALU.max))
        dve(lambda e: e.tensor_tensor(out=gs, in0=m1, in1=m2, op=ALU.add))
        dve(lambda e: e.tensor_reduce(out=gm, in_=gs.rearrange("p (t g) -> p t g", g=4), axis=AX.X, op=ALU.max))
        dve(lambda e: e.tensor_tensor(out=ing.rearrange("p (t g) -> p t g", g=4), in0=gs.rearrange("p (t g) -> p t g", g=4),
                                      in1=gm.unsqueeze(2).to_broadcast([128, NT, 4]), op=ALU.is_equal))
        dve(lambda e: e.tensor_scalar(out=ing, in0=ing, scalar1=-1.0, scalar2=BIGR, op0=ALU.add, op1=ALU.mult))
        dve(lambda e: e.tensor_tensor(out=g4(t1), in0=g4(biased), in1=bc4(ing), op=ALU.add))
        dve(lambda e: e.tensor_reduce(out=gm, in_=v3(t1), axis=AX.X, op=ALU.max))
        dve(lambda e: e.tensor_tensor(out=v3(t2), in0=v3(t1), in1=gm.unsqueeze(2).to_broadcast([128, NT, 16]), op=ALU.is_equal))
        dve(lambda e: e.scalar_tensor_tensor(out=t2, in0=t2, scalar=-BIGR, in1=t1, op0=ALU.mult, op1=ALU.add))
        dve(lambda e: e.tensor_reduce(out=gm, in_=v3(t2), axis=AX.X, op=ALU.max))
        dve(lambda e: e.tensor_tensor(out=v3(t2), in0=v3(t1), in1=gm.unsqueeze(2).to_broadcast([128, NT, 16]), op=ALU.is_ge))
        dve(lambda e: e.tensor_tensor(out=t2, in0=t2, in1=sc2, op=ALU.mult))
        dve(lambda e: e.tensor_reduce(out=gm, in_=v3(t2), axis=AX.X, op=ALU.add))
        dve(lambda e: e.reciprocal(out=gm, in_=gm))
        K.op("dve", lambda e: e.tensor_tensor(out=gates, in0=v3(t2), in1=gm.unsqueeze(2).to_broadcast([128, NT, 16]), op=ALU.mult),
             r=R, w=[b_gates])
        K.barrier()

    def final(self):
        K, A = self.K, self.A
        if not hasattr(self, "epsb"):
            self.make_eps()
        A.mark()
        frow = f32(A, D)
        b_frow = Buf("frow")
        K.dma(frow[0:1, :], self.final_norm, w=[b_frow])
        self.bcast_row(self.bc[0], self.b_bc[0], frow[0:1, :], b_frow)
        scr = f32(A, D)
        b_scr = Buf("scrf")
        o = [f32(A, D) for _ in range(2)]
        b_o = [Buf("o0"), Buf("o1")]
        stats = [f32(A, 4) for _ in range(2)]
        b_stats = [Buf("fst0"), Buf("fst1")]
        oview = self.out.rearrange("(t p) d -> p t d", p=128)
        for t in range(NT):
            i = t % 2
            xt = self.x_sb[:, t, :]
            stat, b_stat = stats[i], b_stats[i]
            K.op("act", lambda e: e.activation(out=scr, in_=xt, func=AF.Square, accum_out=stat[:, 0:1]),
                 r=[self.xb[t]], w=[b_scr, b_stat])
            K.op("act", lambda e: e.activation(out=stat[:, 1:2], in_=stat[:, 0:1], func=AF.Sqrt, scale=1.0 / D, bias=self.epsb),
                 r=[b_stat, self.b_small], w=[b_stat])
            K.op("dve", lambda e: e.reciprocal(out=stat[:, 2:3], in_=stat[:, 1:2]), r=[b_stat], w=[b_stat])
            K.op("dve", lambda e: e.scalar_tensor_tensor(out=o[i], in0=xt, scalar=stat[:, 2:3], in1=self.bc[0],
                                                         op0=ALU.mult, op1=ALU.mult),
                 r=[self.xb[t], b_stat, self.b_bc[0]], w=[b_o[i]])
            K.dma(oview[:, t, :], o[i], r=[b_o[i]])
        A.release()


def _prep_common(inp):
    f = lambda a: np.ascontiguousarray(np.asarray(a, dtype=np.float32))
    com = {}
    aw = f(inp["ada_w"])
    com["ada_w"] = f(aw.reshape(2, 8, 128, 12, 512).transpose(0, 3, 2, 1, 4))
    com["ada_b"] = f(inp["ada_b"])
    com["norm_mix"] = f(inp["norm_mix"])
    com["norm_ffn"] = f(inp["norm_ffn"])
    com["final_norm"] = f(inp["final_norm"]).reshape(1, D)
    com["router_w"] = f(f(inp["router_w"]).reshape(8, 128, 16).transpose(1, 0, 2))
    com["router_b"] = f(inp["router_b"]).reshape(1, 16)
    com["moe_w_gate"] = f(inp["moe_w_gate"])
    com["moe_w_up"] = f(inp["moe_w_up"])
    com["moe_w_down"] = f(inp["moe_w_down"])
    com["ident"] = np.eye(128, dtype=np.float32)
    com["even_w"] = _even_w_blocks(f(inp["even_w_in"])[0])
    com["even_w_out"] = f(inp["even_w_out"])[0]
    com["conv_w"] = f(f(inp["even_conv_w"])[0].reshape(3, 4, 128).transpose(2, 1, 0))
    pos = f(inp["even_cmp_pos"])[0]
    com["cmp_pos"] = f(np.tile(pos.transpose(2, 0, 1), (2, 1, 1)))
    w1 = f(inp["even_cmp_w1"])[0]
    w1 = w1.reshape(2, 32, 64, 256).transpose(0, 2, 1, 3)
    w1 = np.tile(w1, (1, 2, 1, 1)).reshape(2, 128, 4, 8, 256).transpose(0, 2, 1, 3, 4)
    com["cmp_w1"] = f(w1)
    w2 = f(inp["even_cmp_w2"])[0]
    com["cmp_w2"] = f(w2.reshape(2, 2, 128, 64).transpose(2, 0, 1, 3))
    com.update(_even_consts())
    com["odd_w"] = _odd_w_blocks(f(inp["odd_w_in"])[0])
    com["odd_w_out"] = f(inp["odd_w_out"])[0]
    com["pool_w"] = f(inp["odd_pool_w"])[0]
    pk = lambda a: f(f(a).reshape(-1)[:512].reshape(4, 128).T)
    com["pool_scale"] = pk(f(inp["odd_pool_scale"])[0])
    com["chv"] = f(np.stack([pk(f(inp[k])[0]) for k in ("odd_w0", "odd_a0", "odd_k_k", "odd_k_a", "odd_lnx_w", "odd_lnx_b", "odd_r_k")], axis=1))
    com["mu"] = f(f(inp["odd_mu"])[0].reshape(14, 128).T)
    com["wa2"] = f(np.concatenate([f(inp["odd_w2"])[0], f(inp["odd_a2"])[0]], axis=0))
    com["g2"] = f(inp["odd_g2"])[0]
    com.update(_odd_consts())
    return com


_CACHE = {}


def run(inputs, stages=("mix0", "moe0", "mix1", "moe1", "final"), cores=None, x_override=None):
    cores = list(range(8)) if cores is None else cores
    key = tuple(stages)
    if key not in _CACHE:
        _CACHE[key] = build_program(stages)
    nc = _CACHE[key]
    com = _prep_common(inputs)
    x = np.asarray(inputs["x"], dtype=np.float32) if x_override is None else x_override
    c = np.asarray(inputs["c"], dtype=np.float32)
    in_maps = []
    for b in cores:
        m = dict(com)
        m["x"] = np.ascontiguousarray(x[b])
        m["c"] = np.ascontiguousarray(c[b].reshape(8, 128).T)
        in_maps.append(m)
    res = run_bass_kernel_spmd(nc, in_maps, core_ids=list(range(len(cores))))
    return np.stack([r["out"] for r in res.results], axis=0)


def kernel(**inputs):
    return run(inputs).astype(np.float32)


ATTN_SCALE = 0.125
N_EVEN_BLK = 29


def _even_w_blocks(w_in):
    cols = []
    hd = lambda base, h: list(range(base + h * 64, base + (h + 1) * 64))
    sw = lambda c: c[32:] + c[:32]
    for g in range(4):
        cols.append(hd(0, g) + hd(0, g + 4))
    for g in range(4):
        cols.append(sw(hd(0, g)) + sw(hd(0, g + 4)))
    cols.append(list(range(512, 640)))
    cols.append(list(range(640, 768)))
    cols.append(list(range(768, 896)))
    cols.append(sw(hd(768, 0)) + sw(hd(768, 1)))
    cols.append(list(range(1024, 1152)))
    cols.append(sw(hd(1024, 0)) + sw(hd(1024, 1)))
    cols.append(list(range(896, 1024)))
    cols.append(list(range(1152, 1280)))
    cols.append(list(range(1280, 1304)) + [-1] * 104)
    for base in (1304, 1816, 2328):
        for c in range(4):
            cols.append(list(range(base + c * 128, base + (c + 1) * 128)))
    out = np.zeros((len(cols), 128, 8, 128), np.float32)
    for i, cl in enumerate(cols):
        idx = np.array(cl)
        blk = np.where(idx[None, :] >= 0, w_in[:, np.maximum(idx, 0)], 0.0)
        out[i] = blk.reshape(8, 128, 128).transpose(1, 0, 2)
    return out


def _even_consts():
    import ml_dtypes
    bf16 = ml_dtypes.bfloat16
    c = {}
    half = 32
    inv = 10000.0 ** (-np.arange(half, dtype=np.float32) / half)
    ang = np.arange(S, dtype=np.float32)[:, None] * inv[None, :]
    cos = np.cos(ang).T.astype(np.float32)
    sin = np.sin(ang).T.astype(np.float32)
    c["rope_cos"] = np.ascontiguousarray(np.tile(cos, (4, 1)))
    c["rope_sin"] = np.ascontiguousarray(np.tile(np.concatenate([-sin, sin], 0), (2, 1)))
    t = np.arange(S)
    cm = (np.arange(127) * 16 + 31)[None, :] <= t[:, None]
    c["cmpbias"] = np.ascontiguousarray(np.where(cm, 0.0, NEGB).astype(np.float32).reshape(NT, 128, 127).transpose(1, 0, 2)).astype(bf16)
    j = np.arange(32)[None, :]
    cur = t[:, None] // 64
    valid = j * 64 <= t[:, None]
    forced = (j == 0) | ((cur - j >= 0) & (cur - j < 2))
    vm = (valid & ~forced).astype(np.float32)
    am = np.where(forced, 1e9, np.where(valid, 0.0, -1e9)).astype(np.float32)
    ok = valid.astype(np.float32)
    lay = lambda a: np.ascontiguousarray(a.reshape(NT, 128, 32).transpose(1, 0, 2))
    c["selmask"] = np.ascontiguousarray(np.stack([lay(vm), lay(am), lay(ok)], axis=1)).astype(bf16)
    ex = np.zeros((32, 16, 128), np.float32)
    for kt in range(16):
        for p in range(128):
            ex[2 * kt + p // 64, kt, p] = 1.0
    c["expand"] = ex.astype(bf16)
    wb = np.zeros((128, 8, 512), np.float32)
    for jj in range(8):
        s_pos = (jj - 4) * 128 + np.arange(128)[:, None]
        tq = np.arange(512)[None, :]
        diff = tq - s_pos
        wb[:, jj, :] = np.where((diff >= 0) & (diff < 512), 0.0, NEGB)
    c["wbias"] = wb.astype(bf16)
    c["identb"] = np.eye(128, dtype=np.float32).astype(bf16)
    return c


class _EvenMixin:
    def norm_transpose(self, hT, b_hT):
        K, A = self.K, self.A
        if not hasattr(self, "epsb"):
            self.make_eps()
        A.mark()
        htmp = [f32(A, D) for _ in range(2)]
        b_htmp = [Buf("mhtmp0"), Buf("mhtmp1")]
        scr = f32(A, D)
        b_scr = Buf("mscr")
        stats = [f32(A, 4) for _ in range(2)]
        b_stats = [Buf("mst0"), Buf("mst1")]
        for t in range(NT):
            i = t % 2
            self.norm_tile(t, htmp[i], b_htmp[i], scr, b_scr, stats[i], b_stats[i])
            for hf in range(2):
                pt, pb = K.bank()
                for kk in range(4):
                    kc = hf * 4 + kk
                    K.op("pe", lambda e, pt=pt, kk=kk, kc=kc, i=i: e.transpose(
                        out=pt[:, kk * 128:(kk + 1) * 128], in_=htmp[i][:, kc * 128:(kc + 1) * 128], identity=self.ident),
                        r=[b_htmp[i], self.b_ident], w=[pb])
                K.op("act", lambda e, pt=pt, hf=hf, t=t: e.copy(
                    out=hT[:, hf * 4:(hf + 1) * 4, t * 128:(t + 1) * 128], in_=pt[:, :].rearrange("p (k s) -> p k s", k=4)),
                    r=[pb], w=[b_hT[t]])
        K.barrier()
        A.release()

    def make_wloader(self, nslots=3, nstage=2):
        K, A = self.K, self.A
        stg = [f32(A, 1024) for _ in range(nstage)]
        b_stg = [Buf("wstg%d" % i) for i in range(nstage)]
        wb = [bf(A, 1024) for _ in range(nslots)]
        b_wb = [Buf("wblk%d" % i) for i in range(nslots)]
        st = {"s": 0, "w": 0}

        def load(src, mul=None, b_mul=None):
            si = st["s"] % nstage
            st["s"] += 1
            wi = st["w"] % nslots
            st["w"] += 1
            K.dma(stg[si], src, w=[b_stg[si]])
            if mul is None:
                K.op("pool", lambda e: e.tensor_copy(out=wb[wi], in_=stg[si]), r=[b_stg[si]], w=[b_wb[wi]])
            else:
                K.op("pool", lambda e: e.tensor_tensor(out=wb[wi], in0=stg[si], in1=mul, op=ALU.mult),
                     r=[b_stg[si], b_mul], w=[b_wb[wi]])
            return wb[wi], b_wb[wi]
        return load

    def proj_fm(self, wv, b_w, hT, b_hT, G):
        K = self.K
        pt, pb = K.bank()
        w3 = wv.rearrange("p (k n) -> p k n", k=8)
        for kc in range(8):
            K.op("pe", lambda e, kc=kc: e.matmul(pt[:, :], lhsT=w3[:, kc, :], rhs=hT[:, kc, G * 512:(G + 1) * 512],
                                                 start=(kc == 0), stop=(kc == 7)),
                 r=[b_w] + b_hT[4 * G:4 * G + 4], w=[pb])
        return pt, pb

    def proj_tm(self, wv, b_w, hT, b_hT, t):
        K = self.K
        pt, pb = K.bank()
        w3 = wv.rearrange("p (k n) -> p k n", k=8)
        for kc in range(8):
            K.op("pe", lambda e, kc=kc: e.matmul(pt[:, 0:128], lhsT=hT[:, kc, t * 128:(t + 1) * 128], rhs=w3[:, kc, :],
                                                 start=(kc == 0), stop=(kc == 7)),
                 r=[b_w, b_hT[t]], w=[pb])
        return pt, pb

    def even_mixer(self):
        nc, K, A = self.nc, self.K, self.A
        l = 0
        self.load_mod_bc(l, 0, self.norm_mix[l:l + 1, :])
        g1bc, b_g1 = self.bc[2], self.b_bc[2]
        HTB = 8 * S * 2
        hT = A.alloc_top(HTB, BF16).rearrange("p (k s) -> p k s", k=8)
        b_hT = [Buf("mhT%d" % t) for t in range(NT)]
        self.norm_transpose(hT, b_hT)
        wsrc = lambda i: self.even_w[i].rearrange("p k n -> p (k n)")

        A.mark()
        load = self.make_wloader(3)
        u = f32(A, S + 2)
        b_u = Buf("u")
        bgs = f32(A, S)
        b_bgs = Buf("bgs")
        acc = f32(A, S)
        b_acc = Buf("acc")
        yc4 = bf(A, 4 * S).rearrange("p (c s) -> p c s", c=4)
        b_yc = Buf("yc")
        cwo4 = bf(A, 4 * 1024).rearrange("p (c n) -> p c n", c=4)
        b_cwo4 = Buf("cwo4")
        cstage = f32(A, 1024)
        b_cstage = Buf("cstage")
        tmp = [f32(A, 512) for _ in range(2)]
        b_tmp = [Buf("ctmp0"), Buf("ctmp1")]
        cw = f32(A, 12).rearrange("p (c k) -> p c k", c=4)
        b_cw = Buf("cw")
        K.dma(cw, self.conv_w, w=[b_cw])
        K.op("dve", lambda e: e.memset(u[:, 0:2], 0.0), w=[b_u])
        ti = 0
        for c in range(4):
            wx, b_wx = load(wsrc(17 + c))
            wc, b_wc = load(wsrc(25 + c))
            for G in range(4):
                px, pxb = self.proj_fm(wx, b_wx, hT, b_hT, G)
                pc, pcb = self.proj_fm(wc, b_wc, hT, b_hT, G)
                tt, b_tt = tmp[ti % 2], b_tmp[ti % 2]
                ti += 1
                K.op("act", lambda e: e.copy(out=tt, in_=px[:, :]), r=[pxb], w=[b_tt])
                K.op("dve", lambda e: e.tensor_tensor(out=u[:, 2 + G * 512:2 + (G + 1) * 512], in0=tt, in1=pc[:, :], op=ALU.mult),
                     r=[b_tt, pcb], w=[b_u])
            wb_, b_wb_ = load(wsrc(21 + c))
            for G in range(4):
                pb_, pbb = self.proj_fm(wb_, b_wb_, hT, b_hT, G)
                K.op("act", lambda e: e.copy(out=bgs[:, G * 512:(G + 1) * 512], in_=pb_[:, :]), r=[pbb], w=[b_bgs])
            K.op("dve", lambda e: e.tensor_scalar(out=acc, in0=u[:, 2:2 + S], scalar1=cw[:, c, 2:3], scalar2=None, op0=ALU.mult),
                 r=[b_u, b_cw], w=[b_acc])
            K.op("dve", lambda e: e.scalar_tensor_tensor(out=acc, in0=u[:, 1:1 + S], scalar=cw[:, c, 1:2], in1=acc,
                                                         op0=ALU.mult, op1=ALU.add), r=[b_u, b_cw, b_acc], w=[b_acc])
            K.op("dve", lambda e: e.scalar_tensor_tensor(out=acc, in0=u[:, 0:S], scalar=cw[:, c, 0:1], in1=acc,
                                                         op0=ALU.mult, op1=ALU.add), r=[b_u, b_cw, b_acc], w=[b_acc])
            K.op("dve", lambda e: e.tensor_tensor(out=yc4[:, c, :], in0=acc, in1=bgs, op=ALU.mult), r=[b_acc, b_bgs], w=[b_yc])
        self.add_wout_multi(yc4, b_yc, self.even_w_out, 512, 4, cstage, b_cstage, cwo4, b_cwo4)
        K.barrier()
        A.release()
        if os.environ.get("KDBG") == "conv":
            A.release_top(HTB)
            return

        A.mark()
        qn = bf(A, 4 * S).rearrange("p (g s) -> p g s", g=4)
        qr = bf(A, 4 * S).rearrange("p (g s) -> p g s", g=4)
        b_qn = [[Buf("qn%d_%d" % (g, G)) for G in range(4)] for g in range(4)]
        b_qr = [[Buf("qr%d_%d" % (g, G)) for G in range(4)] for g in range(4)]
        kcT, vcT, ksT, kwT = [bf(A, S) for _ in range(4)]
        b_kcT, b_vcT, b_ksT, b_kwT = Buf("kcT"), Buf("vcT"), Buf("ksT"), Buf("kwT")
        Vs = bf(A, NT * 2 * 65).rearrange("p (t h d) -> p t h d", t=NT, h=2)
        Vw = bf(A, NT * 2 * 65).rearrange("p (t h d) -> p t h d", t=NT, h=2)
        b_Vs, b_Vw = Buf("Vs"), Buf("Vw")
        sg = f32(A, NT * 24).rearrange("p (t c) -> p t c", t=NT)
        b_sg = Buf("sg")
        A.mark()
        load = self.make_wloader(3, 1)
        cosT = f32(A, S)
        sinT = f32(A, S)
        b_rope = Buf("rope")
        K.dma(cosT, self.rope_cos, w=[b_rope])
        K.dma(sinT, self.rope_sin, w=[b_rope])
        tmp = [f32(A, 512) for _ in range(2)]
        b_tmp = [Buf("ptmp%d" % i) for i in range(2)]
        ti = 0
        K.op("dve", lambda e: e.memset(Vs[:, :, :, 64:65], 1.0), w=[b_Vs])
        K.op("dve", lambda e: e.memset(Vw[:, :, :, 64:65], 1.0), w=[b_Vw])

        def rope_pair(ia, ib, dst_fn, bdst_fn, nope_fn=None):
            nonlocal ti
            wa, b_wa = load(wsrc(ia))
            wb2, b_wb2 = load(wsrc(ib))
            for G in range(4):
                pa, pab = self.proj_fm(wa, b_wa, hT, b_hT, G)
                ps_, psb = self.proj_fm(wb2, b_wb2, hT, b_hT, G)
                t1, b_t1 = tmp[0], b_tmp[0]
                t2, b_t2 = tmp[1], b_tmp[1]
                sl = slice(G * 512, (G + 1) * 512)
                K.op("dve", lambda e: e.tensor_tensor(out=t1, in0=pa[:, :], in1=cosT[:, sl], op=ALU.mult), r=[pab, b_rope], w=[b_t1])
                if nope_fn is not None:
                    dn, bdn = nope_fn(G)
                    K.op("act", lambda e: e.copy(out=dn, in_=pa[:, :]), r=[pab], w=[bdn])
                K.op("dve", lambda e: e.tensor_tensor(out=t2, in0=ps_[:, :], in1=sinT[:, sl], op=ALU.mult), r=[psb, b_rope], w=[b_t2])
                K.op("pool", lambda e: e.tensor_tensor(out=dst_fn(G), in0=t1, in1=t2, op=ALU.add), r=[b_t1, b_t2], w=[bdst_fn(G)])

        for g in range(4):
            rope_pair(g, 4 + g, lambda G, g=g: qr[:, g, G * 512:(G + 1) * 512], lambda G, g=g: b_qr[g][G],
                      nope_fn=lambda G, g=g: (qn[:, g, G * 512:(G + 1) * 512], b_qn[g][G]))
        rope_pair(10, 11, lambda G: ksT[:, G * 512:(G + 1) * 512], lambda G: b_ksT)
        rope_pair(12, 13, lambda G: kwT[:, G * 512:(G + 1) * 512], lambda G: b_kwT)
        for ib, dst, bd in ((8, kcT, b_kcT), (9, vcT, b_vcT)):
            w_, b_w_ = load(wsrc(ib))
            for G in range(4):
                p_, pb_ = self.proj_fm(w_, b_w_, hT, b_hT, G)
                K.op("act", lambda e: e.copy(out=dst[:, G * 512:(G + 1) * 512], in_=p_[:, :]), r=[pb_], w=[bd])
        for ib, dst, bd in ((14, Vs, b_Vs), (15, Vw, b_Vw)):
            w_, b_w_ = load(wsrc(ib))
            for t in range(NT):
                p_, pb_ = self.proj_tm(w_, b_w_, hT, b_hT, t)
                K.op("act", lambda e: e.copy(out=dst[:, t, :, 0:64], in_=p_[:, 0:128].rearrange("p (h d) -> p h d", h=2)),
                     r=[pb_], w=[bd])
        w_, b_w_ = load(wsrc(16))
        for t in range(NT):
            p_, pb_ = self.proj_tm(w_, b_w_, hT, b_hT, t)
            K.op("act", lambda e: e.activation(out=sg[:, t, :], in_=p_[:, 0:24], func=AF.Sigmoid), r=[pb_], w=[b_sg])
        K.barrier()
        A.release()
        A.release_top(HTB)

        kcmpT = bf(A, 128)
        b_kcmpT = Buf("kcmpT")
        vcmp = bf(A, 2 * 64).rearrange("p (h d) -> p h d", h=2)
        b_vcmp = Buf("vcmp")
        A.mark()
        Bl = bf(A, 32 * 127).rearrange("p (l c) -> p l c", l=32)
        b_Bl = Buf("Bl")
        posT = f32(A, 2 * 32).rearrange("p (j l) -> p j l", j=2)
        b_posT = Buf("posT")
        K.dma(posT, self.cmp_pos, w=[b_posT])
        w1s = [f32(A, 8 * 256) for _ in range(2)]
        b_w1s = [Buf("w1s0"), Buf("w1s1")]
        w1b = [bf(A, 8 * 256).rearrange("p (l n) -> p l n", l=8) for _ in range(2)]
        b_w1b = [Buf("w1b0"), Buf("w1b1")]
        w2s = f32(A, 2 * 2 * 64).rearrange("p (j c d) -> p j c d", j=2, c=2)
        b_w2s = Buf("w2s")
        K.dma(w2s, self.cmp_w2, w=[b_w2s])
        w2p = bf(A, 2 * 2 * 128).rearrange("p (h c m) -> p h c m", h=2, c=2)
        b_w2p = Buf("w2p")
        w2v = bf(A, 2 * 64).rearrange("p (c d) -> p c d", c=2)
        b_w2v = Buf("w2v")
        K.op("dve", lambda e: e.memset(w2p, 0.0), w=[b_w2p])
        for h in range(2):
            K.op("dve", lambda e: e.tensor_copy(out=w2p[:, h, :, h * 64:(h + 1) * 64], in_=w2s[:, 0, :, :]), r=[b_w2s], w=[b_w2p])
        K.op("dve", lambda e: e.tensor_copy(out=w2v, in_=w2s[:, 1, :, :]), r=[b_w2s], w=[b_w2v])
        hx = f32(A, 127)
        hy = f32(A, 127)
        b_hx = Buf("hx")
        hid = bf(A, 2 * 2 * 127).rearrange("p (h c n) -> p h c n", h=2, c=2)
        b_hid = Buf("hid")
        K.bank_rng = (0, 4)
        wi = 0
        for j, (tokT, b_tok) in enumerate(((kcT, b_kcT), (vcT, b_vcT))):
            for l_ in range(32):
                K.op("dve", lambda e: e.tensor_scalar(out=Bl[:, l_, :], in0=tokT[:, l_:l_ + 16 * 126 + 1:16],
                                                      scalar1=posT[:, j, l_:l_ + 1], scalar2=None, op0=ALU.add),
                     r=[b_tok, b_posT], w=[b_Bl])
            accs = [K.bank() for _ in range(4)]
            for lg in range(4):
                si = wi % 2
                wi += 1
                K.dma(w1s[si], self.cmp_w1[j, lg].rearrange("p l n -> p (l n)"), w=[b_w1s[si]])
                K.op("pool", lambda e: e.tensor_copy(out=w1b[si].rearrange("p l n -> p (l n)"), in_=w1s[si]), r=[b_w1s[si]], w=[b_w1b[si]])
                for li in range(8):
                    l_ = lg * 8 + li
                    for h in range(2):
                        for c2 in range(2):
                            pt, pb = accs[h * 2 + c2]
                            K.op("pe", lambda e: e.matmul(pt[:, 0:127], lhsT=w1b[si][64 * h:64 * h + 64, li, c2 * 128:(c2 + 1) * 128],
                                                          rhs=Bl[64 * h:64 * h + 64, l_, :], start=(l_ == 0), stop=(l_ == 31)),
                                 r=[b_w1b[si], b_Bl], w=[pb])
            for h in range(2):
                for c2 in range(2):
                    pt, pb = accs[h * 2 + c2]
                    K.op("act", lambda e: e.copy(out=hx, in_=pt[:, 0:127]), r=[pb], w=[b_hx])
                    K.op("dve", lambda e: e.tensor_tensor(out=hy, in0=hx, in1=hx, op=ALU.mult), r=[b_hx], w=[b_hx])
                    K.op("dve", lambda e: e.tensor_scalar(out=hy, in0=hy, scalar1=0.044715, scalar2=1.0, op0=ALU.mult, op1=ALU.add),
                         r=[b_hx], w=[b_hx])
                    K.op("dve", lambda e: e.tensor_tensor(out=hy, in0=hy, in1=hx, op=ALU.mult), r=[b_hx], w=[b_hx])
                    K.op("act", lambda e: e.activation(out=hy, in_=hy, func=AF.Sigmoid, scale=1.5957691216), r=[b_hx], w=[b_hx])
                    K.op("dve", lambda e: e.tensor_tensor(out=hid[:, h, c2, :], in0=hy, in1=hx, op=ALU.mult), r=[b_hx], w=[b_hid])
            K.bank_rng = (4, 8)
            if j == 0:
                pt, pb = K.bank()
                n_ = 0
                for h in range(2):
                    for c2 in range(2):
                        K.op("pe", lambda e: e.matmul(pt[:, 0:127], lhsT=w2p[:, h, c2, :], rhs=hid[:, h, c2, :],
                                                      start=(n_ == 0), stop=(n_ == 3)), r=[b_w2p, b_hid], w=[pb])
                        n_ += 1
                K.op("act", lambda e: e.copy(out=kcmpT[:, 0:127], in_=pt[:, 0:127]), r=[pb], w=[b_kcmpT])
            else:
                for h in range(2):
                    pt, pb = K.bank()
                    for c2 in range(2):
                        K.op("pe", lambda e: e.matmul(pt[0:127, 0:64], lhsT=hid[:, h, c2, :], rhs=w2v[:, c2, :],
                                                      start=(c2 == 0), stop=(c2 == 1)), r=[b_hid, b_w2v], w=[pb])
                    K.op("act", lambda e: e.copy(out=vcmp[0:127, h, :], in_=pt[0:127, 0:64]), r=[pb], w=[b_vcmp])
            K.bank_rng = (0, 4)
        K.bank_rng = (0, 8)
        K.barrier()
        A.release()

        A.mark()
        expand = bf(A, 16 * 128).rearrange("p (k m) -> p k m", k=16)
        wbias = bf(A, 8 * 512).rearrange("p (j n) -> p j n", j=8)
        identb = bf(A, 128)
        cmpbias = bf(A, NT * 127).rearrange("p (t c) -> p t c", t=NT)
        selm = bf(A, 3 * NT * 32).rearrange("p (m t j) -> p m t j", m=3, t=NT)
        b_tab = Buf("tables")
        K.dma(expand[0:32], self.expand_in, w=[b_tab])
        K.dma(wbias, self.wbias_in, w=[b_tab])
        K.dma(identb, self.identb_in, w=[b_tab])
        K.dma(cmpbias, self.cmpbias_in, w=[b_tab])
        K.dma(selm, self.selmask_in, w=[b_tab])
        wo = bf(A, 4 * 1024).rearrange("p (c n) -> p c n", c=4)
        b_wo = Buf("wo")
        PTraw = [bf(A, 8 * 512) for _ in range(2)]
        wo_s = PTraw[0][:, 0:2048].bitcast(F32)
        b_wo_s = Buf("wo_s")
        for c in range(4):
            K.dma(wo_s, self.even_w_out[c * 128:(c + 1) * 128, :], w=[b_wo_s])
            K.op("pool", lambda e: e.tensor_tensor(out=wo[:, c, :], in0=wo_s, in1=g1bc, op=ALU.mult), r=[b_wo_s, b_g1], w=[b_wo])
        K.barrier()
        PT = [p_.rearrange("p (k n) -> p k n", k=8) for p_ in PTraw]
        b_PT = [Buf("PT0"), Buf("PT1")]
        oT = bf(A, 4 * 512).rearrange("p (c n) -> p c n", c=4)
        b_oT = Buf("oT")
        oacc = f32(A, 4 * 512).rearrange("p (q n) -> p q n", q=4)
        b_oacc = Buf("oacc")
        selT = bf(A, 2 * 512).rearrange("p (h n) -> p h n", h=2)
        b_selT = Buf("selT")
        imp = f32(A, 132)
        b_imp = Buf("imp")
        sm = f32(A, 128)
        b_sm = Buf("sm")
        selb = f32(A, 32)
        b_selb = Buf("selb")
        K.op("dve", lambda e: e.memset(imp, 0.0), w=[b_imp])
        sS4s = [f32(A, 4 * 127).rearrange("p (g c) -> p g c", g=4) for _ in range(2)]
        sP4s = [f32(A, 4 * 127).rearrange("p (g c) -> p g c", g=4) for _ in range(2)]
        pT4s = [bf(A, 4 * 128).rearrange("p (g q) -> p g q", g=4) for _ in range(2)]
        smxs = [f32(A, 8) for _ in range(2)]
        imps = [f32(A, 132) for _ in range(2)]
        b_sS4s, b_sP4s, b_pT4s, b_smxs, b_imps = ([Buf("%s%d" % (n_, i)) for i in range(2)] for n_ in ("sS4", "sP4", "pT4", "smx", "impx"))
        for i in range(2):
            K.op("dve", lambda e: e.memset(imps[i], 0.0), w=[b_imps[i]])
        cit = [0]
        pti = 0
        for G in range(4):
            for qi in range(4):
                t = 4 * G + qi
                for h in range(2):
                    K.bank_rng = (4, 8)
                    bi = cit[0] % 2
                    cit[0] += 1
                    sS4, sP4, pT4, smx, impx = sS4s[bi], sP4s[bi], pT4s[bi], smxs[bi], imps[bi]
                    b_sS4, b_sP4, b_pT4, b_smx, b_impx = b_sS4s[bi], b_sP4s[bi], b_pT4s[bi], b_smxs[bi], b_imps[bi]
                    imp, b_imp = impx, b_impx
                    pt, pb = K.bank()
                    for g in range(4):
                        K.op("pe", lambda e: e.matmul(pt[:, g * 128:g * 128 + 127], lhsT=qn[64 * h:64 * h + 64, g, t * 128:(t + 1) * 128],
                                                      rhs=kcmpT[64 * h:64 * h + 64, 0:127], start=True, stop=True),
                             r=[b_qn[g][G], b_kcmpT], w=[pb])
                    K.op("dve", lambda e: e.scalar_tensor_tensor(out=sS4, in0=pt[:, :].rearrange("p (g c) -> p g c", g=4)[:, :, 0:127], scalar=ATTN_SCALE,
                                                                 in1=cmpbias[:, t, :].unsqueeze(1).to_broadcast([128, 4, 127]),
                                                                 op0=ALU.mult, op1=ALU.add), r=[pb, b_tab], w=[b_sS4])
                    K.op("act", lambda e: e.activation(out=sS4, in_=sS4, func=AF.Exp), r=[b_sS4], w=[b_sS4])
                    K.op("dve", lambda e: e.tensor_reduce(out=smx[:, 0:4], in_=sS4, axis=AX.X, op=ALU.add), r=[b_sS4], w=[b_smx])
                    K.op("dve", lambda e: e.tensor_scalar(out=smx[:, 0:4], in0=smx[:, 0:4], scalar1=1e-30, scalar2=None, op0=ALU.max), r=[b_smx], w=[b_smx])
                    K.op("dve", lambda e: e.reciprocal(out=smx[:, 0:4], in_=smx[:, 0:4]), r=[b_smx], w=[b_smx])
                    K.op("dve", lambda e: e.tensor_tensor(out=sP4, in0=sS4, in1=smx[:, 0:4].unsqueeze(2).to_broadcast([128, 4, 127]), op=ALU.mult),
                         r=[b_sS4, b_smx], w=[b_sP4])
                    K.op("dve", lambda e: e.tensor_reduce(out=imp[:, 0:127], in_=sP4.rearrange("p g c -> p c g"), axis=AX.X, op=ALU.add),
                         r=[b_sP4], w=[b_imp])
                    pt2, pb2 = K.bank()
                    for g in range(4):
                        K.op("pe", lambda e: e.transpose(out=pt2[0:127, g * 128:(g + 1) * 128], in_=sP4[:, g, :], identity=self.ident),
                             r=[b_sP4, self.b_ident], w=[pb2])
                    K.op("act", lambda e: e.copy(out=pT4[0:127], in_=pt2[0:127, :].rearrange("p (g q) -> p g q", g=4)), r=[pb2], w=[b_pT4])
                    pt3, pb3 = K.bank()
                    for g in range(4):
                        K.op("pe", lambda e: e.matmul(pt3[:, g * 64:(g + 1) * 64], lhsT=pT4[0:127, g, :], rhs=vcmp[0:127, h, :], start=True, stop=True),
                             r=[b_pT4, b_vcmp], w=[pb3])
                    K.op("dve", lambda e: e.tensor_tensor(out=oacc[:, qi, h * 256:(h + 1) * 256].rearrange("p (g d) -> p g d", g=4),
                                                          in0=pt3[:, 0:256].rearrange("p (g d) -> p g d", g=4),
                                                          in1=sg[:, t, h * 12:h * 12 + 12:3].unsqueeze(2).to_broadcast([128, 4, 64]), op=ALU.mult),
                         r=[pb3, b_sg], w=[b_oacc])
                    v4 = lambda a: a.rearrange("p (j m) -> p j m", m=4)
                    K.op("dve", lambda e: e.tensor_reduce(out=sm[:, 0:32], in_=v4(imp[:, 0:128]), axis=AX.X, op=ALU.add), r=[b_imp], w=[b_sm])
                    K.op("dve", lambda e: e.tensor_reduce(out=sm[:, 32:64], in_=v4(imp[:, 1:129]), axis=AX.X, op=ALU.add), r=[b_imp], w=[b_sm])
                    K.op("dve", lambda e: e.tensor_tensor(out=sm[:, 0:32], in0=sm[:, 0:32], in1=sm[:, 32:64], op=ALU.add), r=[b_sm], w=[b_sm])
                    K.op("dve", lambda e: e.tensor_tensor(out=sm[:, 0:32], in0=sm[:, 0:32], in1=selm[:, 0, t, :], op=ALU.mult), r=[b_sm, b_tab], w=[b_sm])
                    K.op("dve", lambda e: e.tensor_tensor(out=sm[:, 0:32], in0=sm[:, 0:32], in1=selm[:, 1, t, :], op=ALU.add), r=[b_sm, b_tab], w=[b_sm])
                    K.op("dve", lambda e: e.max(out=sm[:, 64:72], in_=sm[:, 0:32]), r=[b_sm], w=[b_sm])
                    K.op("dve", lambda e: e.tensor_scalar(out=sm[:, 32:64], in0=sm[:, 0:32], scalar1=sm[:, 71:72], scalar2=None, op0=ALU.is_ge),
                         r=[b_sm], w=[b_sm])
                    K.op("dve", lambda e: e.tensor_tensor(out=sm[:, 32:64], in0=sm[:, 32:64], in1=selm[:, 2, t, :], op=ALU.mult), r=[b_sm, b_tab], w=[b_sm])
                    K.op("dve", lambda e: e.tensor_scalar(out=selb, in0=sm[:, 32:64], scalar1=-NEGB, scalar2=NEGB, op0=ALU.mult, op1=ALU.add),
                         r=[b_sm], w=[b_selb])
                    pt4, pb4 = K.bank()
                    K.op("pe", lambda e: e.transpose(out=pt4[0:32, 0:128], in_=selb, identity=self.ident), r=[b_selb, self.b_ident], w=[pb4])
                    K.op("act", lambda e: e.copy(out=selT[0:32, h, qi * 128:(qi + 1) * 128], in_=pt4[0:32, 0:128]), r=[pb4], w=[b_selT])
            pos = [K.banks[i] for i in range(4)]
            items = []
            for br in range(2):
                for h in range(2):
                    for g in range(4):
                        kt_lo = 0 if br == 0 else max(0, 4 * G - 4)
                        kts = list(range(kt_lo, 4 * G + 4))
                        chunks = [kts[c0:c0 + 8] for c0 in range(0, len(kts), 8)]
                        for ci_, chunk in enumerate(chunks):
                            items.append((br, h, g, kts, chunk, ci_ == len(chunks) - 1))

            def emit_qk(k):
                br, h, g, kts, chunk, last = items[k]
                kT, b_kT = (ksT, b_ksT) if br == 0 else (kwT, b_kwT)
                P_, b_P = PT[k % 2], b_PT[k % 2]
                K.bank_rng = (4, 8)
                for ci, kt in enumerate(chunk):
                    pt, pb = K.bank()
                    jj = kt - 4 * G + 4
                    need_bias = (jj >= 4) or (br == 1)
                    mms = [(kT[64 * h:64 * h + 64, kt * 128:(kt + 1) * 128], qr[64 * h:64 * h + 64, g, G * 512:(G + 1) * 512], [b_kT, b_qr[g][G]])]
                    if br == 0:
                        mms.append((expand[0:32, kt, :], selT[0:32, h, :], [b_tab, b_selT]))
                    if need_bias:
                        mms.append((identb, wbias[:, jj, :], [b_tab]))
                    for n_, (lh, rh, rr) in enumerate(mms):
                        K.op("pe", lambda e: e.matmul(pt[:, :], lhsT=lh, rhs=rh, start=(n_ == 0), stop=(n_ == len(mms) - 1)), r=rr, w=[pb])
                    K.op("act", lambda e: e.activation(out=P_[:, ci, :], in_=pt[:, :], func=AF.Exp, scale=ATTN_SCALE), r=[pb], w=[b_P])

            def emit_pv(k):
                br, h, g, kts, chunk, last = items[k]
                V, b_V = (Vs, b_Vs) if br == 0 else (Vw, b_Vw)
                P_, b_P = PT[k % 2], b_PT[k % 2]
                hc = h * 4 + g
                for qi in range(4):
                    t = 4 * G + qi
                    lo_kt = 0 if br == 0 else max(0, t - 4)
                    use = [kt for kt in chunk if lo_kt <= kt <= t]
                    allk = [kt for kt in kts if lo_kt <= kt <= t]
                    po, pob = pos[qi]
                    for kt in use:
                        ci = chunk.index(kt)
                        K.op("pe", lambda e: e.matmul(po[:, 0:65], lhsT=P_[:, ci, qi * 128:(qi + 1) * 128], rhs=V[:, kt, h, :],
                                                      start=(kt == allk[0]), stop=(kt == allk[-1])), r=[b_P, b_V], w=[pob])
                if last:
                    for qi in range(4):
                        t = 4 * G + qi
                        po, pob = pos[qi]
                        K.op("dve", lambda e: e.reciprocal(out=sm[:, 80 + qi:81 + qi], in_=po[:, 64:65]), r=[pob], w=[b_sm])
                        K.op("dve", lambda e: e.tensor_tensor(out=sm[:, 80 + qi:81 + qi], in0=sm[:, 80 + qi:81 + qi],
                                                              in1=sg[:, t, hc * 3 + 1 + br:hc * 3 + 2 + br], op=ALU.mult), r=[b_sm, b_sg], w=[b_sm])
                        K.op("dve", lambda e: e.scalar_tensor_tensor(out=oacc[:, qi, hc * 64:(hc + 1) * 64], in0=po[:, 0:64], scalar=sm[:, 80 + qi:81 + qi],
                                                                     in1=oacc[:, qi, hc * 64:(hc + 1) * 64], op0=ALU.mult, op1=ALU.add),
                             r=[pob, b_sm, b_oacc], w=[b_oacc])
            emit_qk(0)
            for k in range(len(items)):
                if k + 1 < len(items):
                    emit_qk(k + 1)
                emit_pv(k)
            K.bank_rng = (4, 8)
            for qi in range(4):
                t = 4 * G + qi
                pt, pb = K.bank()
                for c in range(4):
                    K.op("pe", lambda e: e.transpose(out=pt[:, c * 128:(c + 1) * 128], in_=oacc[:, qi, c * 128:(c + 1) * 128], identity=self.ident),
                         r=[b_oacc, self.b_ident], w=[pb])
                K.op("act", lambda e: e.copy(out=oT[:, :, qi * 128:(qi + 1) * 128], in_=pt[:, :].rearrange("p (c n) -> p c n", c=4)),
                     r=[pb], w=[b_oT])
            for qi in range(4):
                t = 4 * G + qi
                for hf in range(2):
                    pt, pb = K.bank()
                    for c in range(4):
                        K.op("pe", lambda e: e.matmul(pt[:, :], lhsT=oT[:, c, qi * 128:(qi + 1) * 128], rhs=wo[:, c, hf * 512:(hf + 1) * 512],
                                                      start=(c == 0), stop=(c == 3)), r=[b_oT, b_wo], w=[pb])
                    xs = self.x_sb[:, t, hf * 512:(hf + 1) * 512]
                    K.op("dve", lambda e: e.tensor_tensor(out=xs, in0=pt[:, :], in1=xs, op=ALU.add), r=[pb, self.xb[t]], w=[self.xb[t]])
        K.bank_rng = (0, 8)
        K.barrier()
        A.release()
        A.release()


for _n, _f in list(vars(_EvenMixin).items()):
    if callable(_f) and not _n.startswith("__"):
        setattr(_Prog, _n, _f)


N_ODD_BLK = 18
ENABLE_RWKV = os.environ.get("KERNEL_ENABLE_RWKV", "1") == "1"
LNX_EPS = 64e-5
DECAY_C = -0.6065306597126334


def _odd_w_blocks(w_in):
    cols = []
    for base in (0, 512, 1024):
        for c in range(4):
            cols.append(list(range(base + c * 128, base + (c + 1) * 128)))
    cols.append(list(range(1536, 1664)))
    cols.append(list(range(1664, 1792)))
    for c in range(4):
        cols.append(list(range(1792 + c * 128, 1792 + (c + 1) * 128)))
    out = np.zeros((len(cols), 128, 8, 128), np.float32)
    for i, cl in enumerate(cols):
        out[i] = w_in[:, np.array(cl)].reshape(8, 128, 128).transpose(1, 0, 2)
    return out


def _odd_consts():
    c = {}
    p = np.arange(128)
    c["blockones"] = (p[:, None] // 64 == p[None, :] // 64).astype(np.float32)
    same = (p[:, None] // 64 == p[None, :] // 64)
    mA = same & (p[None, :] < p[:, None])
    mX = same & (p[:, None] < p[None, :])
    mI = same & (p[:, None] <= p[None, :])
    c["rmasks"] = np.ascontiguousarray(np.stack([mA, mX, mI], axis=1).astype(np.float32))
    invc = np.zeros((128, 4, 16), np.float32)
    for gi, w in enumerate((2, 4, 8, 16)):
        invc[:, gi, :] = 1.0 / np.minimum(np.arange(16) + 1, w)
    c["invc"] = invc
    return c


class _OddMixin:
    def add_wout(self, oT, b_oT, src, load):
        K = self.K
        wo, b_wo = load(src, mul=self.bc[2], b_mul=self.b_bc[2])
        for t in range(NT):
            for hf in range(2):
                pt, pb = K.bank()
                K.op("pe", lambda e: e.matmul(pt[:, :], lhsT=oT[:, t * 128:(t + 1) * 128], rhs=wo[:, hf * 512:(hf + 1) * 512],
                                              start=True, stop=True), r=[b_oT, b_wo], w=[pb])
                xs = self.x_sb[:, t, hf * 512:(hf + 1) * 512]
                K.op("dve", lambda e: e.tensor_tensor(out=xs, in0=pt[:, :], in1=xs, op=ALU.add), r=[pb, self.xb[t]], w=[self.xb[t]])

    def add_wout_multi(self, oT4, b_oT4, w_out_dram, row0, nch, stage, b_stage, wo4, b_wo4):
        K = self.K
        for c in range(nch):
            K.dma(stage, w_out_dram[row0 + c * 128:row0 + (c + 1) * 128, :], w=[b_stage])
            K.op("pool", lambda e: e.tensor_tensor(out=wo4[:, c, :], in0=stage, in1=self.bc[2], op=ALU.mult), r=[b_stage, self.b_bc[2]], w=[b_wo4])
        for t in range(NT):
            for hf in range(2):
                pt, pb = K.bank()
                for c in range(nch):
                    K.op("pe", lambda e: e.matmul(pt[:, :], lhsT=oT4[:, c, t * 128:(t + 1) * 128], rhs=wo4[:, c, hf * 512:(hf + 1) * 512],
                                                  start=(c == 0), stop=(c == nch - 1)), r=[b_oT4, b_wo4], w=[pb])
                xs = self.x_sb[:, t, hf * 512:(hf + 1) * 512]
                K.op("dve", lambda e: e.tensor_tensor(out=xs, in0=pt[:, :], in1=xs, op=ALU.add), r=[pb, self.xb[t]], w=[self.xb[t]])

    def odd_mixer(self):
        nc, K, A = self.nc, self.K, self.A
        l = 1
        self.load_mod_bc(l, 0, self.norm_mix[l:l + 1, :])
        HTB = 8 * S * 2
        hT = A.alloc_top(HTB, BF16).rearrange("p (k s) -> p k s", k=8)
        b_hT = [Buf("ohT%d" % t) for t in range(NT)]
        self.norm_transpose(hT, b_hT)
        wsrc = lambda i: self.odd_w[i].rearrange("p k n -> p (k n)")
        scratch = self.bcall[:, 0:2 * D]

        A.mark()
        load = self.make_wloader(3, 1)
        uT = f32(A, 16 + S)
        s1 = f32(A, 16 + S)
        s2 = f32(A, 16 + S)
        b_uT, b_s1, b_s2 = Buf("uT"), Buf("s1"), Buf("s2")
        pooled = bf(A, S)
        b_pooled = Buf("pooled")
        oT4 = bf(A, 4 * S).rearrange("p (c s) -> p c s", c=4)
        b_oT = Buf("poT")
        pwo4 = bf(A, 4 * 1024).rearrange("p (c n) -> p c n", c=4)
        b_pwo4 = Buf("pwo4")
        pstage = f32(A, 1024)
        b_pstage = Buf("pstage")
        pws = f32(A, 128)
        pwb = bf(A, 128)
        b_pws, b_pwb = Buf("pws"), Buf("pwb")
        psc = f32(A, 4)
        invc = f32(A, 64).rearrange("p (g n) -> p g n", g=4)
        t16 = f32(A, 16)
        b_pc = Buf("poolconst")
        b_t16 = Buf("t16")
        K.dma(psc, self.pool_scale, w=[b_pc])
        K.dma(invc, self.invc_in, w=[b_pc])
        for a_, b_ in ((uT, b_uT), (s1, b_s1), (s2, b_s2)):
            K.op("dve", lambda e: e.memset(a_[:, 0:16], 0.0), w=[b_])
        for gi in range(4):
            win = 2 << gi
            wv, b_wv = load(wsrc(14 + gi))
            K.dma(pws, self.pool_w[gi], w=[b_pws])
            K.op("pool", lambda e: e.tensor_copy(out=pwb, in_=pws), r=[b_pws], w=[b_pwb])
            for G in range(4):
                p_, pb_ = self.proj_fm(wv, b_wv, hT, b_hT, G)
                K.op("act", lambda e: e.copy(out=uT[:, 16 + G * 512:16 + (G + 1) * 512], in_=p_[:, :]), r=[pb_], w=[b_uT])
            src, b_src = uT, b_uT
            for step in range(gi + 1):
                sh = 1 << step
                dst, b_dst = (s1, b_s1) if step % 2 == 0 else (s2, b_s2)
                K.op("dve", lambda e: e.tensor_tensor(out=dst[:, 16:16 + S], in0=src[:, 16:16 + S], in1=src[:, 16 - sh:16 - sh + S], op=ALU.add),
                     r=[b_src], w=[b_dst])
                src, b_src = dst, b_dst
            K.op("dve", lambda e: e.scalar_tensor_tensor(out=pooled, in0=src[:, 16:16 + S], scalar=1.0 / win, in1=uT[:, 16:16 + S],
                                                         op0=ALU.mult, op1=ALU.subtract), r=[b_src, b_uT], w=[b_pooled])
            K.op("dve", lambda e: e.tensor_tensor(out=t16, in0=src[:, 16:32], in1=invc[:, gi, :], op=ALU.mult), r=[b_src, b_pc], w=[b_t16])
            K.op("dve", lambda e: e.tensor_tensor(out=pooled[:, 0:16], in0=t16, in1=uT[:, 16:32], op=ALU.subtract),
                 r=[b_t16, b_uT, b_pooled], w=[b_pooled])
            for G in range(4):
                pt, pb = K.bank()
                K.op("pe", lambda e: e.matmul(pt[:, :], lhsT=pwb, rhs=pooled[:, G * 512:(G + 1) * 512], start=True, stop=True),
                     r=[b_pwb, b_pooled], w=[pb])
                K.op("dve", lambda e: e.tensor_scalar(out=oT4[:, gi, G * 512:(G + 1) * 512], in0=pt[:, :], scalar1=psc[:, gi:gi + 1], scalar2=None,
                                                      op0=ALU.mult), r=[pb, b_pc], w=[b_oT])
        self.add_wout_multi(oT4, b_oT, self.odd_w_out, 512, 4, pstage, b_pstage, pwo4, b_pwo4)
        K.barrier()
        A.release()
        if os.environ.get("KDBG") == "pool" or not ENABLE_RWKV:
            A.release_top(HTB)
            return

        A.mark()
        chv = f32(A, 28).rearrange("p (k c) -> p k c", k=7)
        mu = f32(A, 14)
        so = [0]

        def sc(n):
            v = scratch[:, so[0]:so[0] + n]
            so[0] += n
            return v
        bones = sc(128)
        rmask = sc(3 * 128).rearrange("p (m n) -> p m n", m=3)
        ones64 = f32(A, 64)
        wa2s = f32(A, 512)
        wa2b = sc(256).bitcast(BF16)
        g2s = wa2s
        g2b = sc(256).bitcast(BF16)
        b_cst = Buf("rconst")
        b_wa2s, b_wa2b, b_g2s, b_g2b = Buf("wa2s"), Buf("wa2b"), Buf("g2s"), Buf("g2b")
        K.dma(chv, self.chv_in, w=[b_cst])
        K.dma(mu, self.mu_in, w=[b_cst])
        K.dma(bones, self.blockones_in, w=[b_cst])
        K.dma(rmask, self.rmasks_in, w=[b_cst])
        K.op("dve", lambda e: e.memset(ones64, 1.0), w=[b_cst])
        K.dma(wa2s, self.wa2_in, w=[b_wa2s])
        K.op("pool", lambda e: e.tensor_copy(out=wa2b, in_=wa2s), r=[b_wa2s], w=[b_wa2b])
        K.dma(g2s, self.g2_in, w=[b_wa2s])
        K.op("pool", lambda e: e.tensor_copy(out=g2b, in_=g2s), r=[b_wa2s], w=[b_g2b])
        Sx = [f32(A, S) for _ in range(8)]
        b_S = [Buf("S%d" % i) for i in range(8)]
        Tall = f32(A, 3 * S + 16)
        sglT = bf(A, S)
        b_sgl = Buf("sglT")
        T3 = Tall[:, 0:3 * S // 2].bitcast(BF16).rearrange("p (m t c) -> p m t c", m=3, t=NT)
        Ab, Bb, Kb = [Tall[:, 3 * S // 2 + i * (S // 2):3 * S // 2 + (i + 1) * (S // 2)].bitcast(BF16) for i in range(3)]
        b_Ab, b_Bb, b_Kb, b_Rb = Buf("Ab"), Buf("Bb"), Buf("Kb"), Buf("Rb")
        b_T = Buf("Ttm")
        raw = Tall[:, 0:S + 1]
        b_raw = Buf("raw")
        waT = Tall[:, S + 8:S + 8 + S // 2].bitcast(BF16)
        b_waT = Buf("waT")
        lbase = S + 8 + S // 2
        lstg = Tall[:, lbase:lbase + 1024]
        lwb = [Tall[:, lbase + 1024 + i * 512:lbase + 1024 + (i + 1) * 512].bitcast(BF16) for i in range(2)]
        b_lstg, b_lwb = Buf("lstg"), [Buf("lwb0"), Buf("lwb1")]
        lst = {"w": 0}

        def lload(src):
            wi = lst["w"] % 2
            lst["w"] += 1
            K.dma(lstg, src, w=[b_lstg])
            K.op("pool", lambda e: e.tensor_copy(out=lwb[wi], in_=lstg), r=[b_lstg], w=[b_lwb[wi]])
            return lwb[wi], b_lwb[wi]

        Hs = sc(64)
        Hb = sc(32).bitcast(BF16)
        b_Hb = Buf("Hb")
        Wsb = sc(64).bitcast(BF16)
        Usb = sc(64).bitcast(BF16)
        W2sb = sc(128)
        Y1sb = sc(128)
        b_W2, b_Y1 = Buf("W2sb"), Buf("Y1sb")
        PC = sc(32)
        st1 = sc(32)
        st2 = sc(32)
        st3 = sc(32)
        b_H, b_W, b_U, b_PC, b_st = Buf("H"), Buf("W"), Buf("U"), Buf("PC"), Buf("st")
        fmo = sc(0)

        for P in range(4):
            K.op("dve", lambda e: e.memset(raw[:, 0:1], 0.0), w=[b_raw])

            def proj_shift(bi, mucol, dst, b_dst):
                wv, b_wv = lload(wsrc(bi))
                for G in range(4):
                    p_, pb_ = self.proj_fm(wv, b_wv, hT, b_hT, G)
                    K.op("act", lambda e: e.copy(out=raw[:, 1 + G * 512:1 + (G + 1) * 512], in_=p_[:, :]), r=[pb_], w=[b_raw])
                K.op("dve", lambda e: e.tensor_tensor(out=dst, in0=raw[:, 0:S], in1=raw[:, 1:S + 1], op=ALU.subtract), r=[b_raw], w=[b_dst])
                K.op("dve", lambda e: e.scalar_tensor_tensor(out=dst, in0=dst, scalar=mu[:, mucol:mucol + 1], in1=raw[:, 1:S + 1],
                                                             op0=ALU.mult, op1=ALU.add), r=[b_dst, b_raw, b_cst], w=[b_dst])
            proj_shift(P, P, Sx[0], b_S[0])
            proj_shift(4 + P, 4 + P, Sx[1], b_S[1])
            proj_shift(8 + P, 8 + P, Sx[2], b_S[2])
            proj_shift(12, 12, Sx[6], b_S[6])
            K.op("act", lambda e: e.activation(out=waT[0:64, :], in_=Sx[6][0:64, :], func=AF.Tanh), r=[b_S[6]], w=[b_waT])
            K.op("act", lambda e: e.copy(out=waT[64:128, :], in_=Sx[6][64:128, :]), r=[b_S[6]], w=[b_waT])
            proj_shift(13, 13, Sx[6], b_S[6])
            K.op("act", lambda e: e.activation(out=sglT, in_=Sx[6], func=AF.Sigmoid), r=[b_S[6]], w=[b_sgl])
            for G in range(4):
                sl = slice(G * 512, (G + 1) * 512)
                pt, pb = K.bank()
                K.op("pe", lambda e: e.matmul(pt[:, :], lhsT=wa2b[0:64, P * 128:(P + 1) * 128], rhs=waT[0:64, sl], start=True, stop=True),
                     r=[b_wa2b, b_waT], w=[pb])
                K.op("act", lambda e: e.activation(out=Sx[3][:, sl], in_=pt[:, :], func=AF.Sigmoid, bias=chv[:, 0, P:P + 1]),
                     r=[pb, b_cst], w=[b_S[3]])
                pt, pb = K.bank()
                K.op("pe", lambda e: e.matmul(pt[:, :], lhsT=wa2b[64:128, P * 128:(P + 1) * 128], rhs=waT[64:128, sl], start=True, stop=True),
                     r=[b_wa2b, b_waT], w=[pb])
                K.op("act", lambda e: e.activation(out=Sx[4][:, sl], in_=pt[:, :], func=AF.Sigmoid, bias=chv[:, 1, P:P + 1]),
                     r=[pb, b_cst], w=[b_S[4]])
            dve = lambda fn, r, w: K.op("dve", fn, r=r, w=w)
            dve(lambda e: e.tensor_scalar(out=Sx[3], in0=Sx[3], scalar1=DECAY_C, scalar2=None, op0=ALU.mult), [b_S[3]], [b_S[3]])
            dve(lambda e: e.tensor_scalar(out=Sx[5], in0=Sx[1], scalar1=chv[:, 2, P:P + 1], scalar2=None, op0=ALU.mult),
                [b_S[1], b_cst], [b_S[5]])
            K.op("act", lambda e: e.activation(out=raw[:, 0:S], in_=Sx[5], func=AF.Square), r=[b_S[5]], w=[b_raw])
            for G in range(4):
                sl = slice(G * 512, (G + 1) * 512)
                pt, pb = K.bank()
                K.op("pe", lambda e: e.matmul(pt[:, :], lhsT=bones, rhs=raw[:, sl], start=True, stop=True), r=[b_cst, b_raw], w=[pb])
                K.op("act", lambda e: e.activation(out=Sx[7][:, sl], in_=pt[:, :], func=AF.Sqrt), r=[pb], w=[b_S[7]])
            dve(lambda e: e.tensor_scalar(out=Sx[7], in0=Sx[7], scalar1=1e-12, scalar2=None, op0=ALU.max), [b_S[7]], [b_S[7]])
            dve(lambda e: e.reciprocal(out=Sx[7], in_=Sx[7]), [b_S[7]], [b_S[7]])
            dve(lambda e: e.tensor_tensor(out=Sx[5], in0=Sx[5], in1=Sx[7], op=ALU.mult), [b_S[5], b_S[7]], [b_S[5]])
            dve(lambda e: e.tensor_scalar(out=Sx[7], in0=Sx[4], scalar1=-1.0, scalar2=chv[:, 3, P:P + 1], op0=ALU.add, op1=ALU.mult),
                [b_S[4], b_cst], [b_S[7]])
            dve(lambda e: e.scalar_tensor_tensor(out=Sx[1], in0=Sx[7], scalar=1.0, in1=Sx[1], op0=ALU.add, op1=ALU.mult),
                [b_S[7], b_S[1]], [b_S[1]])
            dve(lambda e: e.tensor_tensor(out=Sx[4], in0=Sx[5], in1=Sx[4], op=ALU.mult), [b_S[5], b_S[4]], [b_S[4]])
            dve(lambda e: e.scalar_tensor_tensor(out=raw[:, 0:S], in0=Sx[0], scalar=chv[:, 6, P:P + 1], in1=Sx[1], op0=ALU.mult, op1=ALU.mult),
                [b_S[0], b_S[1], b_cst, b_raw], [b_raw])
            for G in range(4):
                sl = slice(G * 512, (G + 1) * 512)
                pt, pb = K.bank()
                K.op("pe", lambda e: e.matmul(pt[:, :], lhsT=bones, rhs=raw[:, sl], start=True, stop=True), r=[b_cst, b_raw], w=[pb])
                dve(lambda e: e.tensor_tensor(out=Sx[7][:, sl], in0=pt[:, :], in1=Sx[2][:, sl], op=ALU.mult), [pb, b_S[2], b_S[7]], [b_S[7]])
            for c in range(32):
                cs = slice(c * 64, (c + 1) * 64)
                dve(lambda e: e.tensor_tensor_scan(out=Sx[6][:, cs], data0=ones64, data1=Sx[3][:, cs], initial=0.0, op0=ALU.mult, op1=ALU.add),
                    [b_S[3], b_cst, b_S[6]], [b_S[6]])
            K.op("act", lambda e: e.activation(out=PC, in_=Sx[6][:, 63:S:64], func=AF.Exp), r=[b_S[6]], w=[b_PC])
            dve(lambda e: e.tensor_tensor(out=Sx[3], in0=Sx[6], in1=Sx[3], op=ALU.subtract), [b_S[6], b_S[3]], [b_S[3]])
            K.op("act", lambda e: e.activation(out=Sx[3], in_=Sx[3], func=AF.Exp), r=[b_S[3]], w=[b_S[3]])
            dve(lambda e: e.scalar_tensor_tensor(out=Sx[5], in0=Sx[5], scalar=-1.0, in1=Sx[3], op0=ALU.mult, op1=ALU.mult),
                [b_S[5], b_S[3]], [b_S[5]])
            K.op("act", lambda e: e.activation(out=Sx[3], in_=Sx[6], func=AF.Exp), r=[b_S[6], b_S[5]], w=[b_S[3]])
            dve(lambda e: e.tensor_tensor(out=Sx[0], in0=Sx[0], in1=Sx[3], op=ALU.mult), [b_S[0], b_S[3]], [b_S[0]])
            K.op("act", lambda e: e.activation(out=Sx[3], in_=Sx[6], func=AF.Exp, scale=-1.0), r=[b_S[6], b_S[0]], w=[b_S[3]])
            dve(lambda e: e.tensor_tensor(out=Sx[4], in0=Sx[4], in1=Sx[3], op=ALU.mult), [b_S[4], b_S[3]], [b_S[4]])
            dve(lambda e: e.tensor_tensor(out=Sx[1], in0=Sx[1], in1=Sx[3], op=ALU.mult), [b_S[1], b_S[3]], [b_S[1]])
            c3 = lambda a: a.rearrange("p (c n) -> p c n", n=64)
            pcb = PC.unsqueeze(2).to_broadcast([128, 32, 64])
            dve(lambda e: e.tensor_tensor(out=c3(Sx[3]), in0=c3(Sx[4]), in1=pcb, op=ALU.mult), [b_S[4], b_PC, b_S[3]], [b_S[3]])
            dve(lambda e: e.tensor_tensor(out=c3(Sx[6]), in0=c3(Sx[1]), in1=pcb, op=ALU.mult), [b_S[1], b_PC, b_S[6]], [b_S[6]])
            K.barrier()
            CUT = os.environ.get("KCUT2", "")
            if CUT == "prep":
                break
            K.op("act", lambda e: e.copy(out=Ab, in_=Sx[5]), r=[b_S[5]], w=[b_Ab])
            K.op("pool", lambda e: e.tensor_copy(out=Bb, in_=Sx[4]), r=[b_S[4]], w=[b_Bb])
            K.op("dve", lambda e: e.tensor_copy(out=Kb, in_=Sx[1]), r=[b_S[1]], w=[b_Kb])
            for tau in range(NT):
                pt, pb = K.bank()
                for m, si in enumerate((2, 3, 6)):
                    K.op("pe", lambda e: e.transpose(out=pt[:, m * 128:(m + 1) * 128], in_=Sx[si][:, tau * 128:(tau + 1) * 128], identity=self.ident),
                         r=[b_S[si], self.b_ident], w=[pb])
                K.op("act", lambda e: e.copy(out=T3[:, :, tau, :], in_=pt[:, 0:384].rearrange("p (m c) -> p m c", m=3)), r=[pb], w=[b_T])
            K.barrier()
            if CUT == "tm":
                break
            ytm = Sx[2].rearrange("p (t c) -> p t c", t=NT)
            b_ytm = Buf("ytm")
            mats = Sx[3][:, 0:S // 2].bitcast(BF16).rearrange("p (m i n) -> p m i n", m=4, i=4)
            b_mats = Buf("mats")
            Rb = Sx[3][:, S // 2:S].bitcast(BF16)
            dbl = Sx[6][:, 0:S // 2].bitcast(BF16).rearrange("p (m i n) -> p m i n", m=4, i=4)
            b_dbl = [Buf("dbl%d" % i) for i in range(4)]
            K.op("act", lambda e: e.copy(out=Rb, in_=Sx[0]), r=[b_S[0]], w=[b_Rb])
            K.op("dve", lambda e: e.memset(Hs, 0.0), w=[b_H])
            K.op("dve", lambda e: e.memset(Hb, 0.0), w=[b_Hb])
            At, Bt, Kt, Rt = Ab, Bb, Kb, Rb
            b_At, b_Bt, b_Kt, b_Rt = b_Ab, b_Bb, b_Kb, b_Rb
            mbc = lambda m: rmask[:, m, :].unsqueeze(1).to_broadcast([128, 4, 128])
            idbc = self.ident.unsqueeze(1).to_broadcast([128, 4, 128])
            v4 = lambda pt: pt[:, :].rearrange("p (i n) -> p i n", i=4)
            mats2 = Sx[6][:, S // 2:S].bitcast(BF16).rearrange("p (m i n) -> p m i n", m=4, i=4)
            matsb = [mats, mats2]
            b_matsb = [b_mats, Buf("mats2")]

            def pre_steps(nb):
                mt, b_mt = matsb[nb % 2], b_matsb[nb % 2]
                steps = []

                def mm_items(lh, b_lh, rh, b_rh):
                    res = []
                    for h in range(2):
                        pt, pb = K.bank()
                        for tl in range(2):
                            tok = slice((2 * nb + tl) * 128, (2 * nb + tl + 1) * 128)
                            K.op("pe", lambda e: e.matmul(pt[:, tl * 128:(tl + 1) * 128], lhsT=lh[64 * h:64 * h + 64, tok], rhs=rh[64 * h:64 * h + 64, tok],
                                                          start=True, stop=True), r=[b_lh, b_rh], w=[pb])
                        res.append((pt, pb))
                    return res

                def evac_items(res, dst, m, bdst):
                    for h in range(2):
                        pt, pb = res[h]
                        dve(lambda e: e.tensor_tensor(out=dst[:, h::2, :], in0=pt[:, 0:256].rearrange("p (i n) -> p i n", i=2),
                                                      in1=rmask[:, m, :].unsqueeze(1).to_broadcast([128, 2, 128]), op=ALU.mult), [pb, b_cst], [bdst])
                steps.append(lambda: evac_items(mm_items(At, b_At, Bt, b_Bt), dbl[:, 0], 0, b_dbl[0]))
                steps.append(lambda: evac_items(mm_items(Bt, b_Bt, At, b_At), dbl[:, 1], 1, b_dbl[1]))
                steps.append(lambda: evac_items(mm_items(Kt, b_Kt, At, b_At), mt[:, 0], 1, b_mt))
                steps.append(lambda: evac_items(mm_items(Bt, b_Bt, Rt, b_Rt), mt[:, 1], 2, b_mt))
                steps.append(lambda: evac_items(mm_items(Kt, b_Kt, Rt, b_Rt), mt[:, 2], 2, b_mt))
                steps.append(lambda: dve(lambda e: e.tensor_tensor(out=mt[:, 3], in0=dbl[:, 1], in1=idbc, op=ALU.add), [b_dbl[1], self.b_ident], [b_mt]))
                order = [(0, 1, 2, 3), (2, 3, 0, 1)]
                for lev in range(1, 6):
                    ca, cx, na, nx = order[(lev - 1) % 2]

                    def st_a(ca=ca, cx=cx, na=na):
                        pt, pb = K.bank()
                        for idx in range(4):
                            K.op("pe", lambda e: e.matmul(pt[:, idx * 128:(idx + 1) * 128], lhsT=dbl[:, cx, idx, :], rhs=dbl[:, ca, idx, :], start=True, stop=True),
                                 r=[b_dbl[cx], b_dbl[ca]], w=[pb])
                        K.op("act", lambda e: e.copy(out=dbl[:, na], in_=v4(pt)), r=[pb], w=[b_dbl[na]])

                    def st_x(ca=ca, cx=cx, nx=nx):
                        pt, pb = K.bank()
                        for idx in range(4):
                            K.op("pe", lambda e: e.matmul(pt[:, idx * 128:(idx + 1) * 128], lhsT=dbl[:, ca, idx, :], rhs=dbl[:, cx, idx, :], start=True, stop=True),
                                 r=[b_dbl[cx], b_dbl[ca]], w=[pb])
                        K.op("act", lambda e: e.copy(out=dbl[:, nx], in_=v4(pt)), r=[pb], w=[b_dbl[nx]])

                    def st_t(na=na):
                        pt, pb = K.bank()
                        for idx in range(4):
                            K.op("pe", lambda e: e.matmul(pt[:, idx * 128:(idx + 1) * 128], lhsT=dbl[:, na, idx, :], rhs=mt[:, 3, idx, :], start=True, stop=True),
                                 r=[b_dbl[na], b_mt], w=[pb])
                        dve(lambda e: e.tensor_tensor(out=mt[:, 3], in0=mt[:, 3], in1=v4(pt), op=ALU.add), [pb, b_mt], [b_mt])
                    steps.append(st_a)
                    if lev < 5:
                        steps.append(st_x)
                    steps.append(st_t)
                return steps

            def scan_steps(nb):
                mt, b_mt = matsb[nb % 2], b_matsb[nb % 2]
                steps = []
                for tl in range(2):
                    for hf in range(2):
                        def mk(tl=tl, hf=hf):
                            tau = 2 * nb + tl
                            tok = slice(tau * 128, (tau + 1) * 128)
                            c = tau * 2 + hf
                            ph = slice(64 * hf, 64 * hf + 64)
                            HS = [slice(0, 64), slice(64, 128)]

                            def s_w2():
                                for h in range(2):
                                    hs = HS[h]
                                    pW2, pW2b = K.bank()
                                    K.op("pe", lambda e: e.matmul(pW2[:, 0:64], lhsT=mt[ph, 0, tl * 2 + h, :], rhs=T3[ph, 0, tau, hs], start=True, stop=True),
                                         r=[b_mt, b_T], w=[pW2b])
                                    K.op("act", lambda e: e.copy(out=W2sb[ph, hs], in_=pW2[ph, 0:64]), r=[pW2b], w=[b_W2])

                            def s_w():
                                for h in range(2):
                                    hs = HS[h]
                                    pW, pWb = K.bank()
                                    K.op("pe", lambda e: e.matmul(pW[:, 0:64], lhsT=At[hs, tok], rhs=Hb[hs, :], start=True, stop=True), r=[b_At, b_Hb], w=[pWb])
                                    dve(lambda e: e.tensor_tensor(out=Wsb[ph, hs], in0=pW[ph, 0:64], in1=W2sb[ph, hs], op=ALU.add), [pWb, b_W2], [b_W])

                            def s_u():
                                for h in range(2):
                                    hs = HS[h]
                                    pU, pUb = K.bank()
                                    K.op("pe", lambda e: e.matmul(pU[:, 0:64], lhsT=mt[ph, 3, tl * 2 + h, :], rhs=Wsb[ph, hs], start=True, stop=True),
                                         r=[b_mt, b_W], w=[pUb])
                                    dve(lambda e: e.tensor_copy(out=Usb[ph, hs], in_=pU[ph, 0:64]), [pUb], [b_U])

                            def s_y1():
                                for h in range(2):
                                    hs = HS[h]
                                    pY1, pY1b = K.bank()
                                    K.op("pe", lambda e: e.matmul(pY1[:, 0:64], lhsT=Rt[hs, tok], rhs=Hb[hs, :], start=True, stop=True), r=[b_Rt, b_Hb], w=[pY1b])
                                    K.op("act", lambda e: e.copy(out=Y1sb[ph, hs], in_=pY1[ph, 0:64]), r=[pY1b], w=[b_Y1])

                            def s_h():
                                for h in range(2):
                                    hs = HS[h]
                                    pH, pHb = K.bank()
                                    K.op("pe", lambda e: e.matmul(pH[:, 0:64], lhsT=T3[ph, 1, tau, :], rhs=Usb[ph, hs], start=True, stop=False), r=[b_T, b_U], w=[pHb])
                                    K.op("pe", lambda e: e.matmul(pH[:, 0:64], lhsT=T3[ph, 2, tau, :], rhs=T3[ph, 0, tau, hs], start=False, stop=True), r=[b_T], w=[pHb])
                                    dve(lambda e: e.scalar_tensor_tensor(out=Hb[hs, :], in0=Hs[hs, :], scalar=PC[hs, c:c + 1], in1=pH[hs, 0:64],
                                                                         op0=ALU.mult, op1=ALU.add), [pHb, b_H, b_PC], [b_Hb])
                                    dve(lambda e: e.scalar_tensor_tensor(out=Hs[hs, :], in0=Hs[hs, :], scalar=PC[hs, c:c + 1], in1=pH[hs, 0:64],
                                                                         op0=ALU.mult, op1=ALU.add), [pHb, b_H, b_PC], [b_H])

                            def s_y2():
                                for h in range(2):
                                    hs = HS[h]
                                    pY, pYb = K.bank()
                                    K.op("pe", lambda e: e.matmul(pY[:, 0:64], lhsT=mt[ph, 1, tl * 2 + h, :], rhs=Usb[ph, hs], start=True, stop=False),
                                         r=[b_mt, b_U], w=[pYb])
                                    K.op("pe", lambda e: e.matmul(pY[:, 0:64], lhsT=mt[ph, 2, tl * 2 + h, :], rhs=T3[ph, 0, tau, hs], start=False, stop=True),
                                         r=[b_mt, b_T], w=[pYb])
                                    dve(lambda e: e.tensor_tensor(out=ytm[ph, tau, hs], in0=pY[ph, 0:64], in1=Y1sb[ph, hs], op=ALU.add), [pYb, b_Y1], [b_ytm])
                            return [s_w2, s_y1, s_w, s_u, s_h, s_y2]
                        steps.extend(mk())
                return steps

            cur = pre_steps(0)
            for f_ in cur:
                f_()
            for nb in range(8):
                sc_ = scan_steps(nb)
                pr_ = pre_steps(nb + 1) if nb + 1 < 8 else []
                n_ = max(len(sc_), len(pr_))
                for i_ in range(n_):
                    if i_ < len(sc_):
                        sc_[i_]()
                    if i_ < len(pr_):
                        pr_[i_]()
            K.barrier()
            K.barrier()
            if CUT in ("pre", "scan", "pre1", "pre2", "prem"):
                break
            y3 = Sx[2].rearrange("p (g n) -> p g n", n=64)
            sq = Tall[:, S:2 * S]
            b_sq = Buf("sq")
            K.op("act", lambda e: e.activation(out=sq, in_=Sx[2], func=AF.Square), r=[b_ytm], w=[b_sq])
            dve(lambda e: e.tensor_reduce(out=st1, in_=y3, axis=AX.X, op=ALU.add), [b_ytm], [b_st])
            dve(lambda e: e.tensor_reduce(out=st2, in_=sq.rearrange("p (g n) -> p g n", n=64), axis=AX.X, op=ALU.add), [b_sq], [b_st])
            dve(lambda e: e.tensor_scalar(out=st1, in0=st1, scalar1=1.0 / 64, scalar2=None, op0=ALU.mult), [b_st], [b_st])
            dve(lambda e: e.tensor_tensor(out=st3, in0=st1, in1=st1, op=ALU.mult), [b_st], [b_st])
            dve(lambda e: e.scalar_tensor_tensor(out=st2, in0=st2, scalar=1.0 / 64, in1=st3, op0=ALU.mult, op1=ALU.subtract), [b_st], [b_st])
            dve(lambda e: e.tensor_scalar(out=st2, in0=st2, scalar1=LNX_EPS, scalar2=None, op0=ALU.add), [b_st], [b_st])
            K.op("act", lambda e: e.activation(out=st2, in_=st2, func=AF.Sqrt), r=[b_st], w=[b_st])
            dve(lambda e: e.reciprocal(out=st2, in_=st2), [b_st], [b_st])
            dve(lambda e: e.tensor_tensor(out=y3, in0=y3, in1=st1.unsqueeze(2).to_broadcast([128, 32, 64]), op=ALU.subtract), [b_ytm, b_st], [b_ytm])
            dve(lambda e: e.tensor_tensor(out=y3, in0=y3, in1=st2.unsqueeze(2).to_broadcast([128, 32, 64]), op=ALU.mult), [b_ytm, b_st], [b_ytm])
            fm = Tall[:, 2 * S:3 * S]
            b_fm = Buf("fm")
            for tq in range(4):
                pt, pb = K.bank()
                for i4 in range(4):
                    tau = tq * 4 + i4
                    K.op("pe", lambda e: e.transpose(out=pt[:, i4 * 128:(i4 + 1) * 128], in_=ytm[:, tau, :], identity=self.ident),
                         r=[b_ytm, self.b_ident], w=[pb])
                dve(lambda e: e.tensor_scalar(out=fm[:, tq * 512:(tq + 1) * 512], in0=pt[:, :], scalar1=chv[:, 4, P:P + 1], scalar2=chv[:, 5, P:P + 1],
                                              op0=ALU.mult, op1=ALU.add), [pb, b_cst], [b_fm])
            dve(lambda e: e.tensor_tensor(out=fm, in0=fm, in1=Sx[7], op=ALU.add), [b_fm, b_S[7]], [b_fm])
            oTr = Tall[:, S:S + S // 2].bitcast(BF16)
            b_oTr = Buf("oTr")
            for G in range(4):
                sl = slice(G * 512, (G + 1) * 512)
                pt, pb = K.bank()
                K.op("pe", lambda e: e.matmul(pt[:, :], lhsT=g2b[:, P * 128:(P + 1) * 128], rhs=sglT[:, sl], start=True, stop=True),
                     r=[b_g2b, b_sgl], w=[pb])
                dve(lambda e: e.tensor_tensor(out=oTr[:, sl], in0=fm[:, sl], in1=pt[:, :], op=ALU.mult), [pb, b_fm, b_sq], [b_oTr])
            wo_s = Tall[:, 0:1024]
            b_wo_s = Buf("rwo_s")
            wo_b = Tall[:, 1024:1536].bitcast(BF16)
            b_wo_b = Buf("rwo_b")
            K.barrier()
            K.dma(wo_s, self.odd_w_out[P * 128:(P + 1) * 128, :], w=[b_wo_s])
            K.op("pool", lambda e: e.tensor_tensor(out=wo_b, in0=wo_s, in1=self.bc[2], op=ALU.mult), r=[b_wo_s, self.b_bc[2]], w=[b_wo_b])
            for t in range(NT):
                for hf in range(2):
                    pt, pb = K.bank()
                    K.op("pe", lambda e: e.matmul(pt[:, :], lhsT=oTr[:, t * 128:(t + 1) * 128], rhs=wo_b[:, hf * 512:(hf + 1) * 512],
                                                  start=True, stop=True), r=[b_oTr, b_wo_b], w=[pb])
                    xs = self.x_sb[:, t, hf * 512:(hf + 1) * 512]
                    dve(lambda e: e.tensor_tensor(out=xs, in0=pt[:, :], in1=xs, op=ALU.add), [pb, self.xb[t]], [self.xb[t]])
            K.barrier()
        A.release()
        A.release_top(HTB)


for _n, _f in list(vars(_OddMixin).items()):
    if callable(_f) and not _n.startswith("__"):
        setattr(_Prog, _n, _f)
```

```python
import os
import numpy as np
from contextlib import ExitStack
import concourse.bass as bass
import concourse.mybir as mybir
from concourse.bass_utils import run_bass_kernel_spmd

F32 = mybir.dt.float32
BF16 = mybir.dt.bfloat16
AF = mybir.ActivationFunctionType
ALU = mybir.AluOpType
AX = mybir.AxisListType

S = 2048
D = 1024
NT = S // 128
NS_DMA = 8
EPS = 1e-6
NEGB = -30000.0


class Buf:
    __slots__ = ("name", "w", "r", "excl")

    def __init__(self, name, excl=False):
        self.name = name
        self.w = None
        self.r = {}
        self.excl = excl


class Kern:
    def __init__(self, nc, es):
        self.nc = nc
        self.es = es
        self.eng = {"pe": nc.tensor, "dve": nc.vector, "act": nc.scalar, "pool": nc.gpsimd, "sp": nc.sync}
        self.sem = {n: es.enter_context(nc.semaphore("s_" + n)) for n in self.eng}
        self.cnt = {n: 0 for n in self.eng}
        self.seen = {n: {} for n in self.eng}
        self.dsem = {q: [es.enter_context(nc.semaphore("d_%s%d" % (q, i))) for i in range(NS_DMA)]
                     for q in ("sp", "pool", "act")}
        self.duse = {q: [0] * NS_DMA for q in self.dsem}
        self.dnext = {q: 0 for q in self.dsem}
        self.uid = 0
        self.banks = []
        self.bank_i = 0
        self.bank_rng = (0, 8)

    def wait(self, e, tok):
        _, key, h, v = tok
        if self.seen[e].get(key, 0) < v:
            self.eng[e].wait_ge(h, v)
            self.seen[e][key] = v

    def deps(self, e, r, w):
        toks = []
        for b in r:
            if b.w is not None and not (b.w[0] == e and e == "pe"):
                toks.append(b.w)
            if b.excl:
                for t in b.r.values():
                    if t[0] != e:
                        toks.append(t)
        for b in w:
            if b.w is not None and (b.w[0] != e or e != "pe"):
                toks.append(b.w)
            for t in b.r.values():
                if t[0] != e or e != "pe":
                    toks.append(t)
        return toks

    def op(self, e, fn, r=(), w=()):
        for t in self.deps(e, r, w):
            self.wait(e, t)
        ins = fn(self.eng[e])
        self.cnt[e] += 1
        ins.then_inc(self.sem[e], 1)
        T = (e, "s_" + e, self.sem[e], self.cnt[e])
        for b in r:
            b.r[e] = T
        for b in w:
            b.w = T
            b.r = {}
        return T

    def dma(self, out, in_, r=(), w=(), q="sp"):
        for t in self.deps("dma", r, w):
            self.wait(q, t)
        i = self.dnext[q]
        self.dnext[q] = (i + 1) % NS_DMA
        u = self.duse[q][i]
        key = "d_%s%d" % (q, i)
        if u > 0:
            self.wait(q, ("dma", key, self.dsem[q][i], 16 * u))
        ins = self.eng[q].dma_start(out=out, in_=in_)
        ins.then_inc(self.dsem[q][i], 16)
        self.duse[q][i] = u + 1
        T = ("dma", key, self.dsem[q][i], 16 * (u + 1))
        self.uid += 1
        for b in r:
            b.r[("dma", self.uid)] = T
        for b in w:
            b.w = T
            b.r = {}
        return T

    def barrier(self):
        toks = [(n, "s_" + n, self.sem[n], self.cnt[n]) for n in self.eng if self.cnt[n] > 0]
        for q in self.dsem:
            for i in range(NS_DMA):
                if self.duse[q][i] > 0:
                    toks.append(("dma", "d_%s%d" % (q, i), self.dsem[q][i], 16 * self.duse[q][i]))
        for e in self.eng:
            for t in toks:
                self.wait(e, t)

    def finish(self):
        for q in self.dsem:
            for i in range(NS_DMA):
                if self.duse[q][i] > 0:
                    self.wait("sp", ("dma", "d_%s%d" % (q, i), self.dsem[q][i], 16 * self.duse[q][i]))

    def bank(self):
        lo, hi = self.bank_rng
        if not (lo <= self.bank_i < hi):
            self.bank_i = lo
        b = self.banks[self.bank_i]
        self.bank_i += 1
        if self.bank_i >= hi:
            self.bank_i = lo
        return b


class Arena:
    def __init__(self, ap, nbytes):
        self.ap = ap
        self.n = nbytes
        self.off = 0
        self.marks = []

    def alloc(self, nbytes, dtype=F32, shape=None):
        nbytes = (nbytes + 31) // 32 * 32
        assert self.off + nbytes <= self.n, ("arena overflow", self.off, nbytes, self.n)
        v = self.ap[:, self.off // 4:(self.off + nbytes) // 4]
        self.off += nbytes
        if dtype != F32:
            v = v.bitcast(dtype)
        return v

    def alloc_top(self, nbytes, dtype=F32):
        nbytes = (nbytes + 31) // 32 * 32
        self.n -= nbytes
        assert self.off <= self.n, ("arena overflow(top)", self.off, nbytes, self.n)
        v = self.ap[:, self.n // 4:(self.n + nbytes) // 4]
        if dtype != F32:
            v = v.bitcast(dtype)
        return v

    def release_top(self, nbytes):
        self.n += (nbytes + 31) // 32 * 32

    def mark(self):
        self.marks.append(self.off)

    def release(self):
        self.off = self.marks.pop()


def f32(a, n):
    return a.alloc(4 * n, F32)[:, 0:n]


def bf(a, n):
    return a.alloc(2 * n, BF16)[:, 0:n]


def build_program(stages=("mix0", "moe0", "mix1", "moe1", "final"), dbg=None):
    nc = bass.Bass("TRN2", target_bir_lowering=False)
    es = ExitStack()
    with es:
        P = _Prog(nc, es, stages, dbg)
        P.emit()
    return nc


class _Prog:
    def __init__(self, nc, es, stages, dbg):
        self.nc = nc
        self.es = es
        self.stages = stages
        self.dbg = dbg
        self.K = Kern(nc, es)
        di = lambda name, shape: nc.dram_tensor(name, list(shape), F32, kind="ExternalInput").ap()
        self.x_in = di("x", [S, D])
        self.c_in = di("c", [128, 8])
        self.ada_w = di("ada_w", [2, 12, 128, 8, 512])
        self.ada_b = di("ada_b", [2, 6 * D])
        self.norm_mix = di("norm_mix", [2, D])
        self.norm_ffn = di("norm_ffn", [2, D])
        self.final_norm = di("final_norm", [1, D])
        self.router_w = di("router_w", [128, 8, 16])
        self.router_b = di("router_b", [1, 16])
        self.moe_wg = di("moe_w_gate", [2, 16, D, 512])
        self.moe_wu = di("moe_w_up", [2, 16, D, 512])
        self.moe_wd = di("moe_w_down", [2, 16, 512, D])
        self.ident_in = di("ident", [128, 128])
        dib = lambda name, shape: nc.dram_tensor(name, list(shape), BF16, kind="ExternalInput").ap()
        self.even_w = di("even_w", [N_EVEN_BLK, 128, 8, 128])
        self.even_w_out = di("even_w_out", [D, D])
        self.conv_w = di("conv_w", [128, 4, 3])
        self.rope_cos = di("rope_cos", [128, S])
        self.rope_sin = di("rope_sin", [128, S])
        self.cmp_pos = di("cmp_pos", [128, 2, 32])
        self.cmp_w1 = di("cmp_w1", [2, 4, 128, 8, 256])
        self.cmp_w2 = di("cmp_w2", [128, 2, 2, 64])
        self.selmask_in = dib("selmask", [128, 3, NT, 32])
        self.expand_in = dib("expand", [32, 16, 128])
        self.wbias_in = dib("wbias", [128, 8, 512])
        self.identb_in = dib("identb", [128, 128])
        self.cmpbias_in = dib("cmpbias", [128, NT, 127])
        self.odd_w = di("odd_w", [N_ODD_BLK, 128, 8, 128])
        self.odd_w_out = di("odd_w_out", [D, D])
        self.pool_w = di("pool_w", [4, 128, 128])
        self.pool_scale = di("pool_scale", [128, 4])
        self.invc_in = di("invc", [128, 4, 16])
        self.chv_in = di("chv", [128, 7, 4])
        self.mu_in = di("mu", [128, 14])
        self.blockones_in = di("blockones", [128, 128])
        self.rmasks_in = di("rmasks", [128, 3, 128])
        self.wa2_in = di("wa2", [128, 512])
        self.g2_in = di("g2", [128, 512])
        self.out = nc.dram_tensor("out", [S, D], F32, kind="ExternalOutput").ap()
        self.modscr = nc.dram_tensor("modscr", [2, 6 * D], F32, kind="Internal").ap()

    def emit(self):
        nc, K, es = self.nc, self.K, self.es
        ARENA_BYTES = 206 * 1024
        arena_t = es.enter_context(nc.sbuf_tensor("arena", [128, ARENA_BYTES // 4], F32))
        self.A = A = Arena(arena_t, ARENA_BYTES)
        for i in range(8):
            pt = es.enter_context(nc.psum_tensor("bank%d" % i, [128, 512], F32))
            K.banks.append((pt, Buf("bank%d" % i, excl=True)))

        self.x_sb = f32(A, NT * D).rearrange("p (t d) -> p t d", t=NT)
        self.xb = [Buf("x%d" % t) for t in range(NT)]
        self.ident = f32(A, 128)
        self.b_ident = Buf("ident")
        self.ones_row = f32(A, 128)
        self.b_ones = Buf("ones")
        self.modrow = f32(A, 0)
        self.bcall = f32(A, 3 * D)
        self.bc = [self.bcall[:, i * D:(i + 1) * D] for i in range(3)]
        self.b_bc = [Buf("bc%d" % i) for i in range(3)]
        self.small = f32(A, 64)
        self.b_small = Buf("small")

        K.dma(self.ident, self.ident_in, w=[self.b_ident])
        K.op("dve", lambda e: e.memset(self.ones_row, 1.0), w=[self.b_ones])
        xin = self.x_in.rearrange("(t p) d -> p t d", p=128)
        for t in range(NT):
            K.dma(self.x_sb[:, t, :], xin[:, t, :], w=[self.xb[t]])

        self.compute_mod()

        for st in self.stages:
            if st == "mix0":
                self.even_mixer()
            elif st == "mix1":
                self.odd_mixer()
            elif st.startswith("moe"):
                self.moe_layer(int(st[3]))
            elif st == "final":
                self.final()
        if "final" not in self.stages:
            oview = self.out.rearrange("(t p) d -> p t d", p=128)
            for t in range(NT):
                K.dma(oview[:, t, :], self.x_sb[:, t, :], r=[self.xb[t]])
        K.finish()

    def compute_mod(self):
        nc, K, A = self.nc, self.K, self.A
        A.mark()
        cT = f32(A, 8)
        b_c = Buf("cT")
        row = f32(A, 6 * D)
        b_row = Buf("row")
        brow = f32(A, 6 * D)
        b_brow = Buf("brow")
        stg = [f32(A, 4096).rearrange("p (k n) -> p k n", k=8) for _ in range(2)]
        b_stg = [Buf("mstg0"), Buf("mstg1")]
        K.dma(cT, self.c_in, w=[b_c])
        K.op("act", lambda e: e.activation(out=cT, in_=cT, func=AF.Silu), r=[b_c], w=[b_c])
        j = 0
        for l in range(2):
            K.dma(brow[0:1, :], self.ada_b[l:l + 1, :], w=[b_brow])
            for nb in range(12):
                s_, bs_ = stg[j % 2], b_stg[j % 2]
                j += 1
                K.dma(s_, self.ada_w[l, nb], w=[bs_])
                pt, pb = K.bank()
                for kc in range(8):
                    K.op("pe", lambda e, kc=kc, s_=s_, pt=pt: e.matmul(
                        pt[0:1, :], lhsT=cT[:, kc:kc + 1], rhs=s_[:, kc, :], start=(kc == 0), stop=(kc == 7)),
                        r=[b_c, bs_], w=[pb])
                K.op("dve", lambda e, pt=pt, nb=nb: e.tensor_tensor(
                    out=row[0:1, nb * 512:(nb + 1) * 512], in0=pt[0:1, :], in1=brow[0:1, nb * 512:(nb + 1) * 512],
                    op=ALU.add), r=[pb, b_brow], w=[b_row])
            K.dma(self.modscr[l:l + 1, :], row[0:1, :], r=[b_row])
        self.b_modscr = Buf("modscr")
        K.barrier()
        A.release()

    def bcast_row(self, dst, b_dst, row_ap, b_row):
        K = self.K
        for h in range(2):
            pt, pb = K.bank()
            K.op("pe", lambda e, pt=pt, h=h: e.matmul(pt[:, :], lhsT=self.ones_row[0:1, :],
                                                       rhs=row_ap[0:1, h * 512:(h + 1) * 512], start=True, stop=True),
                 r=[self.b_ones, b_row], w=[pb])
            K.op("act", lambda e, pt=pt, h=h: e.copy(out=dst[:, h * 512:(h + 1) * 512], in_=pt[:, :]),
                 r=[pb], w=[b_dst])

    def load_mod_bc(self, l, which, norm_w_ap):
        K, A = self.K, self.A
        A.mark()
        rows = f32(A, 4 * D)
        b_rows = Buf("modrows")
        base = which * 3 * D
        K.dma(rows[0:1, 0:3 * D], self.modscr[l:l + 1, base:base + 3 * D], w=[b_rows])
        K.dma(rows[0:1, 3 * D:4 * D], norm_w_ap, w=[b_rows])
        K.op("dve", lambda e: e.scalar_tensor_tensor(out=rows[0:1, D:2 * D], in0=rows[0:1, D:2 * D], scalar=1.0,
                                                     in1=rows[0:1, 3 * D:4 * D], op0=ALU.add, op1=ALU.mult),
             r=[b_rows], w=[b_rows])
        self.bcast_row(self.bc[0], self.b_bc[0], rows[0:1, D:2 * D], b_rows)
        self.bcast_row(self.bc[1], self.b_bc[1], rows[0:1, 0:D], b_rows)
        self.bcast_row(self.bc[2], self.b_bc[2], rows[0:1, 2 * D:3 * D], b_rows)
        K.barrier()
        A.release()

    def norm_tile(self, t, htmp, b_htmp, scr, b_scr, stat, b_stat):
        K = self.K
        xt = self.x_sb[:, t, :]
        K.op("act", lambda e: e.activation(out=scr, in_=xt, func=AF.Square, accum_out=stat[:, 0:1]),
             r=[self.xb[t]], w=[b_scr, b_stat])
        cut = os.environ.get("KCUT", "z")
        if cut == "a": return
        K.op("act", lambda e: e.activation(out=stat[:, 1:2], in_=stat[:, 0:1], func=AF.Sqrt, scale=1.0 / D, bias=self.epsb),
             r=[b_stat, self.b_small], w=[b_stat])
        if cut == "b": return
        K.op("dve", lambda e: e.reciprocal(out=stat[:, 2:3], in_=stat[:, 1:2]), r=[b_stat], w=[b_stat])
        if cut == "c": return
        K.op("dve", lambda e: e.scalar_tensor_tensor(out=htmp, in0=xt, scalar=stat[:, 2:3], in1=self.bc[0],
                                                     op0=ALU.mult, op1=ALU.mult),
             r=[self.xb[t], b_stat, self.b_bc[0]], w=[b_htmp])
        if cut == "d": return
        K.op("dve", lambda e: e.tensor_tensor(out=htmp, in0=htmp, in1=self.bc[1], op=ALU.add),
             r=[b_htmp, self.b_bc[1]], w=[b_htmp])

    def make_eps(self):
        K = self.K
        self.epsb = self.small[:, 0:1]
        K.op("dve", lambda e: e.memset(self.epsb, EPS), w=[self.b_small])

    def moe_layer(self, l):
        nc, K, A = self.nc, self.K, self.A
        if not hasattr(self, "epsb"):
            self.make_eps()
        self.load_mod_bc(l, 1, self.norm_ffn[l:l + 1, :])
        if os.environ.get("KDBG") == "modbc":
            for t in range(3):
                K.op("dve", lambda e, t=t: e.tensor_copy(out=self.x_sb[:, t, :], in_=self.bc[t]), r=[self.b_bc[t]], w=[self.xb[t]])
            return
        A.mark()
        hT = bf(A, 8 * S).rearrange("p (k s) -> p k s", k=8)
        b_hT = [Buf("hT%d" % t) for t in range(NT)]
        logits = f32(A, NT * 16).rearrange("p (t e) -> p t e", t=NT)
        b_log = Buf("logits")
        gates = f32(A, NT * 16).rearrange("p (t e) -> p t e", t=NT)
        b_gates = Buf("gates")
        rw = f32(A, 8 * 16).rearrange("p (k e) -> p k e", k=8)
        b_rw = Buf("rw")
        rb = f32(A, 16)
        b_rb = Buf("rb")
        K.dma(rw, self.router_w, w=[b_rw])
        rbrow = f32(A, 16)
        b_rbrow = Buf("rbrow")
        K.dma(rbrow[0:1, :], self.router_b, w=[b_rbrow])
        pt, pb = K.bank()
        K.op("pe", lambda e: e.matmul(pt[:, 0:16], lhsT=self.ones_row[0:1, :], rhs=rbrow[0:1, :], start=True, stop=True),
             r=[self.b_ones, b_rbrow], w=[pb])
        K.op("act", lambda e: e.copy(out=rb, in_=pt[:, 0:16]), r=[pb], w=[b_rb])

        A.mark()
        htmp = [f32(A, D) for _ in range(2)]
        b_htmp = [Buf("htmp0"), Buf("htmp1")]
        scr = f32(A, D)
        b_scr = Buf("scr")
        stats = [f32(A, 4) for _ in range(2)]
        b_stats = [Buf("st0"), Buf("st1")]
        hTf = [f32(A, 8 * 128).rearrange("p (k s) -> p k s", k=8) for _ in range(2)]
        b_hTf = [Buf("hTf0"), Buf("hTf1")]
        for t in range(NT):
            i = t % 2
            self.norm_tile(t, htmp[i], b_htmp[i], scr, b_scr, stats[i], b_stats[i])
            if os.environ.get("KDBG") == "n1":
                K.op("dve", lambda e, t=t, i=i: e.tensor_copy(out=self.x_sb[:, t, :], in_=htmp[i]), r=[b_htmp[i]], w=[self.xb[t]])
                continue
            for hf in range(2):
                pt, pb = K.bank()
                for kk in range(4):
                    kc = hf * 4 + kk
                    K.op("pe", lambda e, pt=pt, kk=kk, kc=kc, i=i: e.transpose(
                        out=pt[:, kk * 128:(kk + 1) * 128], in_=htmp[i][:, kc * 128:(kc + 1) * 128], identity=self.ident),
                        r=[b_htmp[i], self.b_ident], w=[pb])
                K.op("act", lambda e, pt=pt, hf=hf, i=i: e.copy(
                    out=hTf[i][:, hf * 4:(hf + 1) * 4, :], in_=pt[:, :].rearrange("p (k s) -> p k s", k=4)),
                    r=[pb], w=[b_hTf[i]])
                K.op("dve", lambda e, hf=hf, t=t, i=i: e.tensor_copy(
                    out=hT[:, hf * 4:(hf + 1) * 4, t * 128:(t + 1) * 128], in_=hTf[i][:, hf * 4:(hf + 1) * 4, :]),
                    r=[b_hTf[i]], w=[b_hT[t]])
            if os.environ.get("KCUT") == "t":
                continue
            pt, pb = K.bank()
            for kc in range(8):
                K.op("pe", lambda e, pt=pt, kc=kc, i=i: e.matmul(pt[:, 0:16], lhsT=hTf[i][:, kc, :], rhs=rw[:, kc, :],
                                                               start=(kc == 0), stop=(kc == 7)),
                     r=[b_hTf[i], b_rw], w=[pb])
            K.op("act", lambda e, pt=pt, t=t: e.activation(out=logits[:, t, :], in_=pt[:, 0:16], func=AF.Sigmoid),
                 r=[pb], w=[b_log])
        K.barrier()
        A.release()
        if os.environ.get("KDBG") in ("norm", "n1"):
            A.release(); return

        A.mark()
        self.routing(logits, b_log, rb, b_rb, gates, b_gates)
        A.release()
        if os.environ.get("KDBG") == "route":
            for t in range(NT):
                K.op("dve", lambda e, t=t: e.tensor_copy(out=self.x_sb[:, t, 0:16], in_=gates[:, t, :]), r=[b_gates], w=[self.xb[t]])
                K.op("dve", lambda e, t=t: e.tensor_copy(out=self.x_sb[:, t, 16:32], in_=logits[:, t, :]), r=[b_log], w=[self.xb[t]])
            K.barrier(); A.release(); return
        NEXP = int(os.environ.get("KNEXP", "16"))

        A.mark()
        NW = 4
        wbf = [bf(A, 4096) for _ in range(NW)]
        b_wbf = [Buf("wbf%d" % i) for i in range(NW)]
        stg = [f32(A, 4096) for _ in range(2)]
        b_stg = [Buf("stg0"), Buf("stg1")]
        heT = bf(A, 4 * S).rearrange("p (c s) -> p c s", c=4)
        b_heT = [[Buf("heT%d_%d" % (c, g)) for g in range(4)] for c in range(4)]
        sil = [f32(A, 512) for _ in range(2)]
        b_sil = [Buf("sil0"), Buf("sil1")]
        g2bc = self.bc[2]
        st = {"w": 0, "s": 0, "sil": 0}

        def load_w(kind, e):
            wi = st["w"] % NW
            st["w"] += 1
            si = st["s"] % 2
            st["s"] += 1
            if kind == 2:
                src = self.moe_wd[l, e].rearrange("(c p) n -> p c n", p=128)
                sv = stg[si].rearrange("p (c n) -> p c n", c=4)
                K.dma(sv, src, w=[b_stg[si]])
                wv = wbf[wi].rearrange("p (c n) -> p c n", c=4)
                for c in range(4):
                    K.op("pool", lambda e_, c=c: e_.tensor_tensor(out=wv[:, c, :], in0=sv[:, c, :], in1=g2bc, op=ALU.mult),
                         r=[b_stg[si], self.b_bc[2]], w=[b_wbf[wi]])
                return wv, b_wbf[wi]
            else:
                src = (self.moe_wg if kind == 0 else self.moe_wu)[l, e].rearrange("(k p) n -> p k n", p=128)
                sv = stg[si].rearrange("p (k n) -> p k n", k=8)
                K.dma(sv, src, w=[b_stg[si]])
                wv = wbf[wi].rearrange("p (k n) -> p k n", k=8)
                K.op("pool", lambda e_: e_.tensor_copy(out=wbf[wi], in_=stg[si]), r=[b_stg[si]], w=[b_wbf[wi]])
                return wv, b_wbf[wi]

        nxt = [load_w(0, 0), load_w(1, 0), load_w(2, 0)]
        for e in range(NEXP):
            (wg, b_wg), (wu, b_wu), (wd, b_wd) = nxt
            for c in range(4):
                for g in range(4):
                    pg, pgb = K.bank()
                    for kc in range(8):
                        K.op("pe", lambda e_, pg=pg, kc=kc, c=c, g=g: e_.matmul(
                            pg[:, :], lhsT=wg[:, kc, c * 128:(c + 1) * 128], rhs=hT[:, kc, g * 512:(g + 1) * 512],
                            start=(kc == 0), stop=(kc == 7)), r=[b_wg] + b_hT[4 * g:4 * g + 4], w=[pgb])
                    pu, pub = K.bank()
                    for kc in range(8):
                        K.op("pe", lambda e_, pu=pu, kc=kc, c=c, g=g: e_.matmul(
                            pu[:, :], lhsT=wu[:, kc, c * 128:(c + 1) * 128], rhs=hT[:, kc, g * 512:(g + 1) * 512],
                            start=(kc == 0), stop=(kc == 7)), r=[b_wu] + b_hT[4 * g:4 * g + 4], w=[pub])
                    si = st["sil"] % 2
                    st["sil"] += 1
                    K.op("act", lambda e_, pg=pg, si=si: e_.activation(out=sil[si], in_=pg[:, :], func=AF.Silu),
                         r=[pgb], w=[b_sil[si]])
                    K.op("dve", lambda e_, pu=pu, si=si, c=c, g=g: e_.tensor_tensor(
                        out=heT[:, c, g * 512:(g + 1) * 512], in0=sil[si], in1=pu[:, :], op=ALU.mult),
                        r=[b_sil[si], pub], w=[b_heT[c][g]])
            if e + 1 < NEXP:
                nxt = [load_w(0, e + 1), load_w(1, e + 1)]
            for t in range(NT):
                for hf in range(2):
                    pd, pdb = K.bank()
                    for c in range(4):
                        K.op("pe", lambda e_, pd=pd, c=c, t=t, hf=hf: e_.matmul(
                            pd[:, :], lhsT=heT[:, c, t * 128:(t + 1) * 128], rhs=wd[:, c, hf * 512:(hf + 1) * 512],
                            start=(c == 0), stop=(c == 3)), r=[b_wd, b_heT[c][t // 4]], w=[pdb])
                    xs = self.x_sb[:, t, hf * 512:(hf + 1) * 512]
                    K.op("dve", lambda e_, pd=pd, xs=xs, t=t, e=e: e_.scalar_tensor_tensor(
                        out=xs, in0=pd[:, :], scalar=gates[:, t, e:e + 1], in1=xs, op0=ALU.mult, op1=ALU.add),
                        r=[pdb, b_gates, self.xb[t]], w=[self.xb[t]])
            if e + 1 < NEXP:
                nxt.append(load_w(2, e + 1))
        K.barrier()
        A.release()
        A.release()

    def routing(self, scores, b_sc, rb, b_rb, gates, b_gates):
        K, A = self.K, self.A
        BIGR = 1000.0
        n = NT * 16
        biased = f32(A, n)
        t1 = f32(A, n)
        t2 = f32(A, n)
        m1 = f32(A, NT * 4)
        m2 = f32(A, NT * 4)
        gs = f32(A, NT * 4)
        gm = f32(A, NT)
        ing = f32(A, NT * 4)
        b_r = Buf("routing")
        sc2 = scores.rearrange("p t e -> p (t e)")
        v3 = lambda a: a.rearrange("p (t e) -> p t e", e=16)
        g4 = lambda a: a.rearrange("p (g e) -> p g e", e=4)
        bc4 = lambda a: a.unsqueeze(2).to_broadcast([128, NT * 4, 4])
        R = [b_r, b_sc, b_rb]
        W = [b_r]
        dve = lambda fn, r=R, w=W: K.op("dve", fn, r=r, w=w)
        dve(lambda e: e.tensor_tensor(out=v3(biased), in0=scores, in1=rb.unsqueeze(1).to_broadcast([128, NT, 16]), op=ALU.add))
        dve(lambda e: e.tensor_reduce(out=m1, in_=g4(biased), axis=AX.X, op=ALU.max))
        dve(lambda e: e.tensor_tensor(out=g4(t1), in0=g4(biased), in1=bc4(m1), op=ALU.is_equal))
        dve(lambda e: e.scalar_tensor_tensor(out=t1, in0=t1, scalar=-BIGR, in1=biased, op0=ALU.mult, op1=ALU.add))
        dve(lambda e: e.tensor_reduce(out=m2, in_=g4(t1), axis=AX.X, op=ALU.max))
        dve(lambda e: e.tensor_tensor(out=gs, in0=m1, in1=m2, op=ALU.add))
        dve(lambda e: e.tensor_reduce(out=gm, in_=gs.rearrange("p (t g) -> p t g", g=4), axis=AX.X, op=ALU.max))
        dve(lambda e: e.tensor_tensor(out=ing.rearrange("p (t g) -> p t g", g=4), in0=gs.rearrange("p (t g) -> p t g", g=4),
                                      in1=gm.unsqueeze(2).to_broadcast([128, NT, 4]), op=ALU.is_equal))
        dve(lambda e: e.tensor_scalar(out=ing, in0=ing, scalar1=-1.0, scalar2=BIGR, op0=ALU.add, op1=ALU.mult))
        dve(lambda e: e.tensor_tensor(out=g4(t1), in0=g4(biased), in1=bc4(ing), op=ALU.add))
        dve(lambda e: e.tensor_reduce(out=gm, in_=v3(t1), axis=AX.X, op=ALU.max))
        dve(lambda e: e.tensor_tensor(out=v3(t2), in0=v3(t1), in1=gm.unsqueeze(2).to_broadcast([128, NT, 16]), op=ALU.is_equal))
        dve(lambda e: e.scalar_tensor_tensor(out=t2, in0=t2, scalar=-BIGR, in1=t1, op0=ALU.mult, op1=ALU.add))
        dve(lambda e: e.tensor_reduce(out=gm, in_=v3(t2), axis=AX.X, op=ALU.max))
        dve(lambda e: e.tensor_tensor(out=v3(t2), in0=v3(t1), in1=gm.unsqueeze(2).to_broadcast([128, NT, 16]), op=ALU.is_ge))
        dve(lambda e: e.tensor_tensor(out=t2, in0=t2, in1=sc2, op=ALU.mult))
        dve(lambda e: e.tensor_reduce(out=gm, in_=v3(t2), axis=AX.X, op=ALU.add))
        dve(lambda e: e.reciprocal(out=gm, in_=gm))
        K.op("dve", lambda e: e.tensor_tensor(out=gates, in0=v3(t2), in1=gm.unsqueeze(2).to_broadcast([128, NT, 16]), op=ALU.mult),
             r=R, w=[b_gates])
        K.barrier()

    def final(self):
        K, A = self.K, self.A
        if not hasattr(self, "epsb"):
            self.make_eps()
        A.mark()
        frow = f32(A, D)
        b_frow = Buf("frow")
        K.dma(frow[0:1, :], self.final_norm, w=[b_frow])
        self.bcast_row(self.bc[0], self.b_bc[0], frow[0:1, :], b_frow)
        scr = f32(A, D)
        b_scr = Buf("scrf")
        o = [f32(A, D) for _ in range(2)]
        b_o = [Buf("o0"), Buf("o1")]
        stats = [f32(A, 4) for _ in range(2)]
        b_stats = [Buf("fst0"), Buf("fst1")]
        oview = self.out.rearrange("(t p) d -> p t d", p=128)
        for t in range(NT):
            i = t % 2
            xt = self.x_sb[:, t, :]
            stat, b_stat = stats[i], b_stats[i]
            K.op("act", lambda e: e.activation(out=scr, in_=xt, func=AF.Square, accum_out=stat[:, 0:1]),
                 r=[self.xb[t]], w=[b_scr, b_stat])
            K.op("act", lambda e: e.activation(out=stat[:, 1:2], in_=stat[:, 0:1], func=AF.Sqrt, scale=1.0 / D, bias=self.epsb),
                 r=[b_stat, self.b_small], w=[b_stat])
            K.op("dve", lambda e: e.reciprocal(out=stat[:, 2:3], in_=stat[:, 1:2]), r=[b_stat], w=[b_stat])
            K.op("dve", lambda e: e.scalar_tensor_tensor(out=o[i], in0=xt, scalar=stat[:, 2:3], in1=self.bc[0],
                                                         op0=ALU.mult, op1=ALU.mult),
                 r=[self.xb[t], b_stat, self.b_bc[0]], w=[b_o[i]])
            K.dma(oview[:, t, :], o[i], r=[b_o[i]])
        A.release()


def _prep_common(inp):
    f = lambda a: np.ascontiguousarray(np.asarray(a, dtype=np.float32))
    com = {}
    aw = f(inp["ada_w"])
    com["ada_w"] = f(aw.reshape(2, 8, 128, 12, 512).transpose(0, 3, 2, 1, 4))
    com["ada_b"] = f(inp["ada_b"])
    com["norm_mix"] = f(inp["norm_mix"])
    com["norm_ffn"] = f(inp["norm_ffn"])
    com["final_norm"] = f(inp["final_norm"]).reshape(1, D)
    com["router_w"] = f(f(inp["router_w"]).reshape(8, 128, 16).transpose(1, 0, 2))
    com["router_b"] = f(inp["router_b"]).reshape(1, 16)
    com["moe_w_gate"] = f(inp["moe_w_gate"])
    com["moe_w_up"] = f(inp["moe_w_up"])
    com["moe_w_down"] = f(inp["moe_w_down"])
    com["ident"] = np.eye(128, dtype=np.float32)
    com["even_w"] = _even_w_blocks(f(inp["even_w_in"])[0])
    com["even_w_out"] = f(inp["even_w_out"])[0]
    com["conv_w"] = f(f(inp["even_conv_w"])[0].reshape(3, 4, 128).transpose(2, 1, 0))
    pos = f(inp["even_cmp_pos"])[0]
    com["cmp_pos"] = f(np.tile(pos.transpose(2, 0, 1), (2, 1, 1)))
    w1 = f(inp["even_cmp_w1"])[0]
    w1 = w1.reshape(2, 32, 64, 256).transpose(0, 2, 1, 3)
    w1 = np.tile(w1, (1, 2, 1, 1)).reshape(2, 128, 4, 8, 256).transpose(0, 2, 1, 3, 4)
    com["cmp_w1"] = f(w1)
    w2 = f(inp["even_cmp_w2"])[0]
    com["cmp_w2"] = f(w2.reshape(2, 2, 128, 64).transpose(2, 0, 1, 3))
    com.update(_even_consts())
    com["odd_w"] = _odd_w_blocks(f(inp["odd_w_in"])[0])
    com["odd_w_out"] = f(inp["odd_w_out"])[0]
    com["pool_w"] = f(inp["odd_pool_w"])[0]
    pk = lambda a: f(f(a).reshape(-1)[:512].reshape(4, 128).T)
    com["pool_scale"] = pk(f(inp["odd_pool_scale"])[0])
    com["chv"] = f(np.stack([pk(f(inp[k])[0]) for k in ("odd_w0", "odd_a0", "odd_k_k", "odd_k_a", "odd_lnx_w", "odd_lnx_b", "odd_r_k")], axis=1))
    com["mu"] = f(f(inp["odd_mu"])[0].reshape(14, 128).T)
    com["wa2"] = f(np.concatenate([f(inp["odd_w2"])[0], f(inp["odd_a2"])[0]], axis=0))
    com["g2"] = f(inp["odd_g2"])[0]
    com.update(_odd_consts())
    return com


_CACHE = {}


def run(inputs, stages=("mix0", "moe0", "mix1", "moe1", "final"), cores=None, x_override=None):
    cores = list(range(8)) if cores is None else cores
    key = tuple(stages)
    if key not in _CACHE:
        _CACHE[key] = build_program(stages)
    nc = _CACHE[key]
    com = _prep_common(inputs)
    x = np.asarray(inputs["x"], dtype=np.float32) if x_override is None else x_override
    c = np.asarray(inputs["c"], dtype=np.float32)
    in_maps = []
    for b in cores:
        m = dict(com)
        m["x"] = np.ascontiguousarray(x[b])
        m["c"] = np.ascontiguousarray(c[b].reshape(8, 128).T)
        in_maps.append(m)
    res = run_bass_kernel_spmd(nc, in_maps, core_ids=list(range(len(cores))))
    return np.stack([r["out"] for r in res.results], axis=0)


def kernel(**inputs):
    return run(inputs).astype(np.float32)


ATTN_SCALE = 0.125
N_EVEN_BLK = 29


def _even_w_blocks(w_in):
    cols = []
    hd = lambda base, h: list(range(base + h * 64, base + (h + 1) * 64))
    sw = lambda c: c[32:] + c[:32]
    for g in range(4):
        cols.append(hd(0, g) + hd(0, g + 4))
    for g in range(4):
        cols.append(sw(hd(0, g)) + sw(hd(0, g + 4)))
    cols.append(list(range(512, 640)))
    cols.append(list(range(640, 768)))
    cols.append(list(range(768, 896)))
    cols.append(sw(hd(768, 0)) + sw(hd(768, 1)))
    cols.append(list(range(1024, 1152)))
    cols.append(sw(hd(1024, 0)) + sw(hd(1024, 1)))
    cols.append(list(range(896, 1024)))
    cols.append(list(range(1152, 1280)))
    cols.append(list(range(1280, 1304)) + [-1] * 104)
    for base in (1304, 1816, 2328):
        for c in range(4):
            cols.append(list(range(base + c * 128, base + (c + 1) * 128)))
    out = np.zeros((len(cols), 128, 8, 128), np.float32)
    for i, cl in enumerate(cols):
        idx = np.array(cl)
        blk = np.where(idx[None, :] >= 0, w_in[:, np.maximum(idx, 0)], 0.0)
        out[i] = blk.reshape(8, 128, 128).transpose(1, 0, 2)
    return out


def _even_consts():
    import ml_dtypes
    bf16 = ml_dtypes.bfloat16
    c = {}
    half = 32
    inv = 10000.0 ** (-np.arange(half, dtype=np.float32) / half)
    ang = np.arange(S, dtype=np.float32)[:, None] * inv[None, :]
    cos = np.cos(ang).T.astype(np.float32)
    sin = np.sin(ang).T.astype(np.float32)
    c["rope_cos"] = np.ascontiguousarray(np.tile(cos, (4, 1)))
    c["rope_sin"] = np.ascontiguousarray(np.tile(np.concatenate([-sin, sin], 0), (2, 1)))
    t = np.arange(S)
    cm = (np.arange(127) * 16 + 31)[None, :] <= t[:, None]
    c["cmpbias"] = np.ascontiguousarray(np.where(cm, 0.0, NEGB).astype(np.float32).reshape(NT, 128, 127).transpose(1, 0, 2)).astype(bf16)
    j = np.arange(32)[None, :]
    cur = t[:, None] // 64
    valid = j * 64 <= t[:, None]
    forced = (j == 0) | ((cur - j >= 0) & (cur - j < 2))
    vm = (valid & ~forced).astype(np.float32)
    am = np.where(forced, 1e9, np.where(valid, 0.0, -1e9)).astype(np.float32)
    ok = valid.astype(np.float32)
    lay = lambda a: np.ascontiguousarray(a.reshape(NT, 128, 32).transpose(1, 0, 2))
    c["selmask"] = np.ascontiguousarray(np.stack([lay(vm), lay(am), lay(ok)], axis=1)).astype(bf16)
    ex = np.zeros((32, 16, 128), np.float32)
    for kt in range(16):
        for p in range(128):
            ex[2 * kt + p // 64, kt, p] = 1.0
    c["expand"] = ex.astype(bf16)
    wb = np.zeros((128, 8, 512), np.float32)
    for jj in range(8):
        s_pos = (jj - 4) * 128 + np.arange(128)[:, None]
        tq = np.arange(512)[None, :]
        diff = tq - s_pos
        wb[:, jj, :] = np.where((diff >= 0) & (diff < 512), 0.0, NEGB)
    c["wbias"] = wb.astype(bf16)
    c["identb"] = np.eye(128, dtype=np.float32).astype(bf16)
    return c


class _EvenMixin:
    def norm_transpose(self, hT, b_hT):
        K, A = self.K, self.A
        if not hasattr(self, "epsb"):
            self.make_eps()
        A.mark()
        htmp = [f32(A, D) for _ in range(2)]
        b_htmp = [Buf("mhtmp0"), Buf("mhtmp1")]
        scr = f32(A, D)
        b_scr = Buf("mscr")
        stats = [f32(A, 4) for _ in range(2)]
        b_stats = [Buf("mst0"), Buf("mst1")]
        for t in range(NT):
            i = t % 2
            self.norm_tile(t, htmp[i], b_htmp[i], scr, b_scr, stats[i], b_stats[i])
            for hf in range(2):
                pt, pb = K.bank()
                for kk in range(4):
                    kc = hf * 4 + kk
                    K.op("pe", lambda e, pt=pt, kk=kk, kc=kc, i=i: e.transpose(
                        out=pt[:, kk * 128:(kk + 1) * 128], in_=htmp[i][:, kc * 128:(kc + 1) * 128], identity=self.ident),
                        r=[b_htmp[i], self.b_ident], w=[pb])
                K.op("act", lambda e, pt=pt, hf=hf, t=t: e.copy(
                    out=hT[:, hf * 4:(hf + 1) * 4, t * 128:(t + 1) * 128], in_=pt[:, :].rearrange("p (k s) -> p k s", k=4)),
                    r=[pb], w=[b_hT[t]])
        K.barrier()
        A.release()

    def make_wloader(self, nslots=3, nstage=2):
        K, A = self.K, self.A
        stg = [f32(A, 1024) for _ in range(nstage)]
        b_stg = [Buf("wstg%d" % i) for i in range(nstage)]
        wb = [bf(A, 1024) for _ in range(nslots)]
        b_wb = [Buf("wblk%d" % i) for i in range(nslots)]
        st = {"s": 0, "w": 0}

        def load(src, mul=None, b_mul=None):
            si = st["s"] % nstage
            st["s"] += 1
            wi = st["w"] % nslots
            st["w"] += 1
            K.dma(stg[si], src, w=[b_stg[si]])
            if mul is None:
                K.op("pool", lambda e: e.tensor_copy(out=wb[wi], in_=stg[si]), r=[b_stg[si]], w=[b_wb[wi]])
            else:
                K.op("pool", lambda e: e.tensor_tensor(out=wb[wi], in0=stg[si], in1=mul, op=ALU.mult),
                     r=[b_stg[si], b_mul], w=[b_wb[wi]])
            return wb[wi], b_wb[wi]
        return load

    def proj_fm(self, wv, b_w, hT, b_hT, G):
        K = self.K
        pt, pb = K.bank()
        w3 = wv.rearrange("p (k n) -> p k n", k=8)
        for kc in range(8):
            K.op("pe", lambda e, kc=kc: e.matmul(pt[:, :], lhsT=w3[:, kc, :], rhs=hT[:, kc, G * 512:(G + 1) * 512],
                                                 start=(kc == 0), stop=(kc == 7)),
                 r=[b_w] + b_hT[4 * G:4 * G + 4], w=[pb])
        return pt, pb

    def proj_tm(self, wv, b_w, hT, b_hT, t):
        K = self.K
        pt, pb = K.bank()
        w3 = wv.rearrange("p (k n) -> p k n", k=8)
        for kc in range(8):
            K.op("pe", lambda e, kc=kc: e.matmul(pt[:, 0:128], lhsT=hT[:, kc, t * 128:(t + 1) * 128], rhs=w3[:, kc, :],
                                                 start=(kc == 0), stop=(kc == 7)),
                 r=[b_w, b_hT[t]], w=[pb])
        return pt, pb

    def even_mixer(self):
        nc, K, A = self.nc, self.K, self.A
        l = 0
        self.load_mod_bc(l, 0, self.norm_mix[l:l + 1, :])
        g1bc, b_g1 = self.bc[2], self.b_bc[2]
        HTB = 8 * S * 2
        hT = A.alloc_top(HTB, BF16).rearrange("p (k s) -> p k s", k=8)
        b_hT = [Buf("mhT%d" % t) for t in range(NT)]
        self.norm_transpose(hT, b_hT)
        wsrc = lambda i: self.even_w[i].rearrange("p k n -> p (k n)")

        A.mark()
        load = self.make_wloader(3)
        u = f32(A, S + 2)
        b_u = Buf("u")
        bgs = f32(A, S)
        b_bgs = Buf("bgs")
        acc = f32(A, S)
        b_acc = Buf("acc")
        yc4 = bf(A, 4 * S).rearrange("p (c s) -> p c s", c=4)
        b_yc = Buf("yc")
        cwo4 = bf(A, 4 * 1024).rearrange("p (c n) -> p c n", c=4)
        b_cwo4 = Buf("cwo4")
        cstage = f32(A, 1024)
        b_cstage = Buf("cstage")
        tmp = [f32(A, 512) for _ in range(2)]
        b_tmp = [Buf("ctmp0"), Buf("ctmp1")]
        cw = f32(A, 12).rearrange("p (c k) -> p c k", c=4)
        b_cw = Buf("cw")
        K.dma(cw, self.conv_w, w=[b_cw])
        K.op("dve", lambda e: e.memset(u[:, 0:2], 0.0), w=[b_u])
        ti = 0
        for c in range(4):
            wx, b_wx = load(wsrc(17 + c))
            wc, b_wc = load(wsrc(25 + c))
            for G in range(4):
                px, pxb = self.proj_fm(wx, b_wx, hT, b_hT, G)
                pc, pcb = self.proj_fm(wc, b_wc, hT, b_hT, G)
                tt, b_tt = tmp[ti % 2], b_tmp[ti % 2]
                ti += 1
                K.op("act", lambda e: e.copy(out=tt, in_=px[:, :]), r=[pxb], w=[b_tt])
                K.op("dve", lambda e: e.tensor_tensor(out=u[:, 2 + G * 512:2 + (G + 1) * 512], in0=tt, in1=pc[:, :], op=ALU.mult),
                     r=[b_tt, pcb], w=[b_u])
            wb_, b_wb_ = load(wsrc(21 + c))
            for G in range(4):
                pb_, pbb = self.proj_fm(wb_, b_wb_, hT, b_hT, G)
                K.op("act", lambda e: e.copy(out=bgs[:, G * 512:(G + 1) * 512], in_=pb_[:, :]), r=[pbb], w=[b_bgs])
            K.op("dve", lambda e: e.tensor_scalar(out=acc, in0=u[:, 2:2 + S], scalar1=cw[:, c, 2:3], scalar2=None, op0=ALU.mult),
                 r=[b_u, b_cw], w=[b_acc])
            K.op("dve", lambda e: e.scalar_tensor_tensor(out=acc, in0=u[:, 1:1 + S], scalar=cw[:, c, 1:2], in1=acc,
                                                         op0=ALU.mult, op1=ALU.add), r=[b_u, b_cw, b_acc], w=[b_acc])
            K.op("dve", lambda e: e.scalar_tensor_tensor(out=acc, in0=u[:, 0:S], scalar=cw[:, c, 0:1], in1=acc,
                                                         op0=ALU.mult, op1=ALU.add), r=[b_u, b_cw, b_acc], w=[b_acc])
            K.op("dve", lambda e: e.tensor_tensor(out=yc4[:, c, :], in0=acc, in1=bgs, op=ALU.mult), r=[b_acc, b_bgs], w=[b_yc])
        self.add_wout_multi(yc4, b_yc, self.even_w_out, 512, 4, cstage, b_cstage, cwo4, b_cwo4)
        K.barrier()
        A.release()
        if os.environ.get("KDBG") == "conv":
            A.release_top(HTB)
            return

        A.mark()
        qn = bf(A, 4 * S).rearrange("p (g s) -> p g s", g=4)
        qr = bf(A, 4 * S).rearrange("p (g s) -> p g s", g=4)
        b_qn = [[Buf("qn%d_%d" % (g, G)) for G in range(4)] for g in range(4)]
        b_qr = [[Buf("qr%d_%d" % (g, G)) for G in range(4)] for g in range(4)]
        kcvc = bf(A, 2 * S)
        kcT, vcT = kcvc[:, 0:S], kcvc[:, S:2 * S]
        b_kcT, b_vcT = Buf("kcT"), Buf("vcT")
        ksP = [bf(A, S) for _ in range(2)]
        kwP = [bf(A, S) for _ in range(2)]
        b_ksT, b_kwT = Buf("ksP"), Buf("kwP")
        Vs = bf(A, NT * 2 * 65).rearrange("p (t h d) -> p t h d", t=NT, h=2)
        Vw = bf(A, NT * 2 * 65).rearrange("p (t h d) -> p t h d", t=NT, h=2)
        b_Vs, b_Vw = Buf("Vs"), Buf("Vw")
        sg = f32(A, NT * 24).rearrange("p (t c) -> p t c", t=NT)
        b_sg = Buf("sg")
        A.mark()
        load = self.make_wloader(3, 1)
        cosT = f32(A, S)
        sinT = f32(A, S)
        b_rope = Buf("rope")
        K.dma(cosT, self.rope_cos, w=[b_rope])
        K.dma(sinT, self.rope_sin, w=[b_rope])
        tmp = [f32(A, 512) for _ in range(2)]
        b_tmp = [Buf("ptmp%d" % i) for i in range(2)]
        ti = 0
        for kp_, bkp_ in ((ksP, b_ksT), (kwP, b_kwT)):
            K.op("dve", lambda e: e.memset(kp_[0][64:128, :], 0.0), w=[bkp_])
            K.op("dve", lambda e: e.memset(kp_[1][0:64, :], 0.0), w=[bkp_])
        K.op("dve", lambda e: e.memset(Vs[:, :, :, 64:65], 1.0), w=[b_Vs])
        K.op("dve", lambda e: e.memset(Vw[:, :, :, 64:65], 1.0), w=[b_Vw])

        def rope_pair(ia, ib, dst_fn, bdst_fn, nope_fn=None):
            nonlocal ti
            wa, b_wa = load(wsrc(ia))
            wb2, b_wb2 = load(wsrc(ib))
            for G in range(4):
                pa, pab = self.proj_fm(wa, b_wa, hT, b_hT, G)
                ps_, psb = self.proj_fm(wb2, b_wb2, hT, b_hT, G)
                t1, b_t1 = tmp[0], b_tmp[0]
                t2, b_t2 = tmp[1], b_tmp[1]
                sl = slice(G * 512, (G + 1) * 512)
                K.op("dve", lambda e: e.tensor_tensor(out=t1, in0=pa[:, :], in1=cosT[:, sl], op=ALU.mult), r=[pab, b_rope], w=[b_t1])
                if nope_fn is not None:
                    dn, bdn = nope_fn(G)
                    K.op("act", lambda e: e.copy(out=dn, in_=pa[:, :]), r=[pab], w=[bdn])
                K.op("dve", lambda e: e.tensor_tensor(out=t2, in0=ps_[:, :], in1=sinT[:, sl], op=ALU.mult), r=[psb, b_rope], w=[b_t2])
                d_ = dst_fn(G)
                if isinstance(d_, tuple):
                    for hh in range(2):
                        K.op("pool", lambda e: e.tensor_tensor(out=d_[hh][64 * hh:64 * hh + 64], in0=t1[64 * hh:64 * hh + 64], in1=t2[64 * hh:64 * hh + 64], op=ALU.add),
                             r=[b_t1, b_t2], w=[bdst_fn(G)])
                else:
                    K.op("pool", lambda e: e.tensor_tensor(out=d_, in0=t1, in1=t2, op=ALU.add), r=[b_t1, b_t2], w=[bdst_fn(G)])

        for g in range(4):
            rope_pair(g, 4 + g, lambda G, g=g: qr[:, g, G * 512:(G + 1) * 512], lambda G, g=g: b_qr[g][G],
                      nope_fn=lambda G, g=g: (qn[:, g, G * 512:(G + 1) * 512], b_qn[g][G]))
        rope_pair(10, 11, lambda G: (ksP[0][:, G * 512:(G + 1) * 512], ksP[1][:, G * 512:(G + 1) * 512]), lambda G: b_ksT)
        rope_pair(12, 13, lambda G: (kwP[0][:, G * 512:(G + 1) * 512], kwP[1][:, G * 512:(G + 1) * 512]), lambda G: b_kwT)
        for ib, dst, bd in ((8, kcT, b_kcT), (9, vcT, b_vcT)):
            w_, b_w_ = load(wsrc(ib))
            for G in range(4):
                p_, pb_ = self.proj_fm(w_, b_w_, hT, b_hT, G)
                K.op("act", lambda e: e.copy(out=dst[:, G * 512:(G + 1) * 512], in_=p_[:, :]), r=[pb_], w=[bd])
        for ib, dst, bd in ((14, Vs, b_Vs), (15, Vw, b_Vw)):
            w_, b_w_ = load(wsrc(ib))
            for t in range(NT):
                p_, pb_ = self.proj_tm(w_, b_w_, hT, b_hT, t)
                K.op("act", lambda e: e.copy(out=dst[:, t, :, 0:64], in_=p_[:, 0:128].rearrange("p (h d) -> p h d", h=2)),
                     r=[pb_], w=[bd])
        w_, b_w_ = load(wsrc(16))
        for t in range(NT):
            p_, pb_ = self.proj_tm(w_, b_w_, hT, b_hT, t)
            K.op("act", lambda e: e.activation(out=sg[:, t, :], in_=p_[:, 0:24], func=AF.Sigmoid), r=[pb_], w=[b_sg])
        K.barrier()
        A.release()
        A.release_top(HTB)

        kcmpT = bf(A, 128)
        b_kcmpT = Buf("kcmpT")
        vcmp = bf(A, 2 * 64).rearrange("p (h d) -> p h d", h=2)
        b_vcmp = Buf("vcmp")
        A.mark()
        Bl = bf(A, 32 * 127).rearrange("p (l c) -> p l c", l=32)
        b_Bl = Buf("Bl")
        posT = f32(A, 2 * 32).rearrange("p (j l) -> p j l", j=2)
        b_posT = Buf("posT")
        K.dma(posT, self.cmp_pos, w=[b_posT])
        w1s = [f32(A, 8 * 256) for _ in range(2)]
        b_w1s = [Buf("w1s0"), Buf("w1s1")]
        w1b = [bf(A, 8 * 256).rearrange("p (l n) -> p l n", l=8) for _ in range(2)]
        b_w1b = [Buf("w1b0"), Buf("w1b1")]
        w2s = f32(A, 2 * 2 * 64).rearrange("p (j c d) -> p j c d", j=2, c=2)
        b_w2s = Buf("w2s")
        K.dma(w2s, self.cmp_w2, w=[b_w2s])
        w2p = bf(A, 2 * 2 * 128).rearrange("p (h c m) -> p h c m", h=2, c=2)
        b_w2p = Buf("w2p")
        w2v = bf(A, 2 * 64).rearrange("p (c d) -> p c d", c=2)
        b_w2v = Buf("w2v")
        K.op("dve", lambda e: e.memset(w2p, 0.0), w=[b_w2p])
        for h in range(2):
            K.op("dve", lambda e: e.tensor_copy(out=w2p[:, h, :, h * 64:(h + 1) * 64], in_=w2s[:, 0, :, :]), r=[b_w2s], w=[b_w2p])
        K.op("dve", lambda e: e.tensor_copy(out=w2v, in_=w2s[:, 1, :, :]), r=[b_w2s], w=[b_w2v])
        hx = f32(A, 127)
        hy = f32(A, 127)
        b_hx = Buf("hx")
        hid = bf(A, 2 * 2 * 127).rearrange("p (h c n) -> p h c n", h=2, c=2)
        b_hid = Buf("hid")
        K.bank_rng = (0, 4)
        wi = 0
        for j, (tokT, b_tok) in enumerate(((kcT, b_kcT), (vcT, b_vcT))):
            for l_ in range(32):
                K.op("dve", lambda e: e.tensor_scalar(out=Bl[:, l_, :], in0=tokT[:, l_:l_ + 16 * 126 + 1:16],
                                                      scalar1=posT[:, j, l_:l_ + 1], scalar2=None, op0=ALU.add),
                     r=[b_tok, b_posT], w=[b_Bl])
            accs = [K.bank() for _ in range(4)]
            for lg in range(4):
                si = wi % 2
                wi += 1
                K.dma(w1s[si], self.cmp_w1[j, lg].rearrange("p l n -> p (l n)"), w=[b_w1s[si]])
                K.op("pool", lambda e: e.tensor_copy(out=w1b[si].rearrange("p l n -> p (l n)"), in_=w1s[si]), r=[b_w1s[si]], w=[b_w1b[si]])
                for li in range(8):
                    l_ = lg * 8 + li
                    for h in range(2):
                        for c2 in range(2):
                            pt, pb = accs[h * 2 + c2]
                            K.op("pe", lambda e: e.matmul(pt[:, 0:127], lhsT=w1b[si][64 * h:64 * h + 64, li, c2 * 128:(c2 + 1) * 128],
                                                          rhs=Bl[64 * h:64 * h + 64, l_, :], start=(l_ == 0), stop=(l_ == 31)),
                                 r=[b_w1b[si], b_Bl], w=[pb])
            for h in range(2):
                for c2 in range(2):
                    pt, pb = accs[h * 2 + c2]
                    K.op("act", lambda e: e.copy(out=hx, in_=pt[:, 0:127]), r=[pb], w=[b_hx])
                    K.op("dve", lambda e: e.tensor_tensor(out=hy, in0=hx, in1=hx, op=ALU.mult), r=[b_hx], w=[b_hx])
                    K.op("dve", lambda e: e.tensor_scalar(out=hy, in0=hy, scalar1=0.044715, scalar2=1.0, op0=ALU.mult, op1=ALU.add),
                         r=[b_hx], w=[b_hx])
                    K.op("dve", lambda e: e.tensor_tensor(out=hy, in0=hy, in1=hx, op=ALU.mult), r=[b_hx], w=[b_hx])
                    K.op("act", lambda e: e.activation(out=hy, in_=hy, func=AF.Sigmoid, scale=1.5957691216), r=[b_hx], w=[b_hx])
                    K.op("dve", lambda e: e.tensor_tensor(out=hid[:, h, c2, :], in0=hy, in1=hx, op=ALU.mult), r=[b_hx], w=[b_hid])
            K.bank_rng = (4, 8)
            if j == 0:
                pt, pb = K.bank()
                n_ = 0
                for h in range(2):
                    for c2 in range(2):
                        K.op("pe", lambda e: e.matmul(pt[:, 0:127], lhsT=w2p[:, h, c2, :], rhs=hid[:, h, c2, :],
                                                      start=(n_ == 0), stop=(n_ == 3)), r=[b_w2p, b_hid], w=[pb])
                        n_ += 1
                K.op("act", lambda e: e.copy(out=kcmpT[:, 0:127], in_=pt[:, 0:127]), r=[pb], w=[b_kcmpT])
            else:
                for h in range(2):
                    pt, pb = K.bank()
                    for c2 in range(2):
                        K.op("pe", lambda e: e.matmul(pt[0:127, 0:64], lhsT=hid[:, h, c2, :], rhs=w2v[:, c2, :],
                                                      start=(c2 == 0), stop=(c2 == 1)), r=[b_hid, b_w2v], w=[pb])
                    K.op("act", lambda e: e.copy(out=vcmp[0:127, h, :], in_=pt[0:127, 0:64]), r=[pb], w=[b_vcmp])
            K.bank_rng = (0, 4)
        K.bank_rng = (0, 8)
        K.barrier()
        A.release()

        A.mark()
        expand = bf(A, 16 * 128).rearrange("p (k m) -> p k m", k=16)
        wbias = bf(A, 8 * 512).rearrange("p (j n) -> p j n", j=8)
        identb = bf(A, 128)
        cmpbias = bf(A, NT * 127).rearrange("p (t c) -> p t c", t=NT)
        selm = bf(A, 3 * NT * 32).rearrange("p (m t j) -> p m t j", m=3, t=NT)
        b_tab = Buf("tables")
        K.op("dve", lambda e: e.memset(expand, 0.0), w=[b_tab])
        K.dma(expand[0:32], self.expand_in, w=[b_tab])
        K.dma(wbias, self.wbias_in, w=[b_tab])
        K.dma(identb, self.identb_in, w=[b_tab])
        K.dma(cmpbias, self.cmpbias_in, w=[b_tab])
        K.dma(selm, self.selmask_in, w=[b_tab])
        wo = kcvc.rearrange("p (c n) -> p c n", c=4)
        b_wo = Buf("wo")
        PTraw = [bf(A, 8 * 512) for _ in range(2)]
        wo_s = PTraw[0][:, 0:2048].bitcast(F32)
        b_wo_s = Buf("wo_s")
        for c in range(4):
            K.dma(wo_s, self.even_w_out[c * 128:(c + 1) * 128, :], w=[b_wo_s])
            K.op("pool", lambda e: e.tensor_tensor(out=wo[:, c, :], in0=wo_s, in1=g1bc, op=ALU.mult), r=[b_wo_s, b_g1], w=[b_wo])
        K.barrier()
        PT = [p_.rearrange("p (k n) -> p k n", k=8) for p_ in PTraw]
        b_PT = [Buf("PT0"), Buf("PT1")]
        oT = bf(A, 4 * 512).rearrange("p (c n) -> p c n", c=4)
        b_oT = Buf("oT")
        oacc = f32(A, 4 * 512).rearrange("p (q n) -> p q n", q=4)
        b_oacc = Buf("oacc")
        selT = bf(A, 2 * 512).rearrange("p (h n) -> p h n", h=2)
        b_selT = Buf("selT")
        imp = f32(A, 132)
        b_imp = Buf("imp")
        sm = f32(A, 128)
        b_sm = Buf("sm")
        selb = f32(A, 32)
        b_selb = Buf("selb")
        K.op("dve", lambda e: e.memset(imp, 0.0), w=[b_imp])
        K.op("dve", lambda e: e.memset(selT, 0.0), w=[b_selT])
        sS4s = [f32(A, 4 * 127).rearrange("p (g c) -> p g c", g=4) for _ in range(2)]
        sP4s = [f32(A, 4 * 127).rearrange("p (g c) -> p g c", g=4) for _ in range(2)]
        pT4s = [bf(A, 4 * 128).rearrange("p (g q) -> p g q", g=4) for _ in range(2)]
        smxs = [f32(A, 8) for _ in range(2)]
        imps = [f32(A, 132) for _ in range(2)]
        b_sS4s, b_sP4s, b_pT4s, b_smxs, b_imps = ([Buf("%s%d" % (n_, i)) for i in range(2)] for n_ in ("sS4", "sP4", "pT4", "smx", "impx"))
        for i in range(2):
            K.op("dve", lambda e: e.memset(imps[i], 0.0), w=[b_imps[i]])
        cit = [0]
        pti = 0
        for G in range(4):
            for qi in range(4):
                t = 4 * G + qi
                for h in range(2):
                    K.bank_rng = (4, 8)
                    bi = cit[0] % 2
                    cit[0] += 1
                    sS4, sP4, pT4, smx, impx = sS4s[bi], sP4s[bi], pT4s[bi], smxs[bi], imps[bi]
                    b_sS4, b_sP4, b_pT4, b_smx, b_impx = b_sS4s[bi], b_sP4s[bi], b_pT4s[bi], b_smxs[bi], b_imps[bi]
                    imp, b_imp = impx, b_impx
                    pt, pb = K.bank()
                    for g in range(4):
                        K.op("pe", lambda e: e.matmul(pt[:, g * 128:g * 128 + 127], lhsT=qn[64 * h:64 * h + 64, g, t * 128:(t + 1) * 128],
                                                      rhs=kcmpT[64 * h:64 * h + 64, 0:127], start=True, stop=True),
                             r=[b_qn[g][G], b_kcmpT], w=[pb])
                    K.op("dve", lambda e: e.scalar_tensor_tensor(out=sS4, in0=pt[:, :].rearrange("p (g c) -> p g c", g=4)[:, :, 0:127], scalar=ATTN_SCALE,
                                                                 in1=cmpbias[:, t, :].unsqueeze(1).to_broadcast([128, 4, 127]),
                                                                 op0=ALU.mult, op1=ALU.add), r=[pb, b_tab], w=[b_sS4])
                    K.op("act", lambda e: e.activation(out=sS4, in_=sS4, func=AF.Exp), r=[b_sS4], w=[b_sS4])
                    K.op("dve", lambda e: e.tensor_reduce(out=smx[:, 0:4], in_=sS4, axis=AX.X, op=ALU.add), r=[b_sS4], w=[b_smx])
                    K.op("dve", lambda e: e.tensor_scalar(out=smx[:, 0:4], in0=smx[:, 0:4], scalar1=1e-30, scalar2=None, op0=ALU.max), r=[b_smx], w=[b_smx])
                    K.op("dve", lambda e: e.reciprocal(out=smx[:, 0:4], in_=smx[:, 0:4]), r=[b_smx], w=[b_smx])
                    K.op("dve", lambda e: e.tensor_tensor(out=sP4, in0=sS4, in1=smx[:, 0:4].unsqueeze(2).to_broadcast([128, 4, 127]), op=ALU.mult),
                         r=[b_sS4, b_smx], w=[b_sP4])
                    K.op("dve", lambda e: e.tensor_reduce(out=imp[:, 0:127], in_=sP4.rearrange("p g c -> p c g"), axis=AX.X, op=ALU.add),
                         r=[b_sP4], w=[b_imp])
                    pt2, pb2 = K.bank()
                    for g in range(4):
                        K.op("pe", lambda e: e.transpose(out=pt2[0:127, g * 128:(g + 1) * 128], in_=sP4[:, g, :], identity=self.ident),
                             r=[b_sP4, self.b_ident], w=[pb2])
                    K.op("act", lambda e: e.copy(out=pT4[0:127], in_=pt2[0:127, :].rearrange("p (g q) -> p g q", g=4)), r=[pb2], w=[b_pT4])
                    pt3, pb3 = K.bank()
                    for g in range(4):
                        K.op("pe", lambda e: e.matmul(pt3[:, g * 64:(g + 1) * 64], lhsT=pT4[0:127, g, :], rhs=vcmp[0:127, h, :], start=True, stop=True),
                             r=[b_pT4, b_vcmp], w=[pb3])
                    K.op("dve", lambda e: e.tensor_tensor(out=oacc[:, qi, h * 256:(h + 1) * 256].rearrange("p (g d) -> p g d", g=4),
                                                          in0=pt3[:, 0:256].rearrange("p (g d) -> p g d", g=4),
                                                          in1=sg[:, t, h * 12:h * 12 + 12:3].unsqueeze(2).to_broadcast([128, 4, 64]), op=ALU.mult),
                         r=[pb3, b_sg], w=[b_oacc])
                    v4 = lambda a: a.rearrange("p (j m) -> p j m", m=4)
                    K.op("dve", lambda e: e.tensor_reduce(out=sm[:, 0:32], in_=v4(imp[:, 0:128]), axis=AX.X, op=ALU.add), r=[b_imp], w=[b_sm])
                    K.op("dve", lambda e: e.tensor_reduce(out=sm[:, 32:64], in_=v4(imp[:, 1:129]), axis=AX.X, op=ALU.add), r=[b_imp], w=[b_sm])
                    K.op("dve", lambda e: e.tensor_tensor(out=sm[:, 0:32], in0=sm[:, 0:32], in1=sm[:, 32:64], op=ALU.add), r=[b_sm], w=[b_sm])
                    K.op("dve", lambda e: e.tensor_tensor(out=sm[:, 0:32], in0=sm[:, 0:32], in1=selm[:, 0, t, :], op=ALU.mult), r=[b_sm, b_tab], w=[b_sm])
                    K.op("dve", lambda e: e.tensor_tensor(out=sm[:, 0:32], in0=sm[:, 0:32], in1=selm[:, 1, t, :], op=ALU.add), r=[b_sm, b_tab], w=[b_sm])
                    K.op("dve", lambda e: e.max(out=sm[:, 64:72], in_=sm[:, 0:32]), r=[b_sm], w=[b_sm])
                    K.op("dve", lambda e: e.tensor_scalar(out=sm[:, 32:64], in0=sm[:, 0:32], scalar1=sm[:, 71:72], scalar2=None, op0=ALU.is_ge),
                         r=[b_sm], w=[b_sm])
                    K.op("dve", lambda e: e.tensor_tensor(out=sm[:, 32:64], in0=sm[:, 32:64], in1=selm[:, 2, t, :], op=ALU.mult), r=[b_sm, b_tab], w=[b_sm])
                    K.op("dve", lambda e: e.tensor_scalar(out=selb, in0=sm[:, 32:64], scalar1=-NEGB, scalar2=NEGB, op0=ALU.mult, op1=ALU.add),
                         r=[b_sm], w=[b_selb])
                    pt4, pb4 = K.bank()
                    K.op("pe", lambda e: e.transpose(out=pt4[0:32, 0:128], in_=selb, identity=self.ident), r=[b_selb, self.b_ident], w=[pb4])
                    K.op("act", lambda e: e.copy(out=selT[0:32, h, qi * 128:(qi + 1) * 128], in_=pt4[0:32, 0:128]), r=[pb4], w=[b_selT])
            pos = [K.banks[i] for i in range(4)]
            items = []
            for br in range(2):
                for h in range(2):
                    for g in range(4):
                        kt_lo = 0 if br == 0 else max(0, 4 * G - 4)
                        kts = list(range(kt_lo, 4 * G + 4))
                        chunks = [kts[c0:c0 + 8] for c0 in range(0, len(kts), 8)]
                        for ci_, chunk in enumerate(chunks):
                            items.append((br, h, g, kts, chunk, ci_ == len(chunks) - 1))

            def emit_qk(k):
                br, h, g, kts, chunk, last = items[k]
                kT, b_kT = (ksP[h], b_ksT) if br == 0 else (kwP[h], b_kwT)
                P_, b_P = PT[k % 2], b_PT[k % 2]
                K.bank_rng = (4, 8)
                for ci, kt in enumerate(chunk):
                    pt, pb = K.bank()
                    jj = kt - 4 * G + 4
                    need_bias = (jj >= 4) or (br == 1)
                    mms = [(kT[:, kt * 128:(kt + 1) * 128], qr[:, g, G * 512:(G + 1) * 512], [b_kT, b_qr[g][G]])]
                    if br == 0:
                        mms.append((expand[:, kt, :], selT[:, h, :], [b_tab, b_selT]))
                    if need_bias:
                        mms.append((identb, wbias[:, jj, :], [b_tab]))
                    for n_, (lh, rh, rr) in enumerate(mms):
                        K.op("pe", lambda e: e.matmul(pt[:, :], lhsT=lh, rhs=rh, start=(n_ == 0), stop=(n_ == len(mms) - 1)), r=rr, w=[pb])
                    K.op("act", lambda e: e.activation(out=P_[:, ci, :], in_=pt[:, :], func=AF.Exp, scale=ATTN_SCALE), r=[pb], w=[b_P])

            def emit_pv(k):
                br, h, g, kts, chunk, last = items[k]
                V, b_V = (Vs, b_Vs) if br == 0 else (Vw, b_Vw)
                P_, b_P = PT[k % 2], b_PT[k % 2]
                hc = h * 4 + g
                for qi in range(4):
                    t = 4 * G + qi
                    lo_kt = 0 if br == 0 else max(0, t - 4)
                    use = [kt for kt in chunk if lo_kt <= kt <= t]
                    allk = [kt for kt in kts if lo_kt <= kt <= t]
                    po, pob = pos[qi]
                    for kt in use:
                        ci = chunk.index(kt)
                        K.op("pe", lambda e: e.matmul(po[:, 0:65], lhsT=P_[:, ci, qi * 128:(qi + 1) * 128], rhs=V[:, kt, h, :],
                                                      start=(kt == allk[0]), stop=(kt == allk[-1])), r=[b_P, b_V], w=[pob])
                if last:
                    for qi in range(4):
                        t = 4 * G + qi
                        po, pob = pos[qi]
                        K.op("dve", lambda e: e.reciprocal(out=sm[:, 80 + qi:81 + qi], in_=po[:, 64:65]), r=[pob], w=[b_sm])
                        K.op("dve", lambda e: e.tensor_tensor(out=sm[:, 80 + qi:81 + qi], in0=sm[:, 80 + qi:81 + qi],
                                                              in1=sg[:, t, hc * 3 + 1 + br:hc * 3 + 2 + br], op=ALU.mult), r=[b_sm, b_sg], w=[b_sm])
                        K.op("dve", lambda e: e.scalar_tensor_tensor(out=oacc[:, qi, hc * 64:(hc + 1) * 64], in0=po[:, 0:64], scalar=sm[:, 80 + qi:81 + qi],
                                                                     in1=oacc[:, qi, hc * 64:(hc + 1) * 64], op0=ALU.mult, op1=ALU.add),
                             r=[pob, b_sm, b_oacc], w=[b_oacc])
            emit_qk(0)
            for k in range(len(items)):
                if k + 1 < len(items):
                    emit_qk(k + 1)
                emit_pv(k)
            K.bank_rng = (4, 8)
            for qi in range(4):
                t = 4 * G + qi
                pt, pb = K.bank()
                for c in range(4):
                    K.op("pe", lambda e: e.transpose(out=pt[:, c * 128:(c + 1) * 128], in_=oacc[:, qi, c * 128:(c + 1) * 128], identity=self.ident),
                         r=[b_oacc, self.b_ident], w=[pb])
                K.op("act", lambda e: e.copy(out=oT[:, :, qi * 128:(qi + 1) * 128], in_=pt[:, :].rearrange("p (c n) -> p c n", c=4)),
                     r=[pb], w=[b_oT])
            for qi in range(4):
                t = 4 * G + qi
                for hf in range(2):
                    pt, pb = K.bank()
                    for c in range(4):
                        K.op("pe", lambda e: e.matmul(pt[:, :], lhsT=oT[:, c, qi * 128:(qi + 1) * 128], rhs=wo[:, c, hf * 512:(hf + 1) * 512],
                                                      start=(c == 0), stop=(c == 3)), r=[b_oT, b_wo], w=[pb])
                    xs = self.x_sb[:, t, hf * 512:(hf + 1) * 512]
                    K.op("dve", lambda e: e.tensor_tensor(out=xs, in0=pt[:, :], in1=xs, op=ALU.add), r=[pb, self.xb[t]], w=[self.xb[t]])
        K.bank_rng = (0, 8)
        K.barrier()
        A.release()
        A.release()


for _n, _f in list(vars(_EvenMixin).items()):
    if callable(_f) and not _n.startswith("__"):
        setattr(_Prog, _n, _f)


N_ODD_BLK = 18
ENABLE_RWKV = os.environ.get("KERNEL_ENABLE_RWKV", "1") == "1"
LNX_EPS = 64e-5
DECAY_C = -0.6065306597126334


def _odd_w_blocks(w_in):
    cols = []
    for base in (0, 512, 1024):
        for c in range(4):
            cols.append(list(range(base + c * 128, base + (c + 1) * 128)))
    cols.append(list(range(1536, 1664)))
    cols.append(list(range(1664, 1792)))
    for c in range(4):
        cols.append(list(range(1792 + c * 128, 1792 + (c + 1) * 128)))
    out = np.zeros((len(cols), 128, 8, 128), np.float32)
    for i, cl in enumerate(cols):
        out[i] = w_in[:, np.array(cl)].reshape(8, 128, 128).transpose(1, 0, 2)
    return out


def _odd_consts():
    c = {}
    p = np.arange(128)
    c["blockones"] = (p[:, None] // 64 == p[None, :] // 64).astype(np.float32)
    same = (p[:, None] // 64 == p[None, :] // 64)
    mA = same & (p[None, :] < p[:, None])
    mX = same & (p[:, None] < p[None, :])
    mI = same & (p[:, None] <= p[None, :])
    c["rmasks"] = np.ascontiguousarray(np.stack([mA, mX, mI], axis=1).astype(np.float32))
    invc = np.zeros((128, 4, 16), np.float32)
    for gi, w in enumerate((2, 4, 8, 16)):
        invc[:, gi, :] = 1.0 / np.minimum(np.arange(16) + 1, w)
    c["invc"] = invc
    return c


class _OddMixin:
    def add_wout(self, oT, b_oT, src, load):
        K = self.K
        wo, b_wo = load(src, mul=self.bc[2], b_mul=self.b_bc[2])
        for t in range(NT):
            for hf in range(2):
                pt, pb = K.bank()
                K.op("pe", lambda e: e.matmul(pt[:, :], lhsT=oT[:, t * 128:(t + 1) * 128], rhs=wo[:, hf * 512:(hf + 1) * 512],
                                              start=True, stop=True), r=[b_oT, b_wo], w=[pb])
                xs = self.x_sb[:, t, hf * 512:(hf + 1) * 512]
                K.op("dve", lambda e: e.tensor_tensor(out=xs, in0=pt[:, :], in1=xs, op=ALU.add), r=[pb, self.xb[t]], w=[self.xb[t]])

    def add_wout_multi(self, oT4, b_oT4, w_out_dram, row0, nch, stage, b_stage, wo4, b_wo4):
        K = self.K
        for c in range(nch):
            K.dma(stage, w_out_dram[row0 + c * 128:row0 + (c + 1) * 128, :], w=[b_stage])
            K.op("pool", lambda e: e.tensor_tensor(out=wo4[:, c, :], in0=stage, in1=self.bc[2], op=ALU.mult), r=[b_stage, self.b_bc[2]], w=[b_wo4])
        for t in range(NT):
            for hf in range(2):
                pt, pb = K.bank()
                for c in range(nch):
                    K.op("pe", lambda e: e.matmul(pt[:, :], lhsT=oT4[:, c, t * 128:(t + 1) * 128], rhs=wo4[:, c, hf * 512:(hf + 1) * 512],
                                                  start=(c == 0), stop=(c == nch - 1)), r=[b_oT4, b_wo4], w=[pb])
                xs = self.x_sb[:, t, hf * 512:(hf + 1) * 512]
                K.op("dve", lambda e: e.tensor_tensor(out=xs, in0=pt[:, :], in1=xs, op=ALU.add), r=[pb, self.xb[t]], w=[self.xb[t]])

    def odd_mixer(self):
        nc, K, A = self.nc, self.K, self.A
        l = 1
        self.load_mod_bc(l, 0, self.norm_mix[l:l + 1, :])
        HTB = 8 * S * 2
        hT = A.alloc_top(HTB, BF16).rearrange("p (k s) -> p k s", k=8)
        b_hT = [Buf("ohT%d" % t) for t in range(NT)]
        self.norm_transpose(hT, b_hT)
        wsrc = lambda i: self.odd_w[i].rearrange("p k n -> p (k n)")
        scratch = self.bcall[:, 0:2 * D]

        A.mark()
        load = self.make_wloader(3, 1)
        uT = f32(A, 16 + S)
        s1 = f32(A, 16 + S)
        s2 = f32(A, 16 + S)
        b_uT, b_s1, b_s2 = Buf("uT"), Buf("s1"), Buf("s2")
        pooled = bf(A, S)
        b_pooled = Buf("pooled")
        oT4 = bf(A, 4 * S).rearrange("p (c s) -> p c s", c=4)
        b_oT = Buf("poT")
        pwo4 = bf(A, 4 * 1024).rearrange("p (c n) -> p c n", c=4)
        b_pwo4 = Buf("pwo4")
        pstage = f32(A, 1024)
        b_pstage = Buf("pstage")
        pws = f32(A, 128)
        pwb = bf(A, 128)
        b_pws, b_pwb = Buf("pws"), Buf("pwb")
        psc = f32(A, 4)
        invc = f32(A, 64).rearrange("p (g n) -> p g n", g=4)
        t16 = f32(A, 16)
        b_pc = Buf("poolconst")
        b_t16 = Buf("t16")
        K.dma(psc, self.pool_scale, w=[b_pc])
        K.dma(invc, self.invc_in, w=[b_pc])
        for a_, b_ in ((uT, b_uT), (s1, b_s1), (s2, b_s2)):
            K.op("dve", lambda e: e.memset(a_[:, 0:16], 0.0), w=[b_])
        for gi in range(4):
            win = 2 << gi
            wv, b_wv = load(wsrc(14 + gi))
            K.dma(pws, self.pool_w[gi], w=[b_pws])
            K.op("pool", lambda e: e.tensor_copy(out=pwb, in_=pws), r=[b_pws], w=[b_pwb])
            for G in range(4):
                p_, pb_ = self.proj_fm(wv, b_wv, hT, b_hT, G)
                K.op("act", lambda e: e.copy(out=uT[:, 16 + G * 512:16 + (G + 1) * 512], in_=p_[:, :]), r=[pb_], w=[b_uT])
            src, b_src = uT, b_uT
            for step in range(gi + 1):
                sh = 1 << step
                dst, b_dst = (s1, b_s1) if step % 2 == 0 else (s2, b_s2)
                K.op("dve", lambda e: e.tensor_tensor(out=dst[:, 16:16 + S], in0=src[:, 16:16 + S], in1=src[:, 16 - sh:16 - sh + S], op=ALU.add),
                     r=[b_src], w=[b_dst])
                src, b_src = dst, b_dst
            K.op("dve", lambda e: e.scalar_tensor_tensor(out=pooled, in0=src[:, 16:16 + S], scalar=1.0 / win, in1=uT[:, 16:16 + S],
                                                         op0=ALU.mult, op1=ALU.subtract), r=[b_src, b_uT], w=[b_pooled])
            K.op("dve", lambda e: e.tensor_tensor(out=t16, in0=src[:, 16:32], in1=invc[:, gi, :], op=ALU.mult), r=[b_src, b_pc], w=[b_t16])
            K.op("dve", lambda e: e.tensor_tensor(out=pooled[:, 0:16], in0=t16, in1=uT[:, 16:32], op=ALU.subtract),
                 r=[b_t16, b_uT, b_pooled], w=[b_pooled])
            for G in range(4):
                pt, pb = K.bank()
                K.op("pe", lambda e: e.matmul(pt[:, :], lhsT=pwb, rhs=pooled[:, G * 512:(G + 1) * 512], start=True, stop=True),
                     r=[b_pwb, b_pooled], w=[pb])
                K.op("dve", lambda e: e.tensor_scalar(out=oT4[:, gi, G * 512:(G + 1) * 512], in0=pt[:, :], scalar1=psc[:, gi:gi + 1], scalar2=None,
                                                      op0=ALU.mult), r=[pb, b_pc], w=[b_oT])
        self.add_wout_multi(oT4, b_oT, self.odd_w_out, 512, 4, pstage, b_pstage, pwo4, b_pwo4)
        K.barrier()
        A.release()
        if os.environ.get("KDBG") == "pool" or not ENABLE_RWKV:
            A.release_top(HTB)
            return

        A.mark()
        chv = f32(A, 28).rearrange("p (k c) -> p k c", k=7)
        mu = f32(A, 14)
        so = [0]

        def sc(n):
            v = scratch[:, so[0]:so[0] + n]
            so[0] += n
            return v
        bones = sc(128)
        rmask = sc(3 * 128).rearrange("p (m n) -> p m n", m=3)
        ones64 = f32(A, 64)
        wa2s = f32(A, 512)
        wa2b = sc(256).bitcast(BF16)
        g2s = wa2s
        g2b = sc(256).bitcast(BF16)
        b_cst = Buf("rconst")
        b_wa2s, b_wa2b, b_g2s, b_g2b = Buf("wa2s"), Buf("wa2b"), Buf("g2s"), Buf("g2b")
        K.dma(chv, self.chv_in, w=[b_cst])
        K.dma(mu, self.mu_in, w=[b_cst])
        K.dma(bones, self.blockones_in, w=[b_cst])
        K.dma(rmask, self.rmasks_in, w=[b_cst])
        K.op("dve", lambda e: e.memset(ones64, 1.0), w=[b_cst])
        K.dma(wa2s, self.wa2_in, w=[b_wa2s])
        K.op("pool", lambda e: e.tensor_copy(out=wa2b, in_=wa2s), r=[b_wa2s], w=[b_wa2b])
        K.dma(g2s, self.g2_in, w=[b_wa2s])
        K.op("pool", lambda e: e.tensor_copy(out=g2b, in_=g2s), r=[b_wa2s], w=[b_g2b])
        Sx = [f32(A, S) for _ in range(8)]
        b_S = [Buf("S%d" % i) for i in range(8)]
        Tall = f32(A, 3 * S + 16)
        sglT = bf(A, S)
        b_sgl = Buf("sglT")
        T3 = Tall[:, 0:3 * S // 2].bitcast(BF16).rearrange("p (m t c) -> p m t c", m=3, t=NT)
        Ab, Bb, Kb = [Tall[:, 3 * S // 2 + i * (S // 2):3 * S // 2 + (i + 1) * (S // 2)].bitcast(BF16) for i in range(3)]
        b_Ab, b_Bb, b_Kb, b_Rb = Buf("Ab"), Buf("Bb"), Buf("Kb"), Buf("Rb")
        b_T = Buf("Ttm")
        raw = Tall[:, 0:S + 1]
        b_raw = Buf("raw")
        waT = Tall[:, S + 8:S + 8 + S // 2].bitcast(BF16)
        b_waT = Buf("waT")
        lbase = S + 8 + S // 2
        lstg = Tall[:, lbase:lbase + 1024]
        lwb = [Tall[:, lbase + 1024 + i * 512:lbase + 1024 + (i + 1) * 512].bitcast(BF16) for i in range(2)]
        b_lstg, b_lwb = Buf("lstg"), [Buf("lwb0"), Buf("lwb1")]
        lst = {"w": 0}

        def lload(src):
            wi = lst["w"] % 2
            lst["w"] += 1
            K.dma(lstg, src, w=[b_lstg])
            K.op("pool", lambda e: e.tensor_copy(out=lwb[wi], in_=lstg), r=[b_lstg], w=[b_lwb[wi]])
            return lwb[wi], b_lwb[wi]

        Hs = sc(64)
        Hb = sc(32).bitcast(BF16)
        b_Hb = Buf("Hb")
        Wsb = sc(64).bitcast(BF16)
        Usb = sc(64).bitcast(BF16)
        W2sb = sc(128)
        Y1sb = sc(128)
        b_W2, b_Y1 = Buf("W2sb"), Buf("Y1sb")
        PC = sc(32)
        st1 = sc(32)
        st2 = sc(32)
        st3 = sc(32)
        b_H, b_W, b_U, b_PC, b_st = Buf("H"), Buf("W"), Buf("U"), Buf("PC"), Buf("st")
        fmo = sc(0)

        for P in range(4):
            K.op("dve", lambda e: e.memset(raw[:, 0:1], 0.0), w=[b_raw])

            def proj_shift(bi, mucol, dst, b_dst):
                wv, b_wv = lload(wsrc(bi))
                for G in range(4):
                    p_, pb_ = self.proj_fm(wv, b_wv, hT, b_hT, G)
                    K.op("act", lambda e: e.copy(out=raw[:, 1 + G * 512:1 + (G + 1) * 512], in_=p_[:, :]), r=[pb_], w=[b_raw])
                K.op("dve", lambda e: e.tensor_tensor(out=dst, in0=raw[:, 0:S], in1=raw[:, 1:S + 1], op=ALU.subtract), r=[b_raw], w=[b_dst])
                K.op("dve", lambda e: e.scalar_tensor_tensor(out=dst, in0=dst, scalar=mu[:, mucol:mucol + 1], in1=raw[:, 1:S + 1],
                                                             op0=ALU.mult, op1=ALU.add), r=[b_dst, b_raw, b_cst], w=[b_dst])
            proj_shift(P, P, Sx[0], b_S[0])
            proj_shift(4 + P, 4 + P, Sx[1], b_S[1])
            proj_shift(8 + P, 8 + P, Sx[2], b_S[2])
            proj_shift(12, 12, Sx[6], b_S[6])
            K.op("act", lambda e: e.activation(out=waT[0:64, :], in_=Sx[6][0:64, :], func=AF.Tanh), r=[b_S[6]], w=[b_waT])
            K.op("act", lambda e: e.copy(out=waT[64:128, :], in_=Sx[6][64:128, :]), r=[b_S[6]], w=[b_waT])
            proj_shift(13, 13, Sx[6], b_S[6])
            K.op("act", lambda e: e.activation(out=sglT, in_=Sx[6], func=AF.Sigmoid), r=[b_S[6]], w=[b_sgl])
            for G in range(4):
                sl = slice(G * 512, (G + 1) * 512)
                pt, pb = K.bank()
                K.op("pe", lambda e: e.matmul(pt[:, :], lhsT=wa2b[0:64, P * 128:(P + 1) * 128], rhs=waT[0:64, sl], start=True, stop=True),
                     r=[b_wa2b, b_waT], w=[pb])
                K.op("act", lambda e: e.activation(out=Sx[3][:, sl], in_=pt[:, :], func=AF.Sigmoid, bias=chv[:, 0, P:P + 1]),
                     r=[pb, b_cst], w=[b_S[3]])
                pt, pb = K.bank()
                K.op("pe", lambda e: e.matmul(pt[:, :], lhsT=wa2b[64:128, P * 128:(P + 1) * 128], rhs=waT[64:128, sl], start=True, stop=True),
                     r=[b_wa2b, b_waT], w=[pb])
                K.op("act", lambda e: e.activation(out=Sx[4][:, sl], in_=pt[:, :], func=AF.Sigmoid, bias=chv[:, 1, P:P + 1]),
                     r=[pb, b_cst], w=[b_S[4]])
            dve = lambda fn, r, w: K.op("dve", fn, r=r, w=w)
            dve(lambda e: e.tensor_scalar(out=Sx[3], in0=Sx[3], scalar1=DECAY_C, scalar2=None, op0=ALU.mult), [b_S[3]], [b_S[3]])
            dve(lambda e: e.tensor_scalar(out=Sx[5], in0=Sx[1], scalar1=chv[:, 2, P:P + 1], scalar2=None, op0=ALU.mult),
                [b_S[1], b_cst], [b_S[5]])
            K.op("act", lambda e: e.activation(out=raw[:, 0:S], in_=Sx[5], func=AF.Square), r=[b_S[5]], w=[b_raw])
            for G in range(4):
                sl = slice(G * 512, (G + 1) * 512)
                pt, pb = K.bank()
                K.op("pe", lambda e: e.matmul(pt[:, :], lhsT=bones, rhs=raw[:, sl], start=True, stop=True), r=[b_cst, b_raw], w=[pb])
                K.op("act", lambda e: e.activation(out=Sx[7][:, sl], in_=pt[:, :], func=AF.Sqrt), r=[pb], w=[b_S[7]])
            dve(lambda e: e.tensor_scalar(out=Sx[7], in0=Sx[7], scalar1=1e-12, scalar2=None, op0=ALU.max), [b_S[7]], [b_S[7]])
            dve(lambda e: e.reciprocal(out=Sx[7], in_=Sx[7]), [b_S[7]], [b_S[7]])
            dve(lambda e: e.tensor_tensor(out=Sx[5], in0=Sx[5], in1=Sx[7], op=ALU.mult), [b_S[5], b_S[7]], [b_S[5]])
            dve(lambda e: e.tensor_scalar(out=Sx[7], in0=Sx[4], scalar1=-1.0, scalar2=chv[:, 3, P:P + 1], op0=ALU.add, op1=ALU.mult),
                [b_S[4], b_cst], [b_S[7]])
            dve(lambda e: e.scalar_tensor_tensor(out=Sx[1], in0=Sx[7], scalar=1.0, in1=Sx[1], op0=ALU.add, op1=ALU.mult),
                [b_S[7], b_S[1]], [b_S[1]])
            dve(lambda e: e.tensor_tensor(out=Sx[4], in0=Sx[5], in1=Sx[4], op=ALU.mult), [b_S[5], b_S[4]], [b_S[4]])
            dve(lambda e: e.scalar_tensor_tensor(out=raw[:, 0:S], in0=Sx[0], scalar=chv[:, 6, P:P + 1], in1=Sx[1], op0=ALU.mult, op1=ALU.mult),
                [b_S[0], b_S[1], b_cst, b_raw], [b_raw])
            for G in range(4):
                sl = slice(G * 512, (G + 1) * 512)
                pt, pb = K.bank()
                K.op("pe", lambda e: e.matmul(pt[:, :], lhsT=bones, rhs=raw[:, sl], start=True, stop=True), r=[b_cst, b_raw], w=[pb])
                dve(lambda e: e.tensor_tensor(out=Sx[7][:, sl], in0=pt[:, :], in1=Sx[2][:, sl], op=ALU.mult), [pb, b_S[2], b_S[7]], [b_S[7]])
            for c in range(32):
                cs = slice(c * 64, (c + 1) * 64)
                dve(lambda e: e.tensor_tensor_scan(out=Sx[6][:, cs], data0=ones64, data1=Sx[3][:, cs], initial=0.0, op0=ALU.mult, op1=ALU.add),
                    [b_S[3], b_cst, b_S[6]], [b_S[6]])
            K.op("act", lambda e: e.activation(out=PC, in_=Sx[6][:, 63:S:64], func=AF.Exp), r=[b_S[6]], w=[b_PC])
            dve(lambda e: e.tensor_tensor(out=Sx[3], in0=Sx[6], in1=Sx[3], op=ALU.subtract), [b_S[6], b_S[3]], [b_S[3]])
            K.op("act", lambda e: e.activation(out=Sx[3], in_=Sx[3], func=AF.Exp), r=[b_S[3]], w=[b_S[3]])
            dve(lambda e: e.scalar_tensor_tensor(out=Sx[5], in0=Sx[5], scalar=-1.0, in1=Sx[3], op0=ALU.mult, op1=ALU.mult),
                [b_S[5], b_S[3]], [b_S[5]])
            K.op("act", lambda e: e.activation(out=Sx[3], in_=Sx[6], func=AF.Exp), r=[b_S[6], b_S[5]], w=[b_S[3]])
            dve(lambda e: e.tensor_tensor(out=Sx[0], in0=Sx[0], in1=Sx[3], op=ALU.mult), [b_S[0], b_S[3]], [b_S[0]])
            K.op("act", lambda e: e.activation(out=Sx[3], in_=Sx[6], func=AF.Exp, scale=-1.0), r=[b_S[6], b_S[0]], w=[b_S[3]])
            dve(lambda e: e.tensor_tensor(out=Sx[4], in0=Sx[4], in1=Sx[3], op=ALU.mult), [b_S[4], b_S[3]], [b_S[4]])
            dve(lambda e: e.tensor_tensor(out=Sx[1], in0=Sx[1], in1=Sx[3], op=ALU.mult), [b_S[1], b_S[3]], [b_S[1]])
            c3 = lambda a: a.rearrange("p (c n) -> p c n", n=64)
            pcb = PC.unsqueeze(2).to_broadcast([128, 32, 64])
            dve(lambda e: e.tensor_tensor(out=c3(Sx[3]), in0=c3(Sx[4]), in1=pcb, op=ALU.mult), [b_S[4], b_PC, b_S[3]], [b_S[3]])
            dve(lambda e: e.tensor_tensor(out=c3(Sx[6]), in0=c3(Sx[1]), in1=pcb, op=ALU.mult), [b_S[1], b_PC, b_S[6]], [b_S[6]])
            K.barrier()
            CUT = os.environ.get("KCUT2", "")
            if CUT == "prep":
                break
            K.op("act", lambda e: e.copy(out=Ab, in_=Sx[5]), r=[b_S[5]], w=[b_Ab])
            K.op("pool", lambda e: e.tensor_copy(out=Bb, in_=Sx[4]), r=[b_S[4]], w=[b_Bb])
            K.op("dve", lambda e: e.tensor_copy(out=Kb, in_=Sx[1]), r=[b_S[1]], w=[b_Kb])
            for tau in range(NT):
                pt, pb = K.bank()
                for m, si in enumerate((2, 3, 6)):
                    K.op("pe", lambda e: e.transpose(out=pt[:, m * 128:(m + 1) * 128], in_=Sx[si][:, tau * 128:(tau + 1) * 128], identity=self.ident),
                         r=[b_S[si], self.b_ident], w=[pb])
                K.op("act", lambda e: e.copy(out=T3[:, :, tau, :], in_=pt[:, 0:384].rearrange("p (m c) -> p m c", m=3)), r=[pb], w=[b_T])
            K.barrier()
            if CUT == "tm":
                break
            ytm = Sx[2].rearrange("p (t c) -> p t c", t=NT)
            b_ytm = Buf("ytm")
            mats = Sx[3][:, 0:S // 2].bitcast(BF16).rearrange("p (m i n) -> p m i n", m=4, i=4)
            b_mats = Buf("mats")
            Rb = Sx[3][:, S // 2:S].bitcast(BF16)
            dbl = Sx[6][:, 0:S // 2].bitcast(BF16).rearrange("p (m i n) -> p m i n", m=4, i=4)
            b_dbl = [Buf("dbl%d" % i) for i in range(4)]
            K.op("act", lambda e: e.copy(out=Rb, in_=Sx[0]), r=[b_S[0]], w=[b_Rb])
            K.op("dve", lambda e: e.memset(Hs, 0.0), w=[b_H])
            K.op("dve", lambda e: e.memset(Hb, 0.0), w=[b_Hb])
            At, Bt, Kt, Rt = Ab, Bb, Kb, Rb
            b_At, b_Bt, b_Kt, b_Rt = b_Ab, b_Bb, b_Kb, b_Rb
            mbc = lambda m: rmask[:, m, :].unsqueeze(1).to_broadcast([128, 4, 128])
            idbc = self.ident.unsqueeze(1).to_broadcast([128, 4, 128])
            v4 = lambda pt: pt[:, :].rearrange("p (i n) -> p i n", i=4)
            mats2 = Sx[6][:, S // 2:S].bitcast(BF16).rearrange("p (m i n) -> p m i n", m=4, i=4)
            matsb = [mats, mats2]
            b_matsb = [b_mats, Buf("mats2")]

            def pre_steps(nb):
                mt, b_mt = matsb[nb % 2], b_matsb[nb % 2]
                steps = []

                def mm_items(lh, b_lh, rh, b_rh):
                    res = []
                    for h in range(2):
                        pt, pb = K.bank()
                        for tl in range(2):
                            tok = slice((2 * nb + tl) * 128, (2 * nb + tl + 1) * 128)
                            K.op("pe", lambda e: e.matmul(pt[:, tl * 128:(tl + 1) * 128], lhsT=lh[64 * h:64 * h + 64, tok], rhs=rh[64 * h:64 * h + 64, tok],
                                                          start=True, stop=True), r=[b_lh, b_rh], w=[pb])
                        res.append((pt, pb))
                    return res

                def evac_items(res, dst, m, bdst):
                    for h in range(2):
                        pt, pb = res[h]
                        dve(lambda e: e.tensor_tensor(out=dst[:, h::2, :], in0=pt[:, 0:256].rearrange("p (i n) -> p i n", i=2),
                                                      in1=rmask[:, m, :].unsqueeze(1).to_broadcast([128, 2, 128]), op=ALU.mult), [pb, b_cst], [bdst])
                steps.append(lambda: evac_items(mm_items(At, b_At, Bt, b_Bt), dbl[:, 0], 0, b_dbl[0]))
                steps.append(lambda: evac_items(mm_items(Bt, b_Bt, At, b_At), dbl[:, 1], 1, b_dbl[1]))
                steps.append(lambda: evac_items(mm_items(Kt, b_Kt, At, b_At), mt[:, 0], 1, b_mt))
                steps.append(lambda: evac_items(mm_items(Bt, b_Bt, Rt, b_Rt), mt[:, 1], 2, b_mt))
                steps.append(lambda: evac_items(mm_items(Kt, b_Kt, Rt, b_Rt), mt[:, 2], 2, b_mt))
                steps.append(lambda: dve(lambda e: e.tensor_tensor(out=mt[:, 3], in0=dbl[:, 1], in1=idbc, op=ALU.add), [b_dbl[1], self.b_ident], [b_mt]))
                order = [(0, 1, 2, 3), (2, 3, 0, 1)]
                for lev in range(1, 6):
                    ca, cx, na, nx = order[(lev - 1) % 2]

                    def st_a(ca=ca, cx=cx, na=na):
                        pt, pb = K.bank()
                        for idx in range(4):
                            K.op("pe", lambda e: e.matmul(pt[:, idx * 128:(idx + 1) * 128], lhsT=dbl[:, cx, idx, :], rhs=dbl[:, ca, idx, :], start=True, stop=True),
                                 r=[b_dbl[cx], b_dbl[ca]], w=[pb])
                        K.op("act", lambda e: e.copy(out=dbl[:, na], in_=v4(pt)), r=[pb], w=[b_dbl[na]])

                    def st_x(ca=ca, cx=cx, nx=nx):
                        pt, pb = K.bank()
                        for idx in range(4):
                            K.op("pe", lambda e: e.matmul(pt[:, idx * 128:(idx + 1) * 128], lhsT=dbl[:, ca, idx, :], rhs=dbl[:, cx, idx, :], start=True, stop=True),
                                 r=[b_dbl[cx], b_dbl[ca]], w=[pb])
                        K.op("act", lambda e: e.copy(out=dbl[:, nx], in_=v4(pt)), r=[pb], w=[b_dbl[nx]])

                    def st_t(na=na):
                        pt, pb = K.bank()
                        for idx in range(4):
                            K.op("pe", lambda e: e.matmul(pt[:, idx * 128:(idx + 1) * 128], lhsT=dbl[:, na, idx, :], rhs=mt[:, 3, idx, :], start=True, stop=True),
                                 r=[b_dbl[na], b_mt], w=[pb])
                        dve(lambda e: e.tensor_tensor(out=mt[:, 3], in0=mt[:, 3], in1=v4(pt), op=ALU.add), [pb, b_mt], [b_mt])
                    steps.append(st_a)
                    if lev < 5:
                        steps.append(st_x)
                    steps.append(st_t)
                return steps

            def scan_steps(nb):
                mt, b_mt = matsb[nb % 2], b_matsb[nb % 2]
                steps = []
                for tl in range(2):
                    for hf in range(2):
                        def mk(tl=tl, hf=hf):
                            tau = 2 * nb + tl
                            tok = slice(tau * 128, (tau + 1) * 128)
                            c = tau * 2 + hf
                            ph = slice(64 * hf, 64 * hf + 64)
                            HS = [slice(0, 64), slice(64, 128)]

                            def s_w2():
                                for h in range(2):
                                    hs = HS[h]
                                    pW2, pW2b = K.bank()
                                    K.op("pe", lambda e: e.matmul(pW2[:, 0:64], lhsT=mt[ph, 0, tl * 2 + h, :], rhs=T3[ph, 0, tau, hs], start=True, stop=True),
                                         r=[b_mt, b_T], w=[pW2b])
                                    K.op("act", lambda e: e.copy(out=W2sb[ph, hs], in_=pW2[ph, 0:64]), r=[pW2b], w=[b_W2])

                            def s_w():
                                for h in range(2):
                                    hs = HS[h]
                                    pW, pWb = K.bank()
                                    K.op("pe", lambda e: e.matmul(pW[:, 0:64], lhsT=At[hs, tok], rhs=Hb[hs, :], start=True, stop=True), r=[b_At, b_Hb], w=[pWb])
                                    dve(lambda e: e.tensor_tensor(out=Wsb[ph, hs], in0=pW[ph, 0:64], in1=W2sb[ph, hs], op=ALU.add), [pWb, b_W2], [b_W])

                            def s_u():
                                for h in range(2):
                                    hs = HS[h]
                                    pU, pUb = K.bank()
                                    K.op("pe", lambda e: e.matmul(pU[:, 0:64], lhsT=mt[ph, 3, tl * 2 + h, :], rhs=Wsb[ph, hs], start=True, stop=True),
                                         r=[b_mt, b_W], w=[pUb])
                                    dve(lambda e: e.tensor_copy(out=Usb[ph, hs], in_=pU[ph, 0:64]), [pUb], [b_U])

                            def s_y1():
                                for h in range(2):
                                    hs = HS[h]
                                    pY1, pY1b = K.bank()
                                    K.op("pe", lambda e: e.matmul(pY1[:, 0:64], lhsT=Rt[hs, tok], rhs=Hb[hs, :], start=True, stop=True), r=[b_Rt, b_Hb], w=[pY1b])
                                    K.op("act", lambda e: e.copy(out=Y1sb[ph, hs], in_=pY1[ph, 0:64]), r=[pY1b], w=[b_Y1])

                            def s_h():
                                for h in range(2):
                                    hs = HS[h]
                                    pH, pHb = K.bank()
                                    K.op("pe", lambda e: e.matmul(pH[:, 0:64], lhsT=T3[ph, 1, tau, :], rhs=Usb[ph, hs], start=True, stop=False), r=[b_T, b_U], w=[pHb])
                                    K.op("pe", lambda e: e.matmul(pH[:, 0:64], lhsT=T3[ph, 2, tau, :], rhs=T3[ph, 0, tau, hs], start=False, stop=True), r=[b_T], w=[pHb])
                                    dve(lambda e: e.scalar_tensor_tensor(out=Hb[hs, :], in0=Hs[hs, :], scalar=PC[hs, c:c + 1], in1=pH[hs, 0:64],
                                                                         op0=ALU.mult, op1=ALU.add), [pHb, b_H, b_PC], [b_Hb])
                                    dve(lambda e: e.scalar_tensor_tensor(out=Hs[hs, :], in0=Hs[hs, :], scalar=PC[hs, c:c + 1], in1=pH[hs, 0:64],
                                                                         op0=ALU.mult, op1=ALU.add), [pHb, b_H, b_PC], [b_H])

                            def s_y2():
                                for h in range(2):
                                    hs = HS[h]
                                    pY, pYb = K.bank()
                                    K.op("pe", lambda e: e.matmul(pY[:, 0:64], lhsT=mt[ph, 1, tl * 2 + h, :], rhs=Usb[ph, hs], start=True, stop=False),
                                         r=[b_mt, b_U], w=[pYb])
                                    K.op("pe", lambda e: e.matmul(pY[:, 0:64], lhsT=mt[ph, 2, tl * 2 + h, :], rhs=T3[ph, 0, tau, hs], start=False, stop=True),
                                         r=[b_mt, b_T], w=[pYb])
                                    dve(lambda e: e.tensor_tensor(out=ytm[ph, tau, hs], in0=pY[ph, 0:64], in1=Y1sb[ph, hs], op=ALU.add), [pYb, b_Y1], [b_ytm])
                            return [s_w2, s_y1, s_w, s_u, s_h, s_y2]
                        steps.extend(mk())
                return steps

            cur = pre_steps(0)
            for f_ in cur:
                f_()
            for nb in range(8):
                sc_ = scan_steps(nb)
                pr_ = pre_steps(nb + 1) if nb + 1 < 8 else []
                n_ = max(len(sc_), len(pr_))
                for i_ in range(n_):
                    if i_ < len(sc_):
                        sc_[i_]()
                    if i_ < len(pr_):
                        pr_[i_]()
            K.barrier()
            K.barrier()
            if CUT in ("pre", "scan", "pre1", "pre2", "prem"):
                break
            y3 = Sx[2].rearrange("p (g n) -> p g n", n=64)
            sq = Tall[:, S:2 * S]
            b_sq = Buf("sq")
            K.op("act", lambda e: e.activation(out=sq, in_=Sx[2], func=AF.Square), r=[b_ytm], w=[b_sq])
            dve(lambda e: e.tensor_reduce(out=st1, in_=y3, axis=AX.X, op=ALU.add), [b_ytm], [b_st])
            dve(lambda e: e.tensor_reduce(out=st2, in_=sq.rearrange("p (g n) -> p g n", n=64), axis=AX.X, op=ALU.add), [b_sq], [b_st])
            dve(lambda e: e.tensor_scalar(out=st1, in0=st1, scalar1=1.0 / 64, scalar2=None, op0=ALU.mult), [b_st], [b_st])
            dve(lambda e: e.tensor_tensor(out=st3, in0=st1, in1=st1, op=ALU.mult), [b_st], [b_st])
            dve(lambda e: e.scalar_tensor_tensor(out=st2, in0=st2, scalar=1.0 / 64, in1=st3, op0=ALU.mult, op1=ALU.subtract), [b_st], [b_st])
            dve(lambda e: e.tensor_scalar(out=st2, in0=st2, scalar1=LNX_EPS, scalar2=None, op0=ALU.add), [b_st], [b_st])
            K.op("act", lambda e: e.activation(out=st2, in_=st2, func=AF.Sqrt), r=[b_st], w=[b_st])
            dve(lambda e: e.reciprocal(out=st2, in_=st2), [b_st], [b_st])
            dve(lambda e: e.tensor_tensor(out=y3, in0=y3, in1=st1.unsqueeze(2).to_broadcast([128, 32, 64]), op=ALU.subtract), [b_ytm, b_st], [b_ytm])
            dve(lambda e: e.tensor_tensor(out=y3, in0=y3, in1=st2.unsqueeze(2).to_broadcast([128, 32, 64]), op=ALU.mult), [b_ytm, b_st], [b_ytm])
            fm = Tall[:, 2 * S:3 * S]
            b_fm = Buf("fm")
            for tq in range(4):
                pt, pb = K.bank()
                for i4 in range(4):
                    tau = tq * 4 + i4
                    K.op("pe", lambda e: e.transpose(out=pt[:, i4 * 128:(i4 + 1) * 128], in_=ytm[:, tau, :], identity=self.ident),
                         r=[b_ytm, self.b_ident], w=[pb])
                dve(lambda e: e.tensor_scalar(out=fm[:, tq * 512:(tq + 1) * 512], in0=pt[:, :], scalar1=chv[:, 4, P:P + 1], scalar2=chv[:, 5, P:P + 1],
                                              op0=ALU.mult, op1=ALU.add), [pb, b_cst], [b_fm])
            dve(lambda e: e.tensor_tensor(out=fm, in0=fm, in1=Sx[7], op=ALU.add), [b_fm, b_S[7]], [b_fm])
            oTr = Tall[:, S:S + S // 2].bitcast(BF16)
            b_oTr = Buf("oTr")
            for G in range(4):
                sl = slice(G * 512, (G + 1) * 512)
                pt, pb = K.bank()
                K.op("pe", lambda e: e.matmul(pt[:, :], lhsT=g2b[:, P * 128:(P + 1) * 128], rhs=sglT[:, sl], start=True, stop=True),
                     r=[b_g2b, b_sgl], w=[pb])
                dve(lambda e: e.tensor_tensor(out=oTr[:, sl], in0=fm[:, sl], in1=pt[:, :], op=ALU.mult), [pb, b_fm, b_sq], [b_oTr])
            wo_s = Tall[:, 0:1024]
            b_wo_s = Buf("rwo_s")
            wo_b = Tall[:, 1024:1536].bitcast(BF16)
            b_wo_b = Buf("rwo_b")
            K.barrier()
            K.dma(wo_s, self.odd_w_out[P * 128:(P + 1) * 128, :], w=[b_wo_s])
            K.op("pool", lambda e: e.tensor_tensor(out=wo_b, in0=wo_s, in1=self.bc[2], op=ALU.mult), r=[b_wo_s, self.b_bc[2]], w=[b_wo_b])
            for t in range(NT):
                for hf in range(2):
                    pt, pb = K.bank()
                    K.op("pe", lambda e: e.matmul(pt[:, :], lhsT=oTr[:, t * 128:(t + 1) * 128], rhs=wo_b[:, hf * 512:(hf + 1) * 512],
                                                  start=True, stop=True), r=[b_oTr, b_wo_b], w=[pb])
                    xs = self.x_sb[:, t, hf * 512:(hf + 1) * 512]
                    dve(lambda e: e.tensor_tensor(out=xs, in0=pt[:, :], in1=xs, op=ALU.add), [pb, self.xb[t]], [self.xb[t]])
            K.barrier()
        A.release()
        A.release_top(HTB)


for _n, _f in list(vars(_OddMixin).items()):
    if callable(_f) and not _n.startswith("__"):
        setattr(_Prog, _n, _f)
```
